# Optimizing a Trainium2 kernel written in Bass

```python
import math
import jax, jax.numpy as jnp
from jax import lax
import numpy as np

D_MODEL = 2048
BATCH = 2
SEQ = 4096
DEPTH = 1

MEM_LEN = 256
ATT_HEADS = 8
HEAD_DIM = 128
ATT_WIDTH = ATT_HEADS * HEAD_DIM
IDX_HEADS = 16
IDX_DIM = 64
TOPK_MAX = 256
Q_BLOCK = 128
POOL_WINDOWS = (2, 4, 8, 16)
N_POOL = len(POOL_WINDOWS)
POOL_GROUP = 128
POOL_WIDTH = N_POOL * POOL_GROUP
MEM_HEADS = 4
MEM_WIDTH = MEM_HEADS * HEAD_DIM
N_BRANCH = 3
D_FF = 5632
ALPHA = (2 * DEPTH) ** 0.25
BETA = (8 * DEPTH) ** -0.25
LN_EPS = 1e-5
SPLIT_SIZES = (ATT_WIDTH, ATT_WIDTH, ATT_WIDTH, IDX_HEADS * IDX_DIM, IDX_DIM, IDX_HEADS, POOL_WIDTH, MEM_WIDTH, N_BRANCH * D_MODEL)
SPLIT_IDX = tuple(int(v) for v in np.cumsum(SPLIT_SIZES)[:-1])
D_IN = int(sum(SPLIT_SIZES))

kernel_name = "hybrid_dsa_pool_mem_macaron_deepnorm"

f32 = jnp.float32


def layer_norm(x, g, b):
    x32 = x.astype(f32)
    mu = jnp.mean(x32, axis=-1, keepdims=True)
    var = jnp.mean(jnp.square(x32 - mu), axis=-1, keepdims=True)
    y = (x32 - mu) * lax.rsqrt(var + LN_EPS) * g.astype(f32) + b.astype(f32)
    return y.astype(x.dtype)


def swiglu(x, w_up, w_down):
    a, u = jnp.split(x @ w_up, 2, axis=-1)
    return (jax.nn.silu(a) * u) @ w_down


def alibi_slopes(n):
    return jnp.exp2(-8.0 * jnp.arange(1, n + 1, dtype=f32) / n)


def dsa_attention(q, k, v, q_idx, k_idx, w_idx):
    B, S = q.shape[0], q.shape[1]
    topk = min(TOPK_MAX, S // 4)
    nb = S // Q_BLOCK
    slopes = alibi_slopes(ATT_HEADS)
    key_pos = jnp.arange(S, dtype=jnp.int32)
    scale = HEAD_DIM ** -0.5

    def blockify(a):
        return a.reshape((B, nb, Q_BLOCK) + a.shape[2:]).swapaxes(0, 1)

    def one_block(args):
        qb, qib, wb, start = args
        q_pos = start + jnp.arange(Q_BLOCK, dtype=jnp.int32)
        causal = key_pos[None, :] <= q_pos[:, None]
        logits = jnp.einsum('bqhd,bsd->bqhs', qib, k_idx, preferred_element_type=f32) * (IDX_DIM ** -0.5)
        wts = wb.astype(f32) * (IDX_HEADS ** -0.5)
        score = jnp.einsum('bqh,bqhs->bqs', wts, jax.nn.relu(logits))
        score = jnp.where(causal[None], score, -jnp.inf)
        _, sel = lax.top_k(score, topk)
        valid = sel <= q_pos[None, :, None]
        k_sel = jax.vmap(lambda a, i: a[i])(k, sel)
        v_sel = jax.vmap(lambda a, i: a[i])(v, sel)
        s = jnp.einsum('bqhd,bqkhd->bqhk', qb, k_sel, preferred_element_type=f32) * scale
        dist = (q_pos[None, :, None] - sel).astype(f32)
        s = s - slopes[None, None, :, None] * dist[:, :, None, :]
        s = jnp.where(valid[:, :, None, :], s, -jnp.inf)
        p = jax.nn.softmax(s, axis=-1).astype(v.dtype)
        return jnp.einsum('bqhk,bqkhd->bqhd', p, v_sel)

    starts = jnp.arange(nb, dtype=jnp.int32) * Q_BLOCK
    out = lax.map(one_block, (blockify(q), blockify(q_idx), blockify(w_idx), starts))
    return out.swapaxes(0, 1).reshape(B, S, ATT_HEADS * HEAD_DIM)


def multiscale_pool(u, w_pool, pool_scale):
    B, S, _ = u.shape
    ug = u.reshape(B, S, N_POOL, POOL_GROUP).astype(f32)
    c = jnp.pad(jnp.cumsum(ug, axis=1), ((0, 0), (1, 0), (0, 0), (0, 0)))
    t = jnp.arange(S)
    outs = []
    for g, w in enumerate(POOL_WINDOWS):
        lo = jnp.maximum(t - w + 1, 0)
        cnt = (t - lo + 1).astype(f32)
        mean = (c[:, t + 1, g] - c[:, lo, g]) / cnt[None, :, None]
        outs.append(mean - ug[:, :, g])
    pooled = jnp.stack(outs, axis=2).astype(u.dtype)
    mixed = jnp.einsum('bsgc,gcd->bsgd', pooled, w_pool)
    return mixed.reshape(B, S, POOL_WIDTH) * pool_scale


def memory_attention(qm, mem, w_mem_kv):
    B, S = qm.shape[0], qm.shape[1]
    kv = (mem @ w_mem_kv).reshape(B, mem.shape[1], 2, MEM_HEADS, HEAD_DIM)
    km, vm = kv[:, :, 0], kv[:, :, 1]
    s = jnp.einsum('bqhd,bmhd->bhqm', qm, km, preferred_element_type=f32) * (HEAD_DIM ** -0.5)
    p = jax.nn.softmax(s, axis=-1).astype(vm.dtype)
    return jnp.einsum('bhqm,bmhd->bqhd', p, vm).reshape(B, S, MEM_WIDTH)


def token_mix(h, mem, w_in, b_gate, w_mem_kv, w_pool, pool_scale, w_br_att, w_br_pool, w_br_mem, w_out):
    B, S, D = h.shape
    z = h @ w_in
    q, k, v, qi, ki, wi, u, qm, gl = jnp.split(z, SPLIT_IDX, axis=-1)
    hd = (B, S, ATT_HEADS, HEAD_DIM)
    a = dsa_attention(q.reshape(hd), k.reshape(hd), v.reshape(hd), qi.reshape(B, S, IDX_HEADS, IDX_DIM), ki, wi)
    p = multiscale_pool(u, w_pool, pool_scale)
    m = memory_attention(qm.reshape(B, S, MEM_HEADS, HEAD_DIM), mem, w_mem_kv)
    gates = jax.nn.sigmoid((gl + b_gate).astype(f32)).astype(h.dtype).reshape(B, S, N_BRANCH, D)
    y = gates[:, :, 0] * (a @ w_br_att) + gates[:, :, 1] * (p @ w_br_pool) + gates[:, :, 2] * (m @ w_br_mem)
    return y @ w_out


def setup_inputs(seed: int = 0) -> dict:
    key = jax.random.key(seed)
    ks = jax.random.split(key, 21)
    L, D = DEPTH, D_MODEL

    def n(k, shape, scale):
        return jax.random.normal(k, shape, jnp.float32) * scale

    return {
        "x": n(ks[0], (BATCH, SEQ, D), 1.0),
        "mem": n(ks[1], (BATCH, MEM_LEN, D), 1.0),
        "w_ffn1_up": n(ks[2], (L, D, 2 * D_FF), D ** -0.5),
        "w_ffn1_down": n(ks[3], (L, D_FF, D), BETA * D_FF ** -0.5),
        "ln1_g": 1.0 + n(ks[4], (L, D), 0.02),
        "ln1_b": n(ks[5], (L, D), 0.02),
        "w_in": n(ks[6], (L, D, D_IN), D ** -0.5),
        "b_gate": n(ks[7], (L, N_BRANCH * D), 0.1),
        "w_mem_kv": n(ks[8], (L, D, 2 * MEM_WIDTH), D ** -0.5),
        "w_pool": n(ks[9], (L, N_POOL, POOL_GROUP, POOL_GROUP), POOL_GROUP ** -0.5),
        "pool_scale": 1.0 + n(ks[10], (L, POOL_WIDTH), 0.02),
        "w_br_att": n(ks[11], (L, ATT_WIDTH, D), ATT_WIDTH ** -0.5),
        "w_br_pool": n(ks[12], (L, POOL_WIDTH, D), POOL_WIDTH ** -0.5),
        "w_br_mem": n(ks[13], (L, MEM_WIDTH, D), MEM_WIDTH ** -0.5),
        "w_out": n(ks[14], (L, D, D), BETA * D ** -0.5),
        "ln2_g": 1.0 + n(ks[15], (L, D), 0.02),
        "ln2_b": n(ks[16], (L, D), 0.02),
        "w_ffn2_up": n(ks[17], (L, D, 2 * D_FF), D ** -0.5),
        "w_ffn2_down": n(ks[18], (L, D_FF, D), BETA * D_FF ** -0.5),
        "ln3_g": 1.0 + n(ks[19], (L, D), 0.02),
        "ln3_b": n(ks[20], (L, D), 0.02),
    }


def reference(x, mem, w_ffn1_up, w_ffn1_down, ln1_g, ln1_b, w_in, b_gate, w_mem_kv, w_pool, pool_scale, w_br_att, w_br_pool, w_br_mem, w_out, ln2_g, ln2_b, w_ffn2_up, w_ffn2_down, ln3_g, ln3_b):
    h = x
    for l in range(DEPTH):
        h = layer_norm(ALPHA * h + 0.5 * swiglu(h, w_ffn1_up[l], w_ffn1_down[l]), ln1_g[l], ln1_b[l])
        mix = token_mix(h, mem, w_in[l], b_gate[l], w_mem_kv[l], w_pool[l], pool_scale[l], w_br_att[l], w_br_pool[l], w_br_mem[l], w_out[l])
        h = layer_norm(ALPHA * h + mix, ln2_g[l], ln2_b[l])
        h = layer_norm(ALPHA * h + 0.5 * swiglu(h, w_ffn2_up[l], w_ffn2_down[l]), ln3_g[l], ln3_b[l])
    return h
```

```python
import numpy as np
import concourse.bass as bass
import concourse.mybir as mybir
from concourse.bass_utils import run_bass_kernel_spmd

F32 = mybir.dt.float32
BF16 = mybir.dt.bfloat16
AF = mybir.ActivationFunctionType
ALU = mybir.AluOpType

D = 2048
SEQ = 4096
NCORE = 8
T = 1024
NSLOT = 8
DFF = 5632
NFC = DFF // 128
NDC = D // 128
ALPHA = 2.0 ** 0.25
LN_EPS = 1e-5
DIN = 11344

DEBUG_STAGE = None
TEST_SKIP = False


class Sched:
    COMPUTE = ("pe", "act", "dve", "pool")
    ALL = ("pe", "act", "dve", "pool", "sp")

    def __init__(self, nc, sems, dsems):
        self.nc = nc
        self.sem = sems
        self.dsem = dsems
        self.cnt = {e: 0 for e in self.COMPUTE}
        self.ops = {e: [] for e in self.ALL}
        self.waited = {e: {} for e in self.ALL}
        self.lastw = {}
        self.readers = {}
        self.dma_k = 0
        self.dma_val = [0] * len(dsems)
        self.dma_owner = [None] * len(dsems)
        self.nops = 0

    def _deps(self, eng, r, w):
        raw = {}
        oth = {}

        def put(d, tok):
            k, v = tok
            if d.get(k, 0) < v:
                d[k] = v

        for res in r:
            if res in self.lastw:
                put(raw, self.lastw[res])
        for res in w:
            if res in self.lastw:
                put(oth, self.lastw[res])
            for k, v in self.readers.get(res, {}).items():
                put(oth, (k, v))
        waits = {}
        for k, v in raw.items():
            if k == eng and eng == "pe":
                continue
            put(waits, (k, v))
        for k, v in oth.items():
            if k == eng:
                continue
            put(waits, (k, v))
        out = []
        wd = self.waited[eng]
        for k, v in waits.items():
            if wd.get(k, 0) >= v:
                continue
            wd[k] = v
            out.append((k, v))
        return out

    def _commit(self, tok, r, w):
        for res in w:
            self.lastw[res] = tok
            self.readers[res] = {}
        for res in r:
            d = self.readers.setdefault(res, {})
            if d.get(tok[0], 0) < tok[1]:
                d[tok[0]] = tok[1]

    def add(self, eng, fn, r=(), w=()):
        waits = self._deps(eng, r, w)
        self.cnt[eng] += 1
        tok = (eng, self.cnt[eng])
        self.ops[eng].append((waits, fn, tok, "c"))
        self._commit(tok, r, w)
        self.nops += 1
        return tok

    def dma(self, q, out, in_, r=(), w=(), **kw):
        waits = self._deps(q, r, w)
        i = self.dma_k % len(self.dsem)
        self.dma_k += 1
        prev = self.dma_val[i]
        key = ("d", i)
        if prev > 0 and self.waited[q].get(key, 0) < prev:
            self.waited[q][key] = prev
            waits.append((key, prev))
        self.dma_val[i] = prev + 16
        self.dma_owner[i] = q
        tok = (key, prev + 16)
        self.ops[q].append((waits, lambda e: e.dma_start(out=out, in_=in_, **kw), tok, "d"))
        self._commit(tok, r, w)
        self.nops += 1
        return tok

    def coll(self, i, fn, r=(), w=()):
        waits = self._deps("pool", r, w)
        tok = (("c", i), 1)
        self.cc_pending = getattr(self, "cc_pending", []) + [tok]
        self.ops["pool"].append((waits, fn, tok, "cc"))
        self._commit(tok, r, w)
        return tok

    def _semobj(self, key):
        if isinstance(key, tuple):
            if key[0] == "c":
                return self.csem[key[1]]
            return self.dsem[key[1]]
        return self.sem[key]

    def flush(self):
        nc = self.nc
        with nc.Block() as block:
            decos = {"sp": block.sync, "act": block.scalar, "dve": block.vector,
                     "pool": block.gpsimd, "pe": block.tensor}
            for eng in self.ALL:
                ops = self.ops[eng]
                tail = [(("d", i), self.dma_val[i]) for i in range(len(self.dsem))
                        if self.dma_owner[i] == eng and self.dma_val[i] > 0]
                if eng == "pool":
                    tail = tail + list(getattr(self, "cc_pending", []))

                def body(e, ops=ops, eng=eng, tail=tail):
                    for waits, fn, tok, kind in ops:
                        for k, v in waits[1:]:
                            e.wait_ge(self._semobj(k), v)
                        ins = fn(e)
                        if waits:
                            ins.wait_op(self._semobj(waits[0][0]), waits[0][1], "sem-ge")
                        if kind == "c":
                            ins.then_inc(self.sem[eng], 1)
                        elif kind == "cc":
                            ins.then_inc(self.csem[tok[0][1]])
                        else:
                            ins.then_inc(self.dsem[tok[0][1]], 16)
                    for k, v in tail:
                        e.wait_ge(self._semobj(k), v)

                decos[eng](body)
        for eng in self.ALL:
            self.ops[eng] = []
            for i in range(len(self.dsem)):
                if self.dma_owner[i] is not None:
                    self.waited[eng][("d", i)] = self.dma_val[i]
            for c in self.COMPUTE:
                self.waited[eng][c] = self.cnt[c]
        self.lastw = {}
        self.readers = {}
        self.cc_pending = []


class Ring:
    def __init__(self, tiles, name):
        self.tiles = tiles
        self.name = name
        self.i = 0

    def next(self):
        k = self.i % len(self.tiles)
        self.i += 1
        return self.tiles[k], (self.name, k)


def build_program(stage=None):
    from contextlib import ExitStack
    nc = bass.Bass("TRN2", target_bir_lowering=False)

    def din(name, shape, dt=F32):
        return nc.dram_tensor(name, list(shape), dt, kind="ExternalInput").ap()

    x_d = din("x", [T, D])
    w1u_d = din("w_ffn1_up", [D, 2 * DFF])
    w1d_d = din("w_ffn1_down", [DFF, D])
    ln1g_d = din("ln1_g", [1, D])
    ln1b_d = din("ln1_b", [1, D])
    ident_d = din("ident", [128, 128])
    win_d = din("w_in", [D, DIN])
    bgate_d = din("b_gate", [128, 48])
    wmkv_d = din("w_mem_kv", [D, 1024])
    mem_d = din("mem", [256, D])
    wpool_d = din("w_pool", [128, 4, 128])
    pscale_d = din("pool_scale", [128, 4])
    wba_d = din("w_br_att", [1024, D])
    wbp_d = din("w_br_pool", [512, D])
    wbm_d = din("w_br_mem", [512, D])
    wout_d = din("w_out", [D, D])
    ln2g_d = din("ln2_g", [1, D])
    ln2b_d = din("ln2_b", [1, D])
    w2u_d = din("w_ffn2_up", [D, 2 * DFF])
    w2d_d = din("w_ffn2_down", [DFF, D])
    ln3g_d = din("ln3_g", [1, D])
    ln3b_d = din("ln3_b", [1, D])
    qpos_d = din("qpos", [128, NSLOT])
    iota_d = din("iota512", [1, 512])
    invc0_d = din("invc0", [1, 4 * 128])
    psel_d = din("psel", [1, 4])
    pow2_d = din("pow2", [1, 32])
    slopes_d = din("slopes", [1, 8])
    c512_d = din("c512", [1, 8])
    h1_scr = [nc.dram_tensor("h1_scr%d" % i, [512, D], F32).ap() for i in range(2)]
    k_loc = [nc.dram_tensor("k_loc%d" % i, [512, 1024], BF16).ap() for i in range(2)]
    k_all = [nc.dram_tensor("k_all%d" % i, [4 * 512, 1024], BF16).ap() for i in range(2)]
    v_loc = [nc.dram_tensor("v_loc%d" % i, [512, 1024], BF16).ap() for i in range(2)]
    v_all = [nc.dram_tensor("v_all%d" % i, [4 * 512, 1024], BF16).ap() for i in range(2)]
    ki_loc = nc.dram_tensor("ki_loc", [64, 1024], BF16).ap()
    ki_all = nc.dram_tensor("ki_all", [4 * 64, 1024], BF16).ap()
    ut_loc = nc.dram_tensor("ut_loc", [NSLOT * 128, 64], F32).ap()
    ut_all = nc.dram_tensor("ut_all", [4 * NSLOT * 128, 64], F32).ap()
    out_d = nc.dram_tensor("out", [T, D], F32, kind="ExternalOutput").ap()

    es = ExitStack()

    def sb(name, shape, dt):
        return es.enter_context(nc.sbuf_tensor("sb_" + name, list(shape), dt))

    sems = {e: es.enter_context(nc.semaphore("s_" + e)) for e in Sched.COMPUTE}
    dsems = [es.enter_context(nc.semaphore("d%d" % i)) for i in range(24)]
    csems = [es.enter_context(nc.semaphore("c%d" % i)) for i in range(6)]
    S = Sched(nc, sems, dsems)
    S.csem = csems

    bank = [es.enter_context(nc.psum_tensor("bank%d" % i, [128, 512], F32)) for i in range(8)]

    def bk(i):
        return ("bank", i)

    hT = sb("hT", [128, NDC, T], BF16)
    acc = sb("acc", [128, NSLOT, D], F32)
    identb = sb("identb", [128, 128], BF16)
    identf = sb("identf", [128, 128], F32)
    wst = Ring([sb("wst%d" % i, [128, 2048], F32) for i in range(3)], "wst")
    wbf = Ring([sb("wbf%d" % i, [128, 2048], BF16) for i in range(3)], "wbf")
    regG = sb("regG", [128, 24 * T], BF16)
    xbf = Ring([sb("xbf%d" % i, [128, D], BF16) for i in range(2)], "xbf")
    sa_ring = Ring([sb("sa%d" % i, [128, 512], F32) for i in range(2)], "sa")
    stats = sb("stats", [128, 4, 6], F32)
    mv = sb("mv", [128, 2], F32)
    rstd = sb("rstd", [128, 1], F32)
    epsb = sb("epsb", [128, 1], F32)

    cast_rr = [0]

    def cast(out, in_, r, w):
        engs = ("act", "dve", "pool")
        eng = engs[cast_rr[0] % 3]
        cast_rr[0] += 1
        if eng == "act":
            S.add("act", lambda e: e.copy(out=out, in_=in_), r=r, w=w)
        else:
            S.add(eng, lambda e: e.tensor_copy(out=out, in_=in_), r=r, w=w)

    wbf_cur = [wbf]

    def load_w(src_ap, view):
        st, st_key = wst.next()
        bf, bf_key = wbf_cur[0].next()
        S.dma("sp", view(st), src_ap, w=[st_key])
        cast(view(bf), view(st), r=[st_key], w=[bf_key])
        return view(bf), bf_key

    S.dma("sp", identf[:, :], ident_d, w=["identf"])
    S.add("dve", lambda e: e.tensor_copy(out=identb[:, :], in_=identf[:, :]), r=["identf"], w=["identb"])
    S.add("dve", lambda e: e.memset(epsb[:, :], LN_EPS), w=["epsb"])

    tr_rr = [0]

    def to_hT(j, src_f32, src_key):
        xb, xb_key = xbf.next()
        S.add("dve", lambda e: e.tensor_copy(out=xb[:, :], in_=src_f32), r=[src_key], w=[xb_key])
        for half in range(2):
            bi = 6 + (tr_rr[0] % 2)
            tr_rr[0] += 1
            pt = bank[bi][:, :].bitcast(BF16)
            for c8 in range(8):
                c = half * 8 + c8
                S.add("pe", lambda e, c=c, c8=c8, pt=pt: e.transpose(
                    out=pt[:, c8 * 128:(c8 + 1) * 128], in_=xb[:, c * 128:(c + 1) * 128], identity=identb[:, :]),
                    r=[xb_key, "identb"], w=[bk(bi)])
            dst = hT[:, half * 8:(half + 1) * 8, j * 128:(j + 1) * 128]
            src = pt.rearrange("p (c n) -> p c n", c=8)
            S.add("act", lambda e, dst=dst, src=src: e.copy(out=dst, in_=src),
                  r=[bk(bi)], w=[("hT", j)])

    xin = Ring([regG[:, 0:2 * D].bitcast(F32), regG[:, 2 * D:4 * D].bitcast(F32)], "xin")
    for j in range(NSLOT):
        xt, xt_key = xin.next()
        S.dma("sp", xt, x_d[j * 128:(j + 1) * 128, :], w=[xt_key])
        S.add("act", lambda e, j=j, xt=xt: e.mul(out=acc[:, j, :], in_=xt, mul=ALPHA),
              r=[xt_key], w=[("acc", j)])
        to_hT(j, xt, xt_key)
    if stage == 0:
        for j in range(NSLOT):
            S.dma("sp", out_d[j * 128:(j + 1) * 128, :], acc[:, j, :], r=[("acc", j)])
        S.flush()
        es.close()
        return nc
    S.flush()

    hT_all = [("hT", j) for j in range(NSLOT)]

    def ffn(wu_d, wd_d):
        gT = regG
        wu_v = wu_d.rearrange("(c p) n -> p c n", p=128)
        wd_v = wd_d.rearrange("(c p) n -> p c n", p=128)
        groups = [(0, 24), (24, 20)]
        bno = [0]
        v3 = lambda t: t[:, :].rearrange("p (c n) -> p c n", c=16)
        v4 = lambda t: t[:, :].rearrange("p (c n) -> p c n", c=4)
        for (f0, nf) in groups:
            for fl in range(nf):
                f = f0 + fl
                wa, wa_key = load_w(wu_v[:, :, f * 128:(f + 1) * 128], v3)
                wu, wu_key = load_w(wu_v[:, :, DFF + f * 128:DFF + (f + 1) * 128], v3)
                for half in range(2):
                    ia = bno[0] % 4
                    iu = (bno[0] + 1) % 4
                    bno[0] += 2
                    for (wt, wk, ib) in ((wa, wa_key, ia), (wu, wu_key, iu)):
                        for c in range(NDC):
                            S.add("pe", lambda e, wt=wt, ib=ib, c=c, half=half: e.matmul(
                                out=bank[ib][:, :], lhsT=wt[:, c, :], rhs=hT[:, c, half * 512:(half + 1) * 512],
                                start=(c == 0), stop=(c == NDC - 1)),
                                r=[wk] + hT_all, w=[bk(ib)])
                    sa, sa_key = sa_ring.next()
                    S.add("act", lambda e, sa=sa, ia=ia: e.activation(out=sa[:, :], in_=bank[ia][:, :], func=AF.Silu),
                          r=[bk(ia)], w=[sa_key])
                    gdst = gT[:, fl * T + half * 512: fl * T + (half + 1) * 512]
                    S.add("dve", lambda e, gdst=gdst, sa=sa, iu=iu: e.tensor_tensor(
                        out=gdst, in0=sa[:, :], in1=bank[iu][:, :], op=ALU.mult),
                        r=[sa_key, bk(iu)], w=[("gT", fl, half)])
            g_all = [("gT", fl, h) for fl in range(nf) for h in range(2)]
            for dq in range(4):
                for u4 in range(nf // 4):
                    c0 = f0 + u4 * 4
                    wd, wd_key = load_w(wd_v[:, c0:c0 + 4, dq * 512:(dq + 1) * 512], v4)
                    for k4 in range(4):
                        fl = u4 * 4 + k4
                        for j in range(NSLOT):
                            S.add("pe", lambda e, fl=fl, j=j, wd=wd, k4=k4, nf=nf: e.matmul(
                                out=bank[j][:, :], lhsT=gT[:, fl * T + j * 128: fl * T + (j + 1) * 128],
                                rhs=wd[:, k4, :], start=(fl == 0), stop=(fl == nf - 1)),
                                r=[wd_key] + g_all, w=[bk(j)])
                for j in range(NSLOT):
                    dst = acc[:, j, dq * 512:(dq + 1) * 512]
                    S.add("dve", lambda e, dst=dst, j=j: e.scalar_tensor_tensor(
                        out=dst, in0=bank[j][:, :], scalar=0.5, in1=dst, op0=ALU.mult, op1=ALU.add),
                        r=[bk(j), ("acc", j)], w=[("acc", j)])
        S.flush()

    def layernorm(g_d, b_d, store_d=None, make_hT=True, post_scale=None):
        gt = regG[:, 0:2 * D].bitcast(F32)
        bt = regG[:, 2 * D:4 * D].bitcast(F32)
        S.dma("sp", gt, g_d.partition_broadcast(128) if False else g_d.broadcast_to([128, D]), w=["lng"])
        S.dma("sp", bt, b_d.broadcast_to([128, D]), w=["lnb"])
        for j in range(NSLOT):
            a_j = acc[:, j, :]
            for q in range(4):
                S.add("dve", lambda e, j=j, q=q: e.bn_stats(out=stats[:, q, :], in_=acc[:, j, q * 512:(q + 1) * 512]),
                      r=[("acc", j)], w=[("stats", q)])
            S.add("dve", lambda e: e.bn_aggr(out=mv[:, :], in_=stats[:, :, :]),
                  r=[("stats", q) for q in range(4)], w=["mv"])
            S.add("act", lambda e: e.activation(out=rstd[:, :], in_=mv[:, 1:2], func=AF.Sqrt, bias=epsb[:, :]),
                  r=["mv", "epsb"], w=["rstd"])
            S.add("dve", lambda e: e.reciprocal(out=rstd[:, :], in_=rstd[:, :]), r=["rstd"], w=["rstd"])
            S.add("dve", lambda e, a_j=a_j: e.tensor_scalar(
                out=a_j, in0=a_j, scalar1=mv[:, 0:1], scalar2=rstd[:, :], op0=ALU.subtract, op1=ALU.mult),
                r=[("acc", j), "mv", "rstd"], w=[("acc", j)])
            S.add("pool", lambda e, a_j=a_j: e.tensor_tensor(out=a_j, in0=a_j, in1=gt, op=ALU.mult),
                  r=[("acc", j), "lng"], w=[("acc", j)])
            S.add("dve", lambda e, a_j=a_j: e.tensor_tensor(out=a_j, in0=a_j, in1=bt, op=ALU.add),
                  r=[("acc", j), "lnb"], w=[("acc", j)])
            if store_d is not None:
                sd = store_d[j // 4][(j % 4) * 128:(j % 4 + 1) * 128, :] if isinstance(store_d, list) else store_d[j * 128:(j + 1) * 128, :]
                S.dma("sp", sd, a_j, r=[("acc", j)])
            if make_hT:
                to_hT(j, a_j, ("acc", j))
            if post_scale is not None:
                S.add("act", lambda e, a_j=a_j: e.mul(out=a_j, in_=a_j, mul=post_scale),
                      r=[("acc", j)], w=[("acc", j)])
        S.flush()

    ffn(w1u_d, w1d_d)
    if stage == 1:
        layernorm(ln1g_d, ln1b_d, store_d=out_d, make_hT=True)
        es.close()
        return nc
    layernorm(ln1g_d, ln1b_d, store_d=h1_scr, make_hT=True)

    accb = acc[:, :, :].rearrange("p a b -> p (a b)")
    score = accb[:, 0:4096]
    logit = accb[:, 4096:8192]
    rel = accb[:, 8192:12288]
    mb = accb[:, 12288:14336].bitcast(BF16)
    pj = accb[:, 14336:16384].bitcast(BF16)
    uT = accb[:, 0:4096].rearrange("p (g t) -> p g t", g=4)
    vtok = accb[:, 4096:8192].bitcast(BF16).rearrange("p (j n) -> p j n", j=NSLOT)
    qT = regG[:, 0:8 * T].rearrange("p (h t) -> p h t", h=8)
    qiT = regG[:, 8 * T:16 * T].rearrange("p (h t) -> p h t", h=8)
    qmT = regG[:, 16 * T:20 * T].rearrange("p (h t) -> p h t", h=4)
    pT = regG[:, 20 * T:24 * T].rearrange("p (h t) -> p h t", h=4)
    hTf = hT[:, :, :].rearrange("p c t -> p (c t)")
    kiT = hTf[:, 0:4096]
    PT = hTf[:, 4096:8192].rearrange("p (b t) -> p b t", b=32)
    kh_ring = Ring([hTf[:, 8192:12288], wst.tiles[0][:, :].bitcast(BF16)], "kh")
    vh_ring = Ring([hTf[:, 12288:16384].rearrange("p (b d) -> p b d", b=32),
                    wst.tiles[1][:, :].bitcast(BF16).rearrange("p (b d) -> p b d", b=32)], "vh")
    memT = accb[:, 8192:10240].bitcast(BF16).rearrange("p (c m) -> p c m", c=16)
    wi_t = sb("wi_t", [128, NSLOT, 16], F32)
    absw = sb("absw", [128, NSLOT, 16], F32)
    sgnw = sb("sgnw", [128, NSLOT, 16], F32)
    qpos = sb("qpos", [128, NSLOT], F32)
    qoff = sb("qoff", [128, 8], F32)
    iota = sb("iota", [128, 512], F32)
    pow2 = sb("pow2", [128, 32], F32)
    steps = sb("steps", [128, 32], F32)
    slopes = sb("slopes", [128, 8], F32)
    psel = sb("psel", [128, 4], F32)
    invc0 = sb("invc0", [128, 4, 128], F32)
    pscale = sb("pscale", [128, 4], F32)
    bgate = sb("bgate", [128, 48], F32)
    wpool_f = accb[:, 15488:16000].rearrange("p (g d) -> p g d", g=4)
    wpool_b = sb("wpool_b", [128, 4, 128], BF16)
    c512 = sb("c512", [128, 8], F32)
    negq = sb("negq", [128, 8], F32)
    sm = sb("sm", [128, 16], F32)
    lo, mid, cnt, ge, w0, rmax, rsum, rinv, hi = (sm[:, i:i + 1] for i in range(9))
    halo = accb[:, 14336:14912].rearrange("p (g t) -> p g t", g=4)
    halo2 = accb[:, 14912:15488].rearrange("p (g t) -> p g t", g=4)
    tails = accb[:, 12288:14336].rearrange("p (r j n) -> p r j n", r=4, j=NSLOT)
    kmT = sb("kmT", [128, 4, 256], BF16)
    vmt = sb("vmt", [128, 2, 512], BF16)

    for (dst, src, key) in ((qpos[:, :], qpos_d, "qpos"), (bgate[:, :], bgate_d, "bgate"),
                            (pscale[:, :], pscale_d, "pscale"), (wpool_f[:, :, :], wpool_d, "wpool_f")):
        S.dma("sp", dst, src, w=[key])
    for (dst, src, key, n) in ((iota[:, :], iota_d, "iota", 512), (pow2[:, :], pow2_d, "pow2", 32),
                               (slopes[:, :], slopes_d, "slopes", 8), (psel[:, :], psel_d, "psel", 4), (c512[:, :], c512_d, "c512", 8),
                               (invc0[:, :, :].rearrange("p g t -> p (g t)"), invc0_d, "invc0", 512)):
        S.dma("sp", dst, src.broadcast_to([128, n]), w=[key])
    S.add("dve", lambda e: e.tensor_copy(out=wpool_b[:, :, :], in_=wpool_f[:, :, :]), r=["wpool_f"], w=["wpool_b"])

    v3 = lambda t: t[:, :].rearrange("p (c n) -> p c n", c=16)
    v4 = lambda t: t[:, :].rearrange("p (c n) -> p c n", c=4)
    win_v = win_d.rearrange("(c p) n -> p c n", p=128)
    ev_rr = [0]

    def evac(out, in_, r, w, scale=None):
        eng = ("act", "dve")[ev_rr[0] % 2]
        ev_rr[0] += 1
        if eng == "act":
            S.add("act", lambda e: e.copy(out=out, in_=in_), r=r, w=w)
        else:
            S.add("dve", lambda e: e.tensor_copy(out=out, in_=in_), r=r, w=w)

    fb = [0]

    def proj_feat(w_v, col0, ncols, act_T, act_keys, nk, ntok, consume):
        view = lambda t: t[:, 0:nk * ncols].rearrange("p (c n) -> p c n", c=nk)
        wt, wk = load_w(w_v[:, 0:nk, col0:col0 + ncols], view)
        for half in range((ntok + 511) // 512):
            n = min(512, ntok - half * 512)
            ib = fb[0] % 4
            fb[0] += 1
            for c in range(nk):
                S.add("pe", lambda e, c=c, ib=ib, half=half, n=n: e.matmul(
                    out=bank[ib][0:ncols, 0:n], lhsT=wt[:, c, :], rhs=act_T[:, c, half * 512:half * 512 + n],
                    start=(c == 0), stop=(c == nk - 1)), r=[wk] + act_keys, w=[bk(ib)])
            consume(half, ib, n)

    kst = Ring([xbf.tiles[0], xbf.tiles[1]], "xbf")

    def to_sbuf(dst3, ci, key):
        def f(half, ib, n):
            evac(dst3[:, ci, half * 512:half * 512 + n], bank[ib][:, 0:n], r=[bk(ib)], w=[(key, ci, half)])
        return f

    for ci in range(8):
        proj_feat(win_v, ci * 128, 128, hT, hT_all, NDC, T, to_sbuf(qT, ci, "qT"))
    for ci in range(8):
        proj_feat(win_v, 3072 + ci * 128, 128, hT, hT_all, NDC, T, to_sbuf(qiT, ci, "qiT"))
    for ci in range(4):
        proj_feat(win_v, 4688 + ci * 128, 128, hT, hT_all, NDC, T, to_sbuf(qmT, ci, "qmT"))
    for ci in range(4):
        proj_feat(win_v, 4176 + ci * 128, 128, hT, hT_all, NDC, T, to_sbuf(uT, ci, "uT"))
    for ci in range(9):
        col0, ncols = (1024 + ci * 128, 128) if ci < 8 else (4096, 64)
        st_t, st_k = kst.next()

        def to_stage(half, ib, n, st_t=st_t, st_k=st_k, ncols=ncols):
            evac(st_t[0:ncols, half * 512:half * 512 + n], bank[ib][0:ncols, 0:n], r=[bk(ib)], w=[(st_k, half)])
        proj_feat(win_v, col0, ncols, hT, hT_all, NDC, T, to_stage)
        kdst = k_loc[ci // 4][(ci % 4) * 128:(ci % 4 + 1) * 128, :] if ci < 8 else ki_loc[:, :]
        S.dma("sp", kdst, st_t[0:ncols, 0:T], r=[(st_k, 0), (st_k, 1)], w=["kv_loc"])
    for hv in range(2):
        for u4 in range(4):
            wt, wk = load_w(win_v[:, u4 * 4:u4 * 4 + 4, 2048 + hv * 512:2048 + (hv + 1) * 512], v4)
            for k4 in range(4):
                c = u4 * 4 + k4
                for j in range(NSLOT):
                    S.add("pe", lambda e, c=c, j=j, wt=wt, k4=k4: e.matmul(
                        out=bank[j][:, :], lhsT=hT[:, c, j * 128:(j + 1) * 128], rhs=wt[:, k4, :],
                        start=(c == 0), stop=(c == NDC - 1)), r=[wk] + hT_all, w=[bk(j)])
        for j in range(NSLOT):
            evac(vtok[:, j, hv * 512:(hv + 1) * 512], bank[j][:, :], r=[bk(j)], w=[("vtok", j, hv)])
    for j in range(NSLOT):
        S.dma("sp", v_loc[j // 4][(j % 4) * 128:(j % 4 + 1) * 128, :], vtok[:, j, :],
              r=[("vtok", j, 0), ("vtok", j, 1)], w=["kv_loc"])
    vw = lambda t: t[:, 0:256].rearrange("p (c n) -> p c n", c=16)
    wt, wk = load_w(win_v[:, :, 4160:4176], vw)
    for j in range(NSLOT):
        for c in range(NDC):
            S.add("pe", lambda e, c=c, j=j, wt=wt: e.matmul(
                out=bank[j][:, 0:16], lhsT=hT[:, c, j * 128:(j + 1) * 128], rhs=wt[:, c, :],
                start=(c == 0), stop=(c == NDC - 1)), r=[wk] + hT_all, w=[bk(j)])
        S.add("dve", lambda e, j=j: e.tensor_copy(out=wi_t[:, j, :], in_=bank[j][:, 0:16]), r=[bk(j)], w=["wi_t"])
    S.add("act", lambda e: e.activation(out=sgnw[:, :, :], in_=wi_t[:, :, :], func=AF.Sign), r=["wi_t"], w=["sgnw"])
    S.add("dve", lambda e: e.scalar_tensor_tensor(out=absw[:, :, :], in0=wi_t[:, :, :], scalar=0.125 * 0.25, in1=sgnw[:, :, :],
                                                   op0=ALU.mult, op1=ALU.mult), r=["wi_t", "sgnw"], w=["absw"])
    for j in range(NSLOT):
        S.dma("sp", ut_loc[j * 128:(j + 1) * 128, :].rearrange("p (g t) -> p g t", g=4),
              uT[:, :, j * 128 + 112:(j + 1) * 128], r=[("uT", g, h) for g in range(4) for h in range(2)], w=["ut_loc"])
    mT_keys = []
    for mbk in range(2):
        xt, xt_key = accb[:, 10240:12288], "memx"
        S.dma("sp", xt, mem_d[mbk * 128:(mbk + 1) * 128, :], w=[xt_key])
        xb, xb_key = kst.next()
        S.add("dve", lambda e, xb=xb, xt=xt: e.tensor_copy(out=xb[:, :], in_=xt), r=[xt_key], w=[xb_key])
        for half in range(2):
            bi = 6 + half
            pt = bank[bi][:, :].bitcast(BF16)
            for c8 in range(8):
                c = half * 8 + c8
                S.add("pe", lambda e, c=c, c8=c8, pt=pt, xb=xb: e.transpose(
                    out=pt[:, c8 * 128:(c8 + 1) * 128], in_=xb[:, c * 128:(c + 1) * 128], identity=identb[:, :]),
                    r=[xb_key, "identb"], w=[bk(bi)])
            S.add("act", lambda e, half=half, mbk=mbk, pt=pt: e.copy(
                out=memT[:, half * 8:(half + 1) * 8, mbk * 128:(mbk + 1) * 128],
                in_=pt.rearrange("p (c n) -> p c n", c=8)), r=[bk(bi)], w=[("memT", mbk, half)])
            mT_keys.append(("memT", mbk, half))
    wmkv_v = wmkv_d.rearrange("(c p) n -> p c n", p=128)
    for ci in range(4):
        def to_km(half, ib, n, ci=ci):
            evac(kmT[:, ci, 0:n], bank[ib][:, 0:n], r=[bk(ib)], w=[("kmT", ci)])
        proj_feat(wmkv_v, ci * 128, 128, memT, mT_keys, NDC, 256, to_km)
    for u4 in range(4):
        wt, wk = load_w(wmkv_v[:, u4 * 4:u4 * 4 + 4, 512:1024], v4)
        for k4 in range(4):
            c = u4 * 4 + k4
            for mbk in range(2):
                S.add("pe", lambda e, c=c, mbk=mbk, wt=wt, k4=k4: e.matmul(
                    out=bank[mbk][:, :], lhsT=memT[:, c, mbk * 128:(mbk + 1) * 128], rhs=wt[:, k4, :],
                    start=(c == 0), stop=(c == NDC - 1)), r=[wk] + mT_keys, w=[bk(mbk)])
    for mbk in range(2):
        evac(vmt[:, mbk, :], bank[mbk][:, :], r=[bk(mbk)], w=[("vmt", mbk)])
    groups = [[0, 1, 2, 3], [4, 5, 6, 7]]
    for ci_, (src_, dst_) in enumerate(((k_loc[0], k_all[0]), (k_loc[1], k_all[1]), (v_loc[0], v_all[0]),
                                        (v_loc[1], v_all[1]), (ki_loc, ki_all))):
        S.coll(ci_, lambda e, src_=src_, dst_=dst_: e.collective_compute(
            "AllGather", ALU.bypass, replica_groups=groups, ins=[src_.opt()], outs=[dst_.opt()]),
            r=["kv_loc"], w=["kv_all"])
    S.coll(5, lambda e: e.collective_compute("AllGather", ALU.bypass, replica_groups=groups,
                                             ins=[ut_loc.opt()], outs=[ut_all.opt()]), r=["ut_loc"], w=["ut_all"])
    for rr in range(4):
        src = ut_all[rr * 1024:(rr + 1) * 1024, :].rearrange("(j p) n -> p j n", p=128)
        S.dma("sp", tails[:, rr, :, :], src, r=["ut_all"], w=["tails"])
    S.flush()
    if stage == 20:
        es.close()
        return nc
    for hp in range(2):
        for rr in range(4):
            dst = kiT[hp * 64:(hp + 1) * 64, :].rearrange("p (c r i) -> p c r i", c=8, r=4)[:, :, rr, :]
            src = ki_all[rr * 64:(rr + 1) * 64, :].rearrange("p (c i) -> p c i", c=8)
            S.dma("sp", dst, src, r=["kv_all"], w=["kiT"])

    u_keys = [("uT", g, h) for g in range(4) for h in range(2)]
    WIN = (2, 4, 8, 16)
    sC = sa_ring.tiles[0][:, 0:432].rearrange("p (g t) -> p g t", g=3)
    for j in range(NSLOT):
        for i in range(4):
            if i == 3 and j == 0:
                continue
            cand = (tails[:, i, j, :] if i < 3 else tails[:, 3, j - 1, :]).rearrange("p (g t) -> p g t", g=4)
            if i == 0:
                S.add("dve", lambda e, cand=cand: e.tensor_scalar(
                    out=halo[:, :, 0:16], in0=cand, scalar1=psel[:, 0:1], scalar2=None, op0=ALU.mult),
                    r=["tails", "psel"], w=["halo"])
            else:
                S.add("dve", lambda e, cand=cand, i=i: e.scalar_tensor_tensor(
                    out=halo[:, :, 0:16], in0=cand, scalar=psel[:, i:i + 1], in1=halo[:, :, 0:16],
                    op0=ALU.mult, op1=ALU.add), r=["tails", "psel", "halo"], w=["halo"])
        S.add("dve", lambda e, j=j: e.tensor_copy(out=halo[:, :, 16:144], in_=uT[:, :, j * 128:(j + 1) * 128]),
              r=u_keys + ["halo"], w=["halo"])
        S.add("dve", lambda e: e.tensor_tensor(out=halo2[:, :, 1:144], in0=halo[:, :, 1:144], in1=halo[:, :, 0:143],
                                                op=ALU.add), r=["halo"], w=["halo2"])
        pb, pb_key = kst.next()
        pooled = pb[:, 0:512].rearrange("p (g t) -> p g t", g=4)

        def emit_pooled(g, src, src_key, j=j, pooled=pooled, pb_key=pb_key):
            if j == 0:
                S.add("dve", lambda e: e.tensor_tensor(out=src, in0=src, in1=invc0[:, g, :], op=ALU.mult),
                      r=[src_key, "invc0"], w=[src_key])
                S.add("dve", lambda e: e.tensor_tensor(out=pooled[:, g, :], in0=src, in1=halo[:, g, 16:144],
                                                        op=ALU.subtract), r=[src_key, "halo"], w=[(pb_key, g)])
            else:
                S.add("dve", lambda e: e.scalar_tensor_tensor(
                    out=pooled[:, g, :], in0=src, scalar=1.0 / WIN[g], in1=halo[:, g, 16:144],
                    op0=ALU.mult, op1=ALU.subtract), r=[src_key, "halo"], w=[(pb_key, g)])

        S.add("dve", lambda e: e.tensor_tensor(out=sC[:, :, 3:144], in0=halo2[:, 1:4, 3:144], in1=halo2[:, 1:4, 1:142],
                                                op=ALU.add), r=["halo2"], w=["sC"])
        emit_pooled(0, halo2[:, 0, 16:144], "halo2")
        S.add("dve", lambda e: e.tensor_tensor(out=halo2[:, 2:4, 7:144], in0=sC[:, 1:3, 7:144], in1=sC[:, 1:3, 3:140],
                                                op=ALU.add), r=["sC", "halo2"], w=["halo2"])
        emit_pooled(1, sC[:, 0, 16:144], "sC")
        S.add("dve", lambda e: e.tensor_tensor(out=sC[:, 2, 15:144], in0=halo2[:, 3, 15:144], in1=halo2[:, 3, 7:136],
                                                op=ALU.add), r=["halo2", "sC"], w=["sC"])
        emit_pooled(2, halo2[:, 2, 16:144], "halo2")
        emit_pooled(3, sC[:, 2, 16:144], "sC")
        for g in range(4):
            ib = fb[0] % 4
            fb[0] += 1
            S.add("pe", lambda e, g=g, ib=ib, pooled=pooled: e.matmul(
                out=bank[ib][:, 0:128], lhsT=wpool_b[:, g, :], rhs=pooled[:, g, :], start=True, stop=True),
                r=["wpool_b", (pb_key, g)], w=[bk(ib)])
            S.add("act", lambda e, g=g, ib=ib, j=j: e.activation(
                out=pT[:, g, j * 128:(j + 1) * 128], in_=bank[ib][:, 0:128], func=AF.Copy, scale=pscale[:, g:g + 1]),
                r=[bk(ib), "pscale"], w=[("pT", j)])
    S.flush()

    NIT = 26
    ATT_SCALE = 128.0 ** -0.5
    for j in range(NSLOT):
        nk = 512 * (j + 1)
        nb = 4 * (j + 1)
        S.add("dve", lambda e, j=j: e.tensor_scalar(out=negq[:, :], in0=c512[:, :], scalar1=qpos[:, j:j + 1],
                                                     scalar2=None, op0=ALU.subtract), r=["c512", "qpos"], w=["negq"])
        for c in range(j + 1):
            S.add("dve", lambda e, c=c: e.tensor_scalar(
                out=rel[:, c * 512:(c + 1) * 512], in0=iota[:, :], scalar1=negq[:, c:c + 1], scalar2=0.0,
                op0=ALU.add, op1=ALU.min), r=["iota", "negq"], w=[("rel", c)])
        for c in range(j + 1):
            for h in range(16 if not TEST_SKIP else 1):
                ib = fb[0] % 4
                fb[0] += 1
                p0 = (h % 2) * 64
                S.add("pe", lambda e, c=c, h=h, ib=ib, p0=p0, j=j: e.matmul(
                    out=bank[ib][:, :], lhsT=qiT[p0:p0 + 64, h // 2, j * 128:(j + 1) * 128],
                    rhs=kiT[p0:p0 + 64, c * 512:(c + 1) * 512], start=True, stop=True),
                    r=[("qiT", h // 2, 0), ("qiT", h // 2, 1), "kiT"], w=[bk(ib)])
                rt, rt_key = sa_ring.next()
                S.add("act", lambda e, rt=rt, ib=ib, h=h, j=j: e.activation(
                    out=rt[:, :], in_=bank[ib][:, :], func=AF.Relu, scale=absw[:, j, h:h + 1]),
                    r=[bk(ib), "absw"], w=[rt_key])
                sc = score[:, c * 512:(c + 1) * 512]
                if h == 0:
                    S.add("dve", lambda e, rt=rt, sc=sc, h=h, j=j: e.tensor_scalar(
                        out=sc, in0=rt[:, :], scalar1=sgnw[:, j, h:h + 1], scalar2=None, op0=ALU.mult),
                        r=[rt_key, "sgnw"], w=[("score", c)])
                else:
                    S.add("dve", lambda e, rt=rt, sc=sc, h=h, j=j: e.scalar_tensor_tensor(
                        out=sc, in0=rt[:, :], scalar=sgnw[:, j, h:h + 1], in1=sc, op0=ALU.mult, op1=ALU.add),
                        r=[rt_key, "sgnw", ("score", c)], w=[("score", c)])
        sc_keys = [("score", c) for c in range(j + 1)]
        S.add("dve", lambda e, nk=nk: e.tensor_reduce(out=hi, in_=score[:, 0:nk], axis=mybir.AxisListType.X, op=ALU.max),
              r=sc_keys, w=["hi"])
        S.add("dve", lambda e, nk=nk: e.tensor_reduce(out=lo, in_=score[:, 0:nk], axis=mybir.AxisListType.X, op=ALU.min),
              r=sc_keys, w=["lo"])
        S.add("dve", lambda e: e.tensor_scalar(out=lo, in0=lo, scalar1=-1.0, scalar2=None, op0=ALU.add), r=["lo"], w=["lo"])
        S.add("dve", lambda e: e.scalar_tensor_tensor(out=w0, in0=hi, scalar=1.0, in1=lo, op0=ALU.add, op1=ALU.subtract),
              r=["hi", "lo"], w=["w0"])
        S.add("dve", lambda e: e.tensor_scalar(out=steps[:, :], in0=pow2[:, :], scalar1=w0, scalar2=None, op0=ALU.mult),
              r=["pow2", "w0"], w=["steps"])
        cm, cm_key = sa_ring.next()
        lastc = score[:, j * 512:(j + 1) * 512]
        S.add("dve", lambda e, cm=cm, j=j: e.tensor_scalar(out=cm[:, :], in0=iota[:, :], scalar1=negq[:, j:j + 1], scalar2=0.0,
                                                            op0=ALU.add, op1=ALU.is_le), r=["iota", "negq"], w=[cm_key])
        S.add("dve", lambda e, cm=cm, lastc=lastc: e.tensor_tensor(out=lastc, in0=lastc, in1=cm[:, :], op=ALU.mult),
              r=[cm_key, ("score", j)], w=[("score", j)])
        S.add("dve", lambda e, cm=cm: e.tensor_scalar(out=cm[:, :], in0=cm[:, :], scalar1=1e30, scalar2=-1e30,
                                                       op0=ALU.mult, op1=ALU.add), r=[cm_key], w=[cm_key])
        S.add("dve", lambda e, cm=cm, lastc=lastc: e.tensor_tensor(out=lastc, in0=lastc, in1=cm[:, :], op=ALU.add),
              r=[cm_key, ("score", j)], w=[("score", j)])
        for it in range(NIT if not TEST_SKIP else 1):
            S.add("dve", lambda e, it=it: e.tensor_tensor(out=mid, in0=lo, in1=steps[:, it:it + 1], op=ALU.add),
                  r=["lo", "steps"], w=["mid"])
            S.add("dve", lambda e, nk=nk: e.tensor_scalar(out=pj[:, 0:nk], in0=score[:, 0:nk], scalar1=mid, scalar2=None,
                                                           op0=ALU.is_ge, op1=ALU.add, accum_out=cnt),
                  r=sc_keys + ["mid"], w=["pj", "cnt"])
            S.add("dve", lambda e: e.tensor_scalar(out=ge, in0=cnt, scalar1=255.5, scalar2=None, op0=ALU.is_ge),
                  r=["cnt"], w=["ge"])
            S.add("dve", lambda e, it=it: e.scalar_tensor_tensor(out=lo, in0=ge, scalar=steps[:, it:it + 1], in1=lo,
                                                                  op0=ALU.mult, op1=ALU.add), r=["ge", "steps", "lo"], w=["lo"])
        S.add("dve", lambda e, nk=nk: e.tensor_scalar(out=mb[:, 0:nk], in0=score[:, 0:nk], scalar1=lo, scalar2=None,
                                                       op0=ALU.is_ge), r=sc_keys + ["lo"], w=["mb"])
        S.add("dve", lambda e, nk=nk: e.tensor_scalar(out=mb[:, 0:nk], in0=mb[:, 0:nk], scalar1=30000.0, scalar2=-30000.0,
                                                       op0=ALU.mult, op1=ALU.add), r=["mb"], w=["mb"])
        rel_keys = [("rel", c) for c in range(j + 1)]
        atok, atok_key = kst.next()
        for h in range(8):
            kh, kh_key = kh_ring.next()
            vh, vh_key = vh_ring.next()
            for rr in range(4):
                ksrc = k_all[h // 4][rr * 512 + (h % 4) * 128:rr * 512 + (h % 4 + 1) * 128, 0:128 * (j + 1)]
                S.dma("sp", kh[:, 0:nk].rearrange("p (c r i) -> p c r i", c=j + 1, r=4)[:, :, rr, :],
                      ksrc.rearrange("p (c i) -> p c i", c=j + 1), r=[], w=[kh_key])
                for vp in range(2):
                    ncp = min(4, j + 1 - 4 * vp)
                    if ncp <= 0:
                        continue
                    vsrc = v_all[vp][rr * 512:rr * 512 + 128 * ncp, h * 128:(h + 1) * 128]
                    S.dma("sp", vh[:, 16 * vp:16 * vp + 4 * ncp, :].rearrange("p (c r) d -> p c r d", r=4)[:, :, rr, :],
                          vsrc.rearrange("(c p) d -> p c d", p=128), r=[], w=[vh_key])
            S.add("dve", lambda e, h=h, nk=nk: e.scalar_tensor_tensor(
                out=logit[:, 0:nk], in0=rel[:, 0:nk], scalar=slopes[:, h:h + 1], in1=mb[:, 0:nk],
                op0=ALU.mult, op1=ALU.add), r=rel_keys + ["mb", "slopes"], w=["logit"])
            for c in range(j + 1):
                ib = fb[0] % 4
                fb[0] += 1
                S.add("pe", lambda e, c=c, h=h, ib=ib, kh=kh, j=j: e.matmul(
                    out=bank[ib][:, :], lhsT=qT[:, h, j * 128:(j + 1) * 128], rhs=kh[:, c * 512:(c + 1) * 512],
                    start=True, stop=True), r=[("qT", h, 0), ("qT", h, 1), kh_key], w=[bk(ib)])
                lc = logit[:, c * 512:(c + 1) * 512]
                S.add("dve", lambda e, ib=ib, lc=lc: e.scalar_tensor_tensor(
                    out=lc, in0=bank[ib][:, :], scalar=ATT_SCALE, in1=lc, op0=ALU.mult, op1=ALU.add),
                    r=[bk(ib), "logit"], w=["logit"])
            S.add("dve", lambda e, nk=nk: e.tensor_reduce(out=rmax, in_=logit[:, 0:nk], axis=mybir.AxisListType.X,
                                                           op=ALU.max, negate=True), r=["logit"], w=["rmax"])
            S.add("act", lambda e, nk=nk: e.activation(out=pj[:, 0:nk], in_=logit[:, 0:nk], func=AF.Exp, bias=rmax,
                                                        accum_out=rsum), r=["logit", "rmax"], w=["pj", "rsum"])
            S.add("dve", lambda e: e.reciprocal(out=rinv, in_=rsum), r=["rsum"], w=["rinv"])
            for b8 in range((nb + 7) // 8):
                bi = 4 + (tr_rr[0] % 2)
                tr_rr[0] += 1
                n8 = min(8, nb - b8 * 8)
                pt = bank[bi][:, :].bitcast(BF16)
                for k in range(n8):
                    blk = b8 * 8 + k
                    S.add("pe", lambda e, k=k, blk=blk, pt=pt: e.transpose(
                        out=pt[:, k * 128:(k + 1) * 128], in_=pj[:, blk * 128:(blk + 1) * 128], identity=identb[:, :]),
                        r=["pj", "identb"], w=[bk(bi)])
                evac(PT[:, b8 * 8:b8 * 8 + n8, :], pt[:, 0:n8 * 128].rearrange("p (b t) -> p b t", b=n8),
                     r=[bk(bi)], w=[("PT", b8)])
            ib = 6 + (h % 2)
            pt_keys = [("PT", b8) for b8 in range((nb + 7) // 8)]
            for blk in range(nb):
                S.add("pe", lambda e, blk=blk, ib=ib, vh=vh, nb=nb: e.matmul(
                    out=bank[ib][:, 0:128], lhsT=PT[:, blk, :], rhs=vh[:, blk, :], start=(blk == 0), stop=(blk == nb - 1)),
                    r=pt_keys + [vh_key], w=[bk(ib)])
            S.add("act", lambda e, ib=ib, h=h, atok=atok: e.activation(
                out=atok[:, h * 128:(h + 1) * 128], in_=bank[ib][:, 0:128], func=AF.Copy, scale=rinv),
                r=[bk(ib), "rinv"], w=[(atok_key, h)])
        bi = 4 + (tr_rr[0] % 2)
        tr_rr[0] += 1
        pt = bank[bi][:, :].bitcast(BF16)
        for h in range(8):
            S.add("pe", lambda e, h=h, pt=pt, atok=atok: e.transpose(
                out=pt[:, h * 128:(h + 1) * 128], in_=atok[:, h * 128:(h + 1) * 128], identity=identb[:, :]),
                r=[(atok_key, h), "identb"], w=[bk(bi)])
        S.add("act", lambda e, pt=pt, j=j: e.copy(out=qT[:, :, j * 128:(j + 1) * 128],
                                                   in_=pt.rearrange("p (h t) -> p h t", h=8)),
              r=[bk(bi)], w=[("qT", h, j // 4) for h in range(8)])
        mtok, mtok_key = kst.next()
        for h in range(4):
            ib = fb[0] % 4
            fb[0] += 1
            S.add("pe", lambda e, h=h, ib=ib, j=j: e.matmul(
                out=bank[ib][:, 0:256], lhsT=qmT[:, h, j * 128:(j + 1) * 128], rhs=kmT[:, h, :], start=True, stop=True),
                r=[("qmT", h, 0), ("qmT", h, 1), ("kmT", h)], w=[bk(ib)])
            S.add("dve", lambda e, ib=ib: e.tensor_reduce(out=rmax, in_=bank[ib][:, 0:256], axis=mybir.AxisListType.X,
                                                           op=ALU.max), r=[bk(ib)], w=["rmax"])
            S.add("dve", lambda e: e.tensor_scalar(out=rmax, in0=rmax, scalar1=-ATT_SCALE, scalar2=None, op0=ALU.mult),
                  r=["rmax"], w=["rmax"])
            S.add("act", lambda e, ib=ib: e.activation(out=pj[:, 0:256], in_=bank[ib][:, 0:256], func=AF.Exp, bias=rmax,
                                                        scale=ATT_SCALE, accum_out=rsum), r=[bk(ib), "rmax"], w=["pj", "rsum"])
            S.add("dve", lambda e: e.reciprocal(out=rinv, in_=rsum), r=["rsum"], w=["rinv"])
            bi = 4 + (tr_rr[0] % 2)
            tr_rr[0] += 1
            pt = bank[bi][:, :].bitcast(BF16)
            for k in range(2):
                S.add("pe", lambda e, k=k, pt=pt: e.transpose(
                    out=pt[:, k * 128:(k + 1) * 128], in_=pj[:, k * 128:(k + 1) * 128], identity=identb[:, :]),
                    r=["pj", "identb"], w=[bk(bi)])
            evac(PT[:, 0:2, :], pt[:, 0:256].rearrange("p (b t) -> p b t", b=2), r=[bk(bi)], w=[("PT", 0)])
            ib2 = 6 + (h % 2)
            for k in range(2):
                S.add("pe", lambda e, k=k, ib2=ib2, h=h: e.matmul(
                    out=bank[ib2][:, 0:128], lhsT=PT[:, k, :], rhs=vmt[:, k, h * 128:(h + 1) * 128],
                    start=(k == 0), stop=(k == 1)), r=[("PT", 0), ("vmt", 0), ("vmt", 1)], w=[bk(ib2)])
            S.add("act", lambda e, ib2=ib2, h=h, mtok=mtok: e.activation(
                out=mtok[:, h * 128:(h + 1) * 128], in_=bank[ib2][:, 0:128], func=AF.Copy, scale=rinv),
                r=[bk(ib2), "rinv"], w=[(mtok_key, h)])
        bi = 4 + (tr_rr[0] % 2)
        tr_rr[0] += 1
        pt = bank[bi][:, :].bitcast(BF16)
        for h in range(4):
            S.add("pe", lambda e, h=h, pt=pt, mtok=mtok: e.transpose(
                out=pt[:, h * 128:(h + 1) * 128], in_=mtok[:, h * 128:(h + 1) * 128], identity=identb[:, :]),
                r=[(mtok_key, h), "identb"], w=[bk(bi)])
        S.add("act", lambda e, pt=pt, j=j: e.copy(out=qmT[:, :, j * 128:(j + 1) * 128],
                                                   in_=pt[:, 0:512].rearrange("p (h t) -> p h t", h=4)),
              r=[bk(bi)], w=[("qmT", h, j // 4) for h in range(4)])
    S.flush()

    if stage == 3:
        for c in range(16):
            src = qT[:, c, :] if c < 8 else (pT[:, c - 8, :] if c < 12 else qmT[:, c - 12, :])
            st, st_key = wst.next()
            S.add("dve", lambda e, st=st, src=src: e.tensor_copy(out=st[:, 0:1024], in_=src), w=[st_key])
            S.dma("sp", out_d[c * 64:(c + 1) * 64, :].rearrange("r (pl t) -> (r pl) t", pl=2), st[:, 0:1024], r=[st_key])
        S.flush()
        es.close()
        return nc
    for j in range(NSLOT):
        xt, xt_key = accb[:, 0:2048] if False else (wst.tiles[j % 2][:, :], ("wst", j % 2))
        S.dma("sp", xt, h1_scr[j // 4][(j % 4) * 128:(j % 4 + 1) * 128, :], w=[xt_key])
        S.add("act", lambda e, j=j, xt=xt: e.mul(out=acc[:, j, :], in_=xt, mul=ALPHA), r=[xt_key], w=[("acc", j)])
        to_hT(j, xt, xt_key)
    S.flush()
    yT = qiT
    wbf_cur[0] = Ring(wbf.tiles + [regG[:, 12 * T:14 * T], regG[:, 14 * T:16 * T]], "wbf")
    sg = [xbf.tiles[0][:, 0:1024].bitcast(F32), xbf.tiles[0][:, 1024:2048].bitcast(F32),
          xbf.tiles[1][:, 0:1024].bitcast(F32), xbf.tiles[1][:, 1024:2048].bitcast(F32)]
    wba_v = wba_d.rearrange("(c p) n -> p c n", p=128)
    wbp_v = wbp_d.rearrange("(c p) n -> p c n", p=128)
    wbm_v = wbm_d.rearrange("(c p) n -> p c n", p=128)
    wout_v = wout_d.rearrange("(c p) n -> p c n", p=128)
    for G in range(4):
        for n4 in range(4):
            n = G * 4 + n4
            wts = []
            for i in range(3):
                col0 = 5200 + i * D + n * 128
                wts.append(load_w(win_v[:, :, col0:col0 + 128], v3))
            st, st_key = wst.next()
            bf, bf_key = wbf_cur[0].next()
            cols = slice(n * 128, (n + 1) * 128)
            vv = lambda t, a, b, c: t[:, a:b].rearrange("p (c n) -> p c n", c=c)
            S.dma("sp", vv(st, 0, 1024, 8), wba_v[:, :, cols], w=[st_key])
            S.dma("sp", vv(st, 1024, 1536, 4), wbp_v[:, :, cols], w=[st_key])
            S.dma("sp", vv(st, 1536, 2048, 4), wbm_v[:, :, cols], w=[st_key])
            cast(bf[:, :], st[:, :], r=[st_key], w=[bf_key])
            wb = [(vv(bf, 0, 1024, 8), bf_key), (vv(bf, 1024, 1536, 4), bf_key), (vv(bf, 1536, 2048, 4), bf_key)]
            brs = [(qT, 8), (pT, 4), (qmT, 4)]
            for half in range(2):
                ts = slice(half * 512, (half + 1) * 512)
                for i in range(3):
                    wt, wk = wts[i]
                    for c in range(NDC):
                        S.add("pe", lambda e, wt=wt, c=c, i=i, ts=ts: e.matmul(
                            out=bank[i][:, :], lhsT=wt[:, c, :], rhs=hT[:, c, ts], start=(c == 0), stop=(c == NDC - 1)),
                            r=[wk] + hT_all, w=[bk(i)])
                    S.add("act", lambda e, i=i, n=n: e.activation(
                        out=sg[i], in_=bank[i][:, :], func=AF.Sigmoid, bias=bgate[:, i * 16 + n:i * 16 + n + 1]),
                        r=[bk(i), "bgate"], w=[("sg", i)])
                for i in range(3):
                    wt, wk = wb[i]
                    src, nkc = brs[i]
                    for c in range(nkc):
                        S.add("pe", lambda e, wt=wt, c=c, i=i, ts=ts, src=src, nkc=nkc: e.matmul(
                            out=bank[3 + i][:, :], lhsT=wt[:, c, :], rhs=src[:, c, ts], start=(c == 0), stop=(c == nkc - 1)),
                            r=[wk], w=[bk(3 + i)])
                    S.add("dve", lambda e, i=i: e.tensor_tensor(out=sg[i], in0=sg[i], in1=bank[3 + i][:, :], op=ALU.mult),
                          r=[("sg", i), bk(3 + i)], w=[("sg", i)])
                S.add("dve", lambda e: e.tensor_tensor(out=sg[0], in0=sg[0], in1=sg[1], op=ALU.add),
                      r=[("sg", 0), ("sg", 1)], w=[("sg", 0)])
                S.add("dve", lambda e, n4=n4, ts=ts: e.tensor_tensor(out=yT[:, n4, ts], in0=sg[0], in1=sg[2], op=ALU.add),
                      r=[("sg", 0), ("sg", 2)], w=[("yT", n4, half)])
        y_all = [("yT", n4, h) for n4 in range(4) for h in range(2)]
        for dq in range(4):
            wt, wk = load_w(wout_v[:, G * 4:G * 4 + 4, dq * 512:(dq + 1) * 512], v4)
            for k4 in range(4):
                for j in range(NSLOT):
                    S.add("pe", lambda e, k4=k4, j=j, wt=wt: e.matmul(
                        out=bank[j][:, :], lhsT=yT[:, k4, j * 128:(j + 1) * 128], rhs=wt[:, k4, :],
                        start=(k4 == 0), stop=(k4 == 3)), r=[wk] + y_all, w=[bk(j)])
            for j in range(NSLOT):
                dst = acc[:, j, dq * 512:(dq + 1) * 512]
                S.add("dve", lambda e, dst=dst, j=j: e.tensor_tensor(out=dst, in0=dst, in1=bank[j][:, :], op=ALU.add),
                      r=[bk(j), ("acc", j)], w=[("acc", j)])
    S.flush()
    wbf_cur[0] = wbf
    if stage == 4:
        layernorm(ln2g_d, ln2b_d, store_d=out_d, make_hT=True, post_scale=ALPHA)
        es.close()
        return nc
    layernorm(ln2g_d, ln2b_d, store_d=None, make_hT=True, post_scale=ALPHA)

    ffn(w2u_d, w2d_d)
    layernorm(ln3g_d, ln3b_d, store_d=out_d, make_hT=False)

    es.close()
    return nc


def _prep_inputs(inputs):
    f = lambda k: np.asarray(inputs[k], dtype=np.float32)
    x = f("x")
    mem = f("mem")
    shared = {"ident": np.eye(128, dtype=np.float32)}
    for k in ("w_ffn1_up", "w_ffn1_down", "w_in", "w_mem_kv", "w_br_att", "w_br_pool", "w_br_mem", "w_out",
              "w_ffn2_up", "w_ffn2_down"):
        shared[k] = np.ascontiguousarray(f(k)[0])
    for k in ("ln1_g", "ln1_b", "ln2_g", "ln2_b", "ln3_g", "ln3_b"):
        shared[k] = np.ascontiguousarray(f(k).reshape(1, D))
    shared["b_gate"] = np.ascontiguousarray(f("b_gate").reshape(48, 128).T)
    shared["w_pool"] = np.ascontiguousarray(f("w_pool")[0].transpose(1, 0, 2))
    shared["pool_scale"] = np.ascontiguousarray(f("pool_scale").reshape(4, 128).T)
    shared["iota512"] = np.arange(512, dtype=np.float32).reshape(1, 512)
    shared["c512"] = (512.0 * np.arange(8, dtype=np.float32)).reshape(1, 8)
    shared["pow2"] = (2.0 ** -(np.arange(32, dtype=np.float64) + 1)).astype(np.float32).reshape(1, 32)
    shared["slopes"] = (2.0 ** -(np.arange(8, dtype=np.float64) + 1)).astype(np.float32).reshape(1, 8)
    wins = np.array([2, 4, 8, 16], dtype=np.float32)
    maps = []
    for c in range(NCORE):
        b, r = divmod(c, 4)
        m = dict(shared)
        m["x"] = np.ascontiguousarray(x[b].reshape(32, 128, D)[r::4].reshape(T, D))
        m["mem"] = np.ascontiguousarray(mem[b])
        p = np.arange(128, dtype=np.float32)[:, None]
        j = np.arange(NSLOT, dtype=np.float32)[None, :]
        m["qpos"] = np.ascontiguousarray((4 * j + r) * 128 + p).astype(np.float32)
        t = np.arange(128, dtype=np.float32)[None, :]
        if r == 0:
            invc = 1.0 / np.minimum(t + 1.0, wins[:, None])
        else:
            invc = np.broadcast_to(1.0 / wins[:, None], (4, 128))
        m["invc0"] = np.ascontiguousarray(invc, dtype=np.float32).reshape(1, 512)
        sel = np.zeros((1, 4), dtype=np.float32)
        sel[0, (r - 1) % 4] = 1.0
        m["psel"] = sel
        maps.append(m)
    return maps


def _assemble(results):
    out = np.zeros((2, SEQ, D), dtype=np.float32)
    for c in range(NCORE):
        b, r = divmod(c, 4)
        o = np.asarray(results[c]["out"]).reshape(NSLOT, 128, D)
        out[b].reshape(32, 128, D)[r::4] = o
    return out


def kernel(**inputs):
    nc = build_program(stage=DEBUG_STAGE)
    maps = _prep_inputs(inputs)
    res = run_bass_kernel_spmd(nc, maps, core_ids=list(range(NCORE)))
    return _assemble(res.results)
```

```python
import numpy as np
import concourse.bass as bass
import concourse.mybir as mybir
from concourse.bass_utils import run_bass_kernel_spmd

F32 = mybir.dt.float32
BF16 = mybir.dt.bfloat16
AF = mybir.ActivationFunctionType
ALU = mybir.AluOpType

D = 2048
SEQ = 4096
NCORE = 8
T = 1024
NSLOT = 8
DFF = 5632
NFC = DFF // 128
NDC = D // 128
ALPHA = 2.0 ** 0.25
LN_EPS = 1e-5
DIN = 11344

DEBUG_STAGE = None
TEST_SKIP = False


class Sched:
    COMPUTE = ("pe", "act", "dve", "pool")
    ALL = ("pe", "act", "dve", "pool", "sp")

    def __init__(self, nc, sems, dsems):
        self.nc = nc
        self.sem = sems
        self.dsem = dsems
        self.cnt = {e: 0 for e in self.COMPUTE}
        self.ops = {e: [] for e in self.ALL}
        self.waited = {e: {} for e in self.ALL}
        self.lastw = {}
        self.readers = {}
        self.dma_k = 0
        self.dma_val = [0] * len(dsems)
        self.dma_owner = [None] * len(dsems)
        self.nops = 0

    def _deps(self, eng, r, w):
        raw = {}
        oth = {}

        def put(d, tok):
            k, v = tok
            if d.get(k, 0) < v:
                d[k] = v

        for res in r:
            if res in self.lastw:
                put(raw, self.lastw[res])
        for res in w:
            if res in self.lastw:
                put(oth, self.lastw[res])
            for k, v in self.readers.get(res, {}).items():
                put(oth, (k, v))
        waits = {}
        for k, v in raw.items():
            if k == eng and eng == "pe":
                continue
            put(waits, (k, v))
        for k, v in oth.items():
            if k == eng:
                continue
            put(waits, (k, v))
        out = []
        wd = self.waited[eng]
        for k, v in waits.items():
            if wd.get(k, 0) >= v:
                continue
            wd[k] = v
            out.append((k, v))
        return out

    def _commit(self, tok, r, w):
        for res in w:
            self.lastw[res] = tok
            self.readers[res] = {}
        for res in r:
            d = self.readers.setdefault(res, {})
            if d.get(tok[0], 0) < tok[1]:
                d[tok[0]] = tok[1]

    def add(self, eng, fn, r=(), w=()):
        waits = self._deps(eng, r, w)
        self.cnt[eng] += 1
        tok = (eng, self.cnt[eng])
        self.ops[eng].append((waits, fn, tok, "c"))
        self._commit(tok, r, w)
        self.nops += 1
        return tok

    def dma(self, q, out, in_, r=(), w=(), **kw):
        waits = self._deps(q, r, w)
        i = self.dma_k % len(self.dsem)
        self.dma_k += 1
        prev = self.dma_val[i]
        key = ("d", i)
        if prev > 0 and self.waited[q].get(key, 0) < prev:
            self.waited[q][key] = prev
            waits.append((key, prev))
        self.dma_val[i] = prev + 16
        self.dma_owner[i] = q
        tok = (key, prev + 16)
        self.ops[q].append((waits, lambda e: e.dma_start(out=out, in_=in_, **kw), tok, "d"))
        self._commit(tok, r, w)
        self.nops += 1
        return tok

    def coll(self, i, fn, r=(), w=()):
        waits = self._deps("pool", r, w)
        tok = (("c", i), 1)
        self.cc_pending = getattr(self, "cc_pending", []) + [tok]
        self.ops["pool"].append((waits, fn, tok, "cc"))
        self._commit(tok, r, w)
        return tok

    def _semobj(self, key):
        if isinstance(key, tuple):
            if key[0] == "c":
                return self.csem[key[1]]
            return self.dsem[key[1]]
        return self.sem[key]

    def flush(self):
        nc = self.nc
        with nc.Block() as block:
            decos = {"sp": block.sync, "act": block.scalar, "dve": block.vector,
                     "pool": block.gpsimd, "pe": block.tensor}
            for eng in self.ALL:
                ops = self.ops[eng]
                tail = [(("d", i), self.dma_val[i]) for i in range(len(self.dsem))
                        if self.dma_owner[i] == eng and self.dma_val[i] > 0]
                if eng == "pool":
                    tail = tail + list(getattr(self, "cc_pending", []))

                def body(e, ops=ops, eng=eng, tail=tail):
                    for waits, fn, tok, kind in ops:
                        for k, v in waits[1:]:
                            e.wait_ge(self._semobj(k), v)
                        ins = fn(e)
                        if waits:
                            ins.wait_op(self._semobj(waits[0][0]), waits[0][1], "sem-ge")
                        if kind == "c":
                            ins.then_inc(self.sem[eng], 1)
                        elif kind == "cc":
                            ins.then_inc(self.csem[tok[0][1]])
                        else:
                            ins.then_inc(self.dsem[tok[0][1]], 16)
                    for k, v in tail:
                        e.wait_ge(self._semobj(k), v)

                decos[eng](body)
        for eng in self.ALL:
            self.ops[eng] = []
            for i in range(len(self.dsem)):
                if self.dma_owner[i] is not None:
                    self.waited[eng][("d", i)] = self.dma_val[i]
            for c in self.COMPUTE:
                self.waited[eng][c] = self.cnt[c]
        self.lastw = {}
        self.readers = {}
        self.cc_pending = []


class Ring:
    def __init__(self, tiles, name):
        self.tiles = tiles
        self.name = name
        self.i = 0

    def next(self):
        k = self.i % len(self.tiles)
        self.i += 1
        return self.tiles[k], (self.name, k)


def build_program(stage=None):
    from contextlib import ExitStack
    nc = bass.Bass("TRN2", target_bir_lowering=False)

    def din(name, shape, dt=F32):
        return nc.dram_tensor(name, list(shape), dt, kind="ExternalInput").ap()

    x_d = din("x", [T, D])
    w1u_d = din("w_ffn1_up", [D, 2 * DFF])
    w1d_d = din("w_ffn1_down", [DFF, D])
    ln1g_d = din("ln1_g", [1, D])
    ln1b_d = din("ln1_b", [1, D])
    ident_d = din("ident", [128, 128])
    win_d = din("w_in", [D, DIN])
    bgate_d = din("b_gate", [128, 48])
    wmkv_d = din("w_mem_kv", [D, 1024])
    mem_d = din("mem", [256, D])
    wpool_d = din("w_pool", [128, 4, 128])
    pscale_d = din("pool_scale", [128, 4])
    wba_d = din("w_br_att", [1024, D])
    wbp_d = din("w_br_pool", [512, D])
    wbm_d = din("w_br_mem", [512, D])
    wout_d = din("w_out", [D, D])
    ln2g_d = din("ln2_g", [1, D])
    ln2b_d = din("ln2_b", [1, D])
    w2u_d = din("w_ffn2_up", [D, 2 * DFF])
    w2d_d = din("w_ffn2_down", [DFF, D])
    ln3g_d = din("ln3_g", [1, D])
    ln3b_d = din("ln3_b", [1, D])
    qpos_d = din("qpos", [128, NSLOT])
    iota_d = din("iota512", [1, 512])
    invc0_d = din("invc0", [1, 4 * 128])
    psel_d = din("psel", [1, 4])
    pow2_d = din("pow2", [1, 32])
    slopes_d = din("slopes", [1, 8])
    c512_d = din("c512", [1, 8])
    aq_d = din("aq", [4, T], BF16)
    ak_d = din("ak", [4, SEQ], BF16)
    h1_scr = [nc.dram_tensor("h1_scr%d" % i, [512, D], F32).ap() for i in range(2)]
    k_loc = [nc.dram_tensor("k_loc%d" % i, [512, 1024], BF16).ap() for i in range(2)]
    k_all = [nc.dram_tensor("k_all%d" % i, [4 * 512, 1024], BF16).ap() for i in range(2)]
    v_loc = [nc.dram_tensor("v_loc%d" % i, [512, 1024], BF16).ap() for i in range(2)]
    v_all = [nc.dram_tensor("v_all%d" % i, [4 * 512, 1024], BF16).ap() for i in range(2)]
    ki_loc = nc.dram_tensor("ki_loc", [64, 1024], BF16).ap()
    ki_all = nc.dram_tensor("ki_all", [4 * 64, 1024], BF16).ap()
    ut_loc = nc.dram_tensor("ut_loc", [NSLOT * 128, 64], F32).ap()
    ut_all = nc.dram_tensor("ut_all", [4 * NSLOT * 128, 64], F32).ap()
    out_d = nc.dram_tensor("out", [T, D], F32, kind="ExternalOutput").ap()

    es = ExitStack()

    def sb(name, shape, dt):
        return es.enter_context(nc.sbuf_tensor("sb_" + name, list(shape), dt))

    sems = {e: es.enter_context(nc.semaphore("s_" + e)) for e in Sched.COMPUTE}
    dsems = [es.enter_context(nc.semaphore("d%d" % i)) for i in range(24)]
    csems = [es.enter_context(nc.semaphore("c%d" % i)) for i in range(6)]
    S = Sched(nc, sems, dsems)
    S.csem = csems

    bank = [es.enter_context(nc.psum_tensor("bank%d" % i, [128, 512], F32)) for i in range(8)]

    def bk(i):
        return ("bank", i)

    hT = sb("hT", [128, NDC, T], BF16)
    acc = sb("acc", [128, NSLOT, D], F32)
    identb = sb("identb", [128, 128], BF16)
    identf = sb("identf", [128, 128], F32)
    wst = Ring([sb("wst%d" % i, [128, 2048], F32) for i in range(3)], "wst")
    wbf = Ring([sb("wbf%d" % i, [128, 2048], BF16) for i in range(3)], "wbf")
    regG = sb("regG", [128, 24 * T], BF16)
    xbf = Ring([sb("xbf%d" % i, [128, D], BF16) for i in range(2)], "xbf")
    sa_ring = Ring([sb("sa%d" % i, [128, 512], F32) for i in range(2)], "sa")
    stats = sb("stats", [128, 4, 6], F32)
    mv = sb("mv", [128, 2], F32)
    rstd = sb("rstd", [128, 1], F32)
    epsb = sb("epsb", [128, 1], F32)

    cast_rr = [0]

    def cast(out, in_, r, w):
        engs = ("act", "dve", "pool")
        eng = engs[cast_rr[0] % 3]
        cast_rr[0] += 1
        if eng == "act":
            S.add("act", lambda e: e.copy(out=out, in_=in_), r=r, w=w)
        else:
            S.add(eng, lambda e: e.tensor_copy(out=out, in_=in_), r=r, w=w)

    wbf_cur = [wbf]

    def load_w(src_ap, view):
        st, st_key = wst.next()
        bf, bf_key = wbf_cur[0].next()
        S.dma("sp", view(st), src_ap, w=[st_key])
        cast(view(bf), view(st), r=[st_key], w=[bf_key])
        return view(bf), bf_key

    S.dma("sp", identf[:, :], ident_d, w=["identf"])
    S.add("dve", lambda e: e.tensor_copy(out=identb[:, :], in_=identf[:, :]), r=["identf"], w=["identb"])
    S.add("dve", lambda e: e.memset(epsb[:, :], LN_EPS), w=["epsb"])

    tr_rr = [0]

    def to_hT(j, src_f32, src_key):
        xb, xb_key = xbf.next()
        S.add("dve", lambda e: e.tensor_copy(out=xb[:, :], in_=src_f32), r=[src_key], w=[xb_key])
        for half in range(2):
            bi = 6 + (tr_rr[0] % 2)
            tr_rr[0] += 1
            pt = bank[bi][:, :].bitcast(BF16)
            for c8 in range(8):
                c = half * 8 + c8
                S.add("pe", lambda e, c=c, c8=c8, pt=pt: e.transpose(
                    out=pt[:, c8 * 128:(c8 + 1) * 128], in_=xb[:, c * 128:(c + 1) * 128], identity=identb[:, :]),
                    r=[xb_key, "identb"], w=[bk(bi)])
            dst = hT[:, half * 8:(half + 1) * 8, j * 128:(j + 1) * 128]
            src = pt.rearrange("p (c n) -> p c n", c=8)
            S.add("act", lambda e, dst=dst, src=src: e.copy(out=dst, in_=src),
                  r=[bk(bi)], w=[("hT", j)])

    xin = Ring([regG[:, 0:2 * D].bitcast(F32), regG[:, 2 * D:4 * D].bitcast(F32)], "xin")
    for j in range(NSLOT):
        xt, xt_key = xin.next()
        S.dma("sp", xt, x_d[j * 128:(j + 1) * 128, :], w=[xt_key])
        S.add("act", lambda e, j=j, xt=xt: e.mul(out=acc[:, j, :], in_=xt, mul=ALPHA),
              r=[xt_key], w=[("acc", j)])
        to_hT(j, xt, xt_key)
    if stage == 0:
        for j in range(NSLOT):
            S.dma("sp", out_d[j * 128:(j + 1) * 128, :], acc[:, j, :], r=[("acc", j)])
        S.flush()
        es.close()
        return nc
    S.flush()

    hT_all = [("hT", j) for j in range(NSLOT)]

    def ffn(wu_d, wd_d):
        gT = regG
        wu_v = wu_d.rearrange("(c p) n -> p c n", p=128)
        wd_v = wd_d.rearrange("(c p) n -> p c n", p=128)
        groups = [(0, 24), (24, 20)]
        bno = [0]
        v3 = lambda t: t[:, :].rearrange("p (c n) -> p c n", c=16)
        v4 = lambda t: t[:, :].rearrange("p (c n) -> p c n", c=4)
        for (f0, nf) in groups:
            for fl in range(nf):
                f = f0 + fl
                wa, wa_key = load_w(wu_v[:, :, f * 128:(f + 1) * 128], v3)
                wu, wu_key = load_w(wu_v[:, :, DFF + f * 128:DFF + (f + 1) * 128], v3)
                for half in range(2):
                    ia = bno[0] % 4
                    iu = (bno[0] + 1) % 4
                    bno[0] += 2
                    for (wt, wk, ib) in ((wa, wa_key, ia), (wu, wu_key, iu)):
                        for c in range(NDC):
                            S.add("pe", lambda e, wt=wt, ib=ib, c=c, half=half: e.matmul(
                                out=bank[ib][:, :], lhsT=wt[:, c, :], rhs=hT[:, c, half * 512:(half + 1) * 512],
                                start=(c == 0), stop=(c == NDC - 1)),
                                r=[wk] + hT_all, w=[bk(ib)])
                    sa, sa_key = sa_ring.next()
                    S.add("act", lambda e, sa=sa, ia=ia: e.activation(out=sa[:, :], in_=bank[ia][:, :], func=AF.Silu),
                          r=[bk(ia)], w=[sa_key])
                    gdst = gT[:, fl * T + half * 512: fl * T + (half + 1) * 512]
                    S.add("dve", lambda e, gdst=gdst, sa=sa, iu=iu: e.tensor_tensor(
                        out=gdst, in0=sa[:, :], in1=bank[iu][:, :], op=ALU.mult),
                        r=[sa_key, bk(iu)], w=[("gT", fl, half)])
            g_all = [("gT", fl, h) for fl in range(nf) for h in range(2)]
            for dq in range(4):
                for u4 in range(nf // 4):
                    c0 = f0 + u4 * 4
                    wd, wd_key = load_w(wd_v[:, c0:c0 + 4, dq * 512:(dq + 1) * 512], v4)
                    for k4 in range(4):
                        fl = u4 * 4 + k4
                        for j in range(NSLOT):
                            S.add("pe", lambda e, fl=fl, j=j, wd=wd, k4=k4, nf=nf: e.matmul(
                                out=bank[j][:, :], lhsT=gT[:, fl * T + j * 128: fl * T + (j + 1) * 128],
                                rhs=wd[:, k4, :], start=(fl == 0), stop=(fl == nf - 1)),
                                r=[wd_key] + g_all, w=[bk(j)])
                for j in range(NSLOT):
                    dst = acc[:, j, dq * 512:(dq + 1) * 512]
                    S.add("dve", lambda e, dst=dst, j=j: e.scalar_tensor_tensor(
                        out=dst, in0=bank[j][:, :], scalar=0.5, in1=dst, op0=ALU.mult, op1=ALU.add),
                        r=[bk(j), ("acc", j)], w=[("acc", j)])
        S.flush()

    def layernorm(g_d, b_d, store_d=None, make_hT=True, post_scale=None):
        gt = regG[:, 0:2 * D].bitcast(F32)
        bt = regG[:, 2 * D:4 * D].bitcast(F32)
        S.dma("sp", gt, g_d.partition_broadcast(128) if False else g_d.broadcast_to([128, D]), w=["lng"])
        S.dma("sp", bt, b_d.broadcast_to([128, D]), w=["lnb"])
        for j in range(NSLOT):
            a_j = acc[:, j, :]
            for q in range(4):
                S.add("dve", lambda e, j=j, q=q: e.bn_stats(out=stats[:, q, :], in_=acc[:, j, q * 512:(q + 1) * 512]),
                      r=[("acc", j)], w=[("stats", q)])
            S.add("dve", lambda e: e.bn_aggr(out=mv[:, :], in_=stats[:, :, :]),
                  r=[("stats", q) for q in range(4)], w=["mv"])
            S.add("act", lambda e: e.activation(out=rstd[:, :], in_=mv[:, 1:2], func=AF.Sqrt, bias=epsb[:, :]),
                  r=["mv", "epsb"], w=["rstd"])
            S.add("dve", lambda e: e.reciprocal(out=rstd[:, :], in_=rstd[:, :]), r=["rstd"], w=["rstd"])
            S.add("dve", lambda e, a_j=a_j: e.tensor_scalar(
                out=a_j, in0=a_j, scalar1=mv[:, 0:1], scalar2=rstd[:, :], op0=ALU.subtract, op1=ALU.mult),
                r=[("acc", j), "mv", "rstd"], w=[("acc", j)])
            S.add("pool", lambda e, a_j=a_j: e.tensor_tensor(out=a_j, in0=a_j, in1=gt, op=ALU.mult),
                  r=[("acc", j), "lng"], w=[("acc", j)])
            S.add("dve", lambda e, a_j=a_j: e.tensor_tensor(out=a_j, in0=a_j, in1=bt, op=ALU.add),
                  r=[("acc", j), "lnb"], w=[("acc", j)])
            if store_d is not None:
                sd = store_d[j // 4][(j % 4) * 128:(j % 4 + 1) * 128, :] if isinstance(store_d, list) else store_d[j * 128:(j + 1) * 128, :]
                S.dma("sp", sd, a_j, r=[("acc", j)])
            if make_hT:
                to_hT(j, a_j, ("acc", j))
            if post_scale is not None:
                S.add("act", lambda e, a_j=a_j: e.mul(out=a_j, in_=a_j, mul=post_scale),
                      r=[("acc", j)], w=[("acc", j)])
        S.flush()

    ffn(w1u_d, w1d_d)
    if stage == 1:
        layernorm(ln1g_d, ln1b_d, store_d=out_d, make_hT=True)
        es.close()
        return nc
    layernorm(ln1g_d, ln1b_d, store_d=h1_scr, make_hT=True)

    accb = acc[:, :, :].rearrange("p a b -> p (a b)")
    score = accb[:, 0:4096]
    logit = accb[:, 4096:8192]
    rel = accb[:, 8192:12288]
    mb = accb[:, 12288:14336].bitcast(BF16)
    pj = accb[:, 14336:16384].bitcast(BF16)
    uT = accb[:, 0:4096].rearrange("p (g t) -> p g t", g=4)
    vtok = accb[:, 4096:8192].bitcast(BF16).rearrange("p (j n) -> p j n", j=NSLOT)
    qT = regG[:, 0:8 * T].rearrange("p (h t) -> p h t", h=8)
    qiT = regG[:, 8 * T:16 * T].rearrange("p (h t) -> p h t", h=8)
    qmT = regG[:, 16 * T:20 * T].rearrange("p (h t) -> p h t", h=4)
    pT = regG[:, 20 * T:24 * T].rearrange("p (h t) -> p h t", h=4)
    hTf = hT[:, :, :].rearrange("p c t -> p (c t)")
    kiT = hTf[:, 0:4096]
    PT = hTf[:, 4096:8192].rearrange("p (b t) -> p b t", b=32)
    kh_ring = Ring([hTf[:, 8192:12288], wst.tiles[0][:, :].bitcast(BF16)], "kh")
    vh_ring = Ring([hTf[:, 12288:16384].rearrange("p (b d) -> p b d", b=32),
                    wst.tiles[1][:, :].bitcast(BF16).rearrange("p (b d) -> p b d", b=32)], "vh")
    memT = accb[:, 8192:10240].bitcast(BF16).rearrange("p (c m) -> p c m", c=16)
    wi_t = sb("wi_t", [128, NSLOT, 16], F32)
    absw = sb("absw", [128, NSLOT, 16], F32)
    sgnw = sb("sgnw", [128, NSLOT, 16], F32)
    qpos = sb("qpos", [128, NSLOT], F32)
    qoff = sb("qoff", [128, 8], F32)
    iota = sb("iota", [128, 512], F32)
    pow2 = sb("pow2", [128, 32], F32)
    steps = sb("steps", [128, 32], F32)
    slopes = sb("slopes", [128, 8], F32)
    psel = sb("psel", [128, 4], F32)
    invc0 = sb("invc0", [128, 4, 128], F32)
    pscale = sb("pscale", [128, 4], F32)
    bgate = sb("bgate", [128, 48], F32)
    wpool_f = accb[:, 15488:16000].rearrange("p (g d) -> p g d", g=4)
    wpool_b = sb("wpool_b", [128, 4, 128], BF16)
    c512 = sb("c512", [128, 8], F32)
    negq = sb("negq", [128, 8], F32)
    sm = sb("sm", [128, 16], F32)
    lo, mid, cnt, ge, w0, rmax, rsum, rinv, hi = (sm[:, i:i + 1] for i in range(9))
    halo = accb[:, 14336:14912].rearrange("p (g t) -> p g t", g=4)
    halo2 = accb[:, 14912:15488].rearrange("p (g t) -> p g t", g=4)
    tails = accb[:, 12288:14336].rearrange("p (r j n) -> p r j n", r=4, j=NSLOT)
    kmT = sb("kmT", [128, 4, 256], BF16)
    vmt = sb("vmt", [128, 2, 512], BF16)

    for (dst, src, key) in ((qpos[:, :], qpos_d, "qpos"), (bgate[:, :], bgate_d, "bgate"),
                            (pscale[:, :], pscale_d, "pscale"), (wpool_f[:, :, :], wpool_d, "wpool_f")):
        S.dma("sp", dst, src, w=[key])
    for (dst, src, key, n) in ((iota[:, :], iota_d, "iota", 512), (pow2[:, :], pow2_d, "pow2", 32),
                               (slopes[:, :], slopes_d, "slopes", 8), (psel[:, :], psel_d, "psel", 4), (c512[:, :], c512_d, "c512", 8),
                               (invc0[:, :, :].rearrange("p g t -> p (g t)"), invc0_d, "invc0", 512)):
        S.dma("sp", dst, src.broadcast_to([128, n]), w=[key])
    S.add("dve", lambda e: e.tensor_copy(out=wpool_b[:, :, :], in_=wpool_f[:, :, :]), r=["wpool_f"], w=["wpool_b"])

    v3 = lambda t: t[:, :].rearrange("p (c n) -> p c n", c=16)
    v4 = lambda t: t[:, :].rearrange("p (c n) -> p c n", c=4)
    win_v = win_d.rearrange("(c p) n -> p c n", p=128)
    ev_rr = [0]

    def evac(out, in_, r, w, scale=None):
        eng = ("act", "dve")[ev_rr[0] % 2]
        ev_rr[0] += 1
        if eng == "act":
            S.add("act", lambda e: e.copy(out=out, in_=in_), r=r, w=w)
        else:
            S.add("dve", lambda e: e.tensor_copy(out=out, in_=in_), r=r, w=w)

    fb = [0]

    def proj_feat(w_v, col0, ncols, act_T, act_keys, nk, ntok, consume):
        view = lambda t: t[:, 0:nk * ncols].rearrange("p (c n) -> p c n", c=nk)
        wt, wk = load_w(w_v[:, 0:nk, col0:col0 + ncols], view)
        for half in range((ntok + 511) // 512):
            n = min(512, ntok - half * 512)
            ib = fb[0] % 4
            fb[0] += 1
            for c in range(nk):
                S.add("pe", lambda e, c=c, ib=ib, half=half, n=n: e.matmul(
                    out=bank[ib][0:ncols, 0:n], lhsT=wt[:, c, :], rhs=act_T[:, c, half * 512:half * 512 + n],
                    start=(c == 0), stop=(c == nk - 1)), r=[wk] + act_keys, w=[bk(ib)])
            consume(half, ib, n)

    kst = Ring([xbf.tiles[0], xbf.tiles[1]], "xbf")

    def to_sbuf(dst3, ci, key, scale=None):
        def f(half, ib, n):
            if scale is None:
                evac(dst3[:, ci, half * 512:half * 512 + n], bank[ib][:, 0:n], r=[bk(ib)], w=[(key, ci, half)])
            else:
                S.add("act", lambda e: e.mul(out=dst3[:, ci, half * 512:half * 512 + n], in_=bank[ib][:, 0:n], mul=scale),
                      r=[bk(ib)], w=[(key, ci, half)])
        return f

    for ci in range(4):
        proj_feat(win_v, 4176 + ci * 128, 128, hT, hT_all, NDC, T, to_sbuf(uT, ci, "uT"))
    for ci in range(9):
        col0, ncols = (1024 + ci * 128, 128) if ci < 8 else (4096, 64)
        st_t, st_k = kst.next()

        def to_stage(half, ib, n, st_t=st_t, st_k=st_k, ncols=ncols):
            evac(st_t[0:ncols, half * 512:half * 512 + n], bank[ib][0:ncols, 0:n], r=[bk(ib)], w=[(st_k, half)])
        proj_feat(win_v, col0, ncols, hT, hT_all, NDC, T, to_stage)
        kdst = k_loc[ci // 4][(ci % 4) * 128:(ci % 4 + 1) * 128, :] if ci < 8 else ki_loc[:, :]
        S.dma("sp", kdst, st_t[0:ncols, 0:T], r=[(st_k, 0), (st_k, 1)], w=["kv_loc"])
    for hv in range(2):
        for u4 in range(4):
            wt, wk = load_w(win_v[:, u4 * 4:u4 * 4 + 4, 2048 + hv * 512:2048 + (hv + 1) * 512], v4)
            for k4 in range(4):
                c = u4 * 4 + k4
                for j in range(NSLOT):
                    S.add("pe", lambda e, c=c, j=j, wt=wt, k4=k4: e.matmul(
                        out=bank[j][:, :], lhsT=hT[:, c, j * 128:(j + 1) * 128], rhs=wt[:, k4, :],
                        start=(c == 0), stop=(c == NDC - 1)), r=[wk] + hT_all, w=[bk(j)])
        for j in range(NSLOT):
            evac(vtok[:, j, hv * 512:(hv + 1) * 512], bank[j][:, :], r=[bk(j)], w=[("vtok", j, hv)])
    for j in range(NSLOT):
        S.dma("sp", v_loc[j // 4][(j % 4) * 128:(j % 4 + 1) * 128, :], vtok[:, j, :],
              r=[("vtok", j, 0), ("vtok", j, 1)], w=["kv_loc"])
    for j in range(NSLOT):
        S.dma("sp", ut_loc[j * 128:(j + 1) * 128, :].rearrange("p (g t) -> p g t", g=4),
              uT[:, :, j * 128 + 112:(j + 1) * 128], r=[("uT", g, h) for g in range(4) for h in range(2)], w=["ut_loc"])
    groups = [[0, 1, 2, 3], [4, 5, 6, 7]]
    for ci_, (src_, dst_) in enumerate(((k_loc[0], k_all[0]), (k_loc[1], k_all[1]), (v_loc[0], v_all[0]),
                                        (v_loc[1], v_all[1]), (ki_loc, ki_all))):
        S.coll(ci_, lambda e, src_=src_, dst_=dst_: e.collective_compute(
            "AllGather", ALU.bypass, replica_groups=groups, ins=[src_.opt()], outs=[dst_.opt()]),
            r=["kv_loc"], w=["kv_all"])
    S.coll(5, lambda e: e.collective_compute("AllGather", ALU.bypass, replica_groups=groups,
                                             ins=[ut_loc.opt()], outs=[ut_all.opt()]), r=["ut_loc"], w=["ut_all"])
    for ci in range(8):
        proj_feat(win_v, ci * 128, 128, hT, hT_all, NDC, T, to_sbuf(qT, ci, "qT", scale=128.0 ** -0.5))
    for ci in range(8):
        proj_feat(win_v, 3072 + ci * 128, 128, hT, hT_all, NDC, T, to_sbuf(qiT, ci, "qiT"))
    for ci in range(4):
        proj_feat(win_v, 4688 + ci * 128, 128, hT, hT_all, NDC, T, to_sbuf(qmT, ci, "qmT"))
    vw = lambda t: t[:, 0:256].rearrange("p (c n) -> p c n", c=16)
    wt, wk = load_w(win_v[:, :, 4160:4176], vw)
    for j in range(NSLOT):
        for c in range(NDC):
            S.add("pe", lambda e, c=c, j=j, wt=wt: e.matmul(
                out=bank[j][:, 0:16], lhsT=hT[:, c, j * 128:(j + 1) * 128], rhs=wt[:, c, :],
                start=(c == 0), stop=(c == NDC - 1)), r=[wk] + hT_all, w=[bk(j)])
        S.add("dve", lambda e, j=j: e.tensor_copy(out=wi_t[:, j, :], in_=bank[j][:, 0:16]), r=[bk(j)], w=["wi_t"])
    S.add("act", lambda e: e.activation(out=sgnw[:, :, :], in_=wi_t[:, :, :], func=AF.Sign), r=["wi_t"], w=["sgnw"])
    S.add("dve", lambda e: e.scalar_tensor_tensor(out=absw[:, :, :], in0=wi_t[:, :, :], scalar=0.125 * 0.25, in1=sgnw[:, :, :],
                                                   op0=ALU.mult, op1=ALU.mult), r=["wi_t", "sgnw"], w=["absw"])
    mT_keys = []
    for mbk in range(2):
        xt, xt_key = accb[:, 10240:12288], "memx"
        S.dma("sp", xt, mem_d[mbk * 128:(mbk + 1) * 128, :], w=[xt_key])
        xb, xb_key = kst.next()
        S.add("dve", lambda e, xb=xb, xt=xt: e.tensor_copy(out=xb[:, :], in_=xt), r=[xt_key], w=[xb_key])
        for half in range(2):
            bi = 6 + half
            pt = bank[bi][:, :].bitcast(BF16)
            for c8 in range(8):
                c = half * 8 + c8
                S.add("pe", lambda e, c=c, c8=c8, pt=pt, xb=xb: e.transpose(
                    out=pt[:, c8 * 128:(c8 + 1) * 128], in_=xb[:, c * 128:(c + 1) * 128], identity=identb[:, :]),
                    r=[xb_key, "identb"], w=[bk(bi)])
            S.add("act", lambda e, half=half, mbk=mbk, pt=pt: e.copy(
                out=memT[:, half * 8:(half + 1) * 8, mbk * 128:(mbk + 1) * 128],
                in_=pt.rearrange("p (c n) -> p c n", c=8)), r=[bk(bi)], w=[("memT", mbk, half)])
            mT_keys.append(("memT", mbk, half))
    wmkv_v = wmkv_d.rearrange("(c p) n -> p c n", p=128)
    for ci in range(4):
        def to_km(half, ib, n, ci=ci):
            evac(kmT[:, ci, 0:n], bank[ib][:, 0:n], r=[bk(ib)], w=[("kmT", ci)])
        proj_feat(wmkv_v, ci * 128, 128, memT, mT_keys, NDC, 256, to_km)
    for u4 in range(4):
        wt, wk = load_w(wmkv_v[:, u4 * 4:u4 * 4 + 4, 512:1024], v4)
        for k4 in range(4):
            c = u4 * 4 + k4
            for mbk in range(2):
                S.add("pe", lambda e, c=c, mbk=mbk, wt=wt, k4=k4: e.matmul(
                    out=bank[mbk][:, :], lhsT=memT[:, c, mbk * 128:(mbk + 1) * 128], rhs=wt[:, k4, :],
                    start=(c == 0), stop=(c == NDC - 1)), r=[wk] + mT_keys, w=[bk(mbk)])
    for mbk in range(2):
        evac(vmt[:, mbk, :], bank[mbk][:, :], r=[bk(mbk)], w=[("vmt", mbk)])
    for rr in range(4):
        src = ut_all[rr * 1024:(rr + 1) * 1024, :].rearrange("(j p) n -> p j n", p=128)
        S.dma("sp", tails[:, rr, :, :], src, r=["ut_all"], w=["tails"])
    S.flush()
    if stage == 20:
        es.close()
        return nc
    ak = accb[0:4, 8192:10240].bitcast(BF16)
    aq = accb[0:4, 10240:10752].bitcast(BF16)
    aqs_ring = Ring([sb("aqs%d" % i, [4, 128], BF16) for i in range(2)], "aqs")
    S.dma("sp", ak, ak_d, w=["ak"])
    S.dma("sp", aq, aq_d, w=["aq"])
    for hp in range(2):
        for rr in range(4):
            dst = kiT[hp * 64:(hp + 1) * 64, :].rearrange("p (c r i) -> p c r i", c=8, r=4)[:, :, rr, :]
            src = ki_all[rr * 64:(rr + 1) * 64, :].rearrange("p (c i) -> p c i", c=8)
            S.dma("sp", dst, src, r=["kv_all"], w=["kiT"])

    u_keys = [("uT", g, h) for g in range(4) for h in range(2)]
    WIN = (2, 4, 8, 16)
    sC = sa_ring.tiles[0][:, 0:432].rearrange("p (g t) -> p g t", g=3)
    for j in range(NSLOT):
        for i in range(4):
            if i == 3 and j == 0:
                continue
            cand = (tails[:, i, j, :] if i < 3 else tails[:, 3, j - 1, :]).rearrange("p (g t) -> p g t", g=4)
            if i == 0:
                S.add("dve", lambda e, cand=cand: e.tensor_scalar(
                    out=halo[:, :, 0:16], in0=cand, scalar1=psel[:, 0:1], scalar2=None, op0=ALU.mult),
                    r=["tails", "psel"], w=["halo"])
            else:
                S.add("dve", lambda e, cand=cand, i=i: e.scalar_tensor_tensor(
                    out=halo[:, :, 0:16], in0=cand, scalar=psel[:, i:i + 1], in1=halo[:, :, 0:16],
                    op0=ALU.mult, op1=ALU.add), r=["tails", "psel", "halo"], w=["halo"])
        S.add("dve", lambda e, j=j: e.tensor_copy(out=halo[:, :, 16:144], in_=uT[:, :, j * 128:(j + 1) * 128]),
              r=u_keys + ["halo"], w=["halo"])
        S.add("dve", lambda e: e.tensor_tensor(out=halo2[:, :, 1:144], in0=halo[:, :, 1:144], in1=halo[:, :, 0:143],
                                                op=ALU.add), r=["halo"], w=["halo2"])
        pb, pb_key = kst.next()
        pooled = pb[:, 0:512].rearrange("p (g t) -> p g t", g=4)

        def emit_pooled(g, src, src_key, j=j, pooled=pooled, pb_key=pb_key):
            if j == 0:
                S.add("dve", lambda e: e.tensor_tensor(out=src, in0=src, in1=invc0[:, g, :], op=ALU.mult),
                      r=[src_key, "invc0"], w=[src_key])
                S.add("dve", lambda e: e.tensor_tensor(out=pooled[:, g, :], in0=src, in1=halo[:, g, 16:144],
                                                        op=ALU.subtract), r=[src_key, "halo"], w=[(pb_key, g)])
            else:
                S.add("dve", lambda e: e.scalar_tensor_tensor(
                    out=pooled[:, g, :], in0=src, scalar=1.0 / WIN[g], in1=halo[:, g, 16:144],
                    op0=ALU.mult, op1=ALU.subtract), r=[src_key, "halo"], w=[(pb_key, g)])

        S.add("dve", lambda e: e.tensor_tensor(out=sC[:, :, 3:144], in0=halo2[:, 1:4, 3:144], in1=halo2[:, 1:4, 1:142],
                                                op=ALU.add), r=["halo2"], w=["sC"])
        emit_pooled(0, halo2[:, 0, 16:144], "halo2")
        S.add("dve", lambda e: e.tensor_tensor(out=halo2[:, 2:4, 7:144], in0=sC[:, 1:3, 7:144], in1=sC[:, 1:3, 3:140],
                                                op=ALU.add), r=["sC", "halo2"], w=["halo2"])
        emit_pooled(1, sC[:, 0, 16:144], "sC")
        S.add("dve", lambda e: e.tensor_tensor(out=sC[:, 2, 15:144], in0=halo2[:, 3, 15:144], in1=halo2[:, 3, 7:136],
                                                op=ALU.add), r=["halo2", "sC"], w=["sC"])
        emit_pooled(2, halo2[:, 2, 16:144], "halo2")
        emit_pooled(3, sC[:, 2, 16:144], "sC")
        for g in range(4):
            ib = fb[0] % 4
            fb[0] += 1
            S.add("pe", lambda e, g=g, ib=ib, pooled=pooled: e.matmul(
                out=bank[ib][:, 0:128], lhsT=wpool_b[:, g, :], rhs=pooled[:, g, :], start=True, stop=True),
                r=["wpool_b", (pb_key, g)], w=[bk(ib)])
            S.add("act", lambda e, g=g, ib=ib, j=j: e.activation(
                out=pT[:, g, j * 128:(j + 1) * 128], in_=bank[ib][:, 0:128], func=AF.Copy, scale=pscale[:, g:g + 1]),
                r=[bk(ib), "pscale"], w=[("pT", j)])
    S.flush()

    NIT = 20
    ATT_SCALE = 128.0 ** -0.5
    for j in range(NSLOT):
        nk = 512 * (j + 1)
        nb = 4 * (j + 1)
        S.add("dve", lambda e, j=j: e.tensor_scalar(out=negq[:, :], in0=c512[:, :], scalar1=qpos[:, j:j + 1],
                                                     scalar2=None, op0=ALU.subtract), r=["c512", "qpos"], w=["negq"])
        for c in range(j + 1):
            for h in range(16 if not TEST_SKIP else 1):
                ib = fb[0] % 4
                fb[0] += 1
                p0 = (h % 2) * 64
                S.add("pe", lambda e, c=c, h=h, ib=ib, p0=p0, j=j: e.matmul(
                    out=bank[ib][:, :], lhsT=qiT[p0:p0 + 64, h // 2, j * 128:(j + 1) * 128],
                    rhs=kiT[p0:p0 + 64, c * 512:(c + 1) * 512], start=True, stop=True),
                    r=[("qiT", h // 2, 0), ("qiT", h // 2, 1), "kiT"], w=[bk(ib)])
                rt, rt_key = sa_ring.next()
                S.add("act", lambda e, rt=rt, ib=ib, h=h, j=j: e.activation(
                    out=rt[:, :], in_=bank[ib][:, :], func=AF.Relu, scale=absw[:, j, h:h + 1]),
                    r=[bk(ib), "absw"], w=[rt_key])
                sc = score[:, c * 512:(c + 1) * 512]
                if h == 0:
                    S.add("dve", lambda e, rt=rt, sc=sc, h=h, j=j: e.tensor_scalar(
                        out=sc, in0=rt[:, :], scalar1=sgnw[:, j, h:h + 1], scalar2=None, op0=ALU.mult),
                        r=[rt_key, "sgnw"], w=[("score", c)])
                else:
                    S.add("dve", lambda e, rt=rt, sc=sc, h=h, j=j: e.scalar_tensor_tensor(
                        out=sc, in0=rt[:, :], scalar=sgnw[:, j, h:h + 1], in1=sc, op0=ALU.mult, op1=ALU.add),
                        r=[rt_key, "sgnw", ("score", c)], w=[("score", c)])
        sc_keys = [("score", c) for c in range(j + 1)]
        S.add("dve", lambda e, nk=nk: e.tensor_reduce(out=hi, in_=score[:, 0:nk], axis=mybir.AxisListType.X, op=ALU.max),
              r=sc_keys, w=["hi"])
        S.add("dve", lambda e, nk=nk: e.tensor_reduce(out=lo, in_=score[:, 0:nk], axis=mybir.AxisListType.X, op=ALU.min),
              r=sc_keys, w=["lo"])
        S.add("dve", lambda e: e.tensor_scalar(out=lo, in0=lo, scalar1=-1.0, scalar2=None, op0=ALU.add), r=["lo"], w=["lo"])
        S.add("dve", lambda e: e.scalar_tensor_tensor(out=w0, in0=hi, scalar=1.0, in1=lo, op0=ALU.add, op1=ALU.subtract),
              r=["hi", "lo"], w=["w0"])
        S.add("dve", lambda e: e.tensor_scalar(out=steps[:, :], in0=pow2[:, :], scalar1=w0, scalar2=None, op0=ALU.mult),
              r=["pow2", "w0"], w=["steps"])
        cm, cm_key = sa_ring.next()
        lastc = score[:, j * 512:(j + 1) * 512]
        S.add("dve", lambda e, cm=cm, j=j: e.tensor_scalar(out=cm[:, :], in0=iota[:, :], scalar1=negq[:, j:j + 1], scalar2=0.0,
                                                            op0=ALU.add, op1=ALU.is_le), r=["iota", "negq"], w=[cm_key])
        S.add("dve", lambda e, cm=cm, lastc=lastc: e.tensor_tensor(out=lastc, in0=lastc, in1=cm[:, :], op=ALU.mult),
              r=[cm_key, ("score", j)], w=[("score", j)])
        S.add("dve", lambda e, cm=cm: e.tensor_scalar(out=cm[:, :], in0=cm[:, :], scalar1=1e30, scalar2=-1e30,
                                                       op0=ALU.mult, op1=ALU.add), r=[cm_key], w=[cm_key])
        S.add("dve", lambda e, cm=cm, lastc=lastc: e.tensor_tensor(out=lastc, in0=lastc, in1=cm[:, :], op=ALU.add),
              r=[cm_key, ("score", j)], w=[("score", j)])
        for it in range(NIT if not TEST_SKIP else 1):
            S.add("dve", lambda e, it=it: e.tensor_tensor(out=mid, in0=lo, in1=steps[:, it:it + 1], op=ALU.add),
                  r=["lo", "steps"], w=["mid"])
            S.add("dve", lambda e, nk=nk: e.tensor_scalar(out=pj[:, 0:nk], in0=score[:, 0:nk], scalar1=mid, scalar2=None,
                                                           op0=ALU.is_ge, op1=ALU.add, accum_out=cnt),
                  r=sc_keys + ["mid"], w=["pj", "cnt"])
            S.add("dve", lambda e: e.tensor_scalar(out=ge, in0=cnt, scalar1=255.5, scalar2=None, op0=ALU.is_ge),
                  r=["cnt"], w=["ge"])
            S.add("dve", lambda e, it=it: e.scalar_tensor_tensor(out=lo, in0=ge, scalar=steps[:, it:it + 1], in1=lo,
                                                                  op0=ALU.mult, op1=ALU.add), r=["ge", "steps", "lo"], w=["lo"])
        S.add("dve", lambda e, nk=nk: e.tensor_scalar(out=mb[:, 0:nk], in0=score[:, 0:nk], scalar1=lo, scalar2=None,
                                                       op0=ALU.is_ge), r=sc_keys + ["lo"], w=["mb"])
        S.add("dve", lambda e, nk=nk: e.tensor_scalar(out=mb[:, 0:nk], in0=mb[:, 0:nk], scalar1=30000.0, scalar2=-30000.0,
                                                       op0=ALU.mult, op1=ALU.add), r=["mb"], w=["mb"])
        atok, atok_key = kst.next()
        for h in range(8):
            kh, kh_key = kh_ring.next()
            vh, vh_key = vh_ring.next()
            for rr in range(4):
                ksrc = k_all[h // 4][rr * 512 + (h % 4) * 128:rr * 512 + (h % 4 + 1) * 128, 0:128 * (j + 1)]
                S.dma("sp", kh[:, 0:nk].rearrange("p (c r i) -> p c r i", c=j + 1, r=4)[:, :, rr, :],
                      ksrc.rearrange("p (c i) -> p c i", c=j + 1), r=[], w=[kh_key])
                for vp in range(2):
                    ncp = min(4, j + 1 - 4 * vp)
                    if ncp <= 0:
                        continue
                    vsrc = v_all[vp][rr * 512:rr * 512 + 128 * ncp, h * 128:(h + 1) * 128]
                    S.dma("sp", vh[:, 16 * vp:16 * vp + 4 * ncp, :].rearrange("p (c r) d -> p c r d", r=4)[:, :, rr, :],
                          vsrc.rearrange("(c p) d -> p c d", p=128), r=[], w=[vh_key])
            aqs, aqs_key = aqs_ring.next()
            S.add("pool", lambda e, aqs=aqs, h=h, j=j: e.tensor_scalar(
                out=aqs[:, :], in0=aq[:, j * 128:(j + 1) * 128], scalar1=2.0 ** -(h + 1), scalar2=None, op0=ALU.mult),
                r=["aq"], w=[aqs_key])
            for c in range(j + 1):
                ib = fb[0] % 4
                fb[0] += 1
                cs = slice(c * 512, (c + 1) * 512)
                S.add("pe", lambda e, cs=cs, h=h, ib=ib, kh=kh, j=j: e.matmul(
                    out=bank[ib][:, :], lhsT=qT[:, h, j * 128:(j + 1) * 128], rhs=kh[:, cs],
                    start=True, stop=False), r=[("qT", h, 0), ("qT", h, 1), kh_key], w=[bk(ib)])
                S.add("pe", lambda e, cs=cs, ib=ib, aqs=aqs: e.matmul(
                    out=bank[ib][:, :], lhsT=aqs[:, :], rhs=ak[:, cs], start=False, stop=False),
                    r=[aqs_key, "ak"], w=[bk(ib)])
                S.add("pe", lambda e, cs=cs, ib=ib: e.matmul(
                    out=bank[ib][:, :], lhsT=identb[:, :], rhs=mb[:, cs], start=False, stop=True),
                    r=["identb", "mb"], w=[bk(ib)])
                S.add("act", lambda e, ib=ib, cs=cs: e.copy(out=logit[:, cs], in_=bank[ib][:, :]),
                      r=[bk(ib)], w=["logit"])
            S.add("dve", lambda e, nk=nk: e.tensor_reduce(out=rmax, in_=logit[:, 0:nk], axis=mybir.AxisListType.X,
                                                           op=ALU.max, negate=True), r=["logit"], w=["rmax"])
            S.add("act", lambda e, nk=nk: e.activation(out=pj[:, 0:nk], in_=logit[:, 0:nk], func=AF.Exp, bias=rmax,
                                                        accum_out=rsum), r=["logit", "rmax"], w=["pj", "rsum"])
            S.add("dve", lambda e: e.reciprocal(out=rinv, in_=rsum), r=["rsum"], w=["rinv"])
            for b8 in range((nb + 7) // 8):
                bi = 4 + (tr_rr[0] % 2)
                tr_rr[0] += 1
                n8 = min(8, nb - b8 * 8)
                pt = bank[bi][:, :].bitcast(BF16)
                for k in range(n8):
                    blk = b8 * 8 + k
                    S.add("pe", lambda e, k=k, blk=blk, pt=pt: e.transpose(
                        out=pt[:, k * 128:(k + 1) * 128], in_=pj[:, blk * 128:(blk + 1) * 128], identity=identb[:, :]),
                        r=["pj", "identb"], w=[bk(bi)])
                S.add("act", lambda e, b8=b8, n8=n8, pt=pt: e.copy(
                    out=PT[:, b8 * 8:b8 * 8 + n8, :], in_=pt[:, 0:n8 * 128].rearrange("p (b t) -> p b t", b=n8)),
                    r=[bk(bi)], w=[("PT", b8)])
            ib = 6 + (h % 2)
            pt_keys = [("PT", b8) for b8 in range((nb + 7) // 8)]
            for blk in range(nb):
                S.add("pe", lambda e, blk=blk, ib=ib, vh=vh, nb=nb: e.matmul(
                    out=bank[ib][:, 0:128], lhsT=PT[:, blk, :], rhs=vh[:, blk, :], start=(blk == 0), stop=(blk == nb - 1)),
                    r=pt_keys + [vh_key], w=[bk(ib)])
            S.add("act", lambda e, ib=ib, h=h, atok=atok: e.activation(
                out=atok[:, h * 128:(h + 1) * 128], in_=bank[ib][:, 0:128], func=AF.Copy, scale=rinv),
                r=[bk(ib), "rinv"], w=[(atok_key, h)])
        bi = 4 + (tr_rr[0] % 2)
        tr_rr[0] += 1
        pt = bank[bi][:, :].bitcast(BF16)
        for h in range(8):
            S.add("pe", lambda e, h=h, pt=pt, atok=atok: e.transpose(
                out=pt[:, h * 128:(h + 1) * 128], in_=atok[:, h * 128:(h + 1) * 128], identity=identb[:, :]),
                r=[(atok_key, h), "identb"], w=[bk(bi)])
        S.add("act", lambda e, pt=pt, j=j: e.copy(out=qT[:, :, j * 128:(j + 1) * 128],
                                                   in_=pt.rearrange("p (h t) -> p h t", h=8)),
              r=[bk(bi)], w=[("qT", h, j // 4) for h in range(8)])
        mtok, mtok_key = kst.next()
        for h in range(4):
            ib = fb[0] % 4
            fb[0] += 1
            S.add("pe", lambda e, h=h, ib=ib, j=j: e.matmul(
                out=bank[ib][:, 0:256], lhsT=qmT[:, h, j * 128:(j + 1) * 128], rhs=kmT[:, h, :], start=True, stop=True),
                r=[("qmT", h, 0), ("qmT", h, 1), ("kmT", h)], w=[bk(ib)])
            S.add("dve", lambda e, ib=ib: e.tensor_reduce(out=rmax, in_=bank[ib][:, 0:256], axis=mybir.AxisListType.X,
                                                           op=ALU.max), r=[bk(ib)], w=["rmax"])
            S.add("dve", lambda e: e.tensor_scalar(out=rmax, in0=rmax, scalar1=-ATT_SCALE, scalar2=None, op0=ALU.mult),
                  r=["rmax"], w=["rmax"])
            S.add("act", lambda e, ib=ib: e.activation(out=pj[:, 0:256], in_=bank[ib][:, 0:256], func=AF.Exp, bias=rmax,
                                                        scale=ATT_SCALE, accum_out=rsum), r=[bk(ib), "rmax"], w=["pj", "rsum"])
            S.add("dve", lambda e: e.reciprocal(out=rinv, in_=rsum), r=["rsum"], w=["rinv"])
            bi = 4 + (tr_rr[0] % 2)
            tr_rr[0] += 1
            pt = bank[bi][:, :].bitcast(BF16)
            for k in range(2):
                S.add("pe", lambda e, k=k, pt=pt: e.transpose(
                    out=pt[:, k * 128:(k + 1) * 128], in_=pj[:, k * 128:(k + 1) * 128], identity=identb[:, :]),
                    r=["pj", "identb"], w=[bk(bi)])
            evac(PT[:, 0:2, :], pt[:, 0:256].rearrange("p (b t) -> p b t", b=2), r=[bk(bi)], w=[("PT", 0)])
            ib2 = 6 + (h % 2)
            for k in range(2):
                S.add("pe", lambda e, k=k, ib2=ib2, h=h: e.matmul(
                    out=bank[ib2][:, 0:128], lhsT=PT[:, k, :], rhs=vmt[:, k, h * 128:(h + 1) * 128],
                    start=(k == 0), stop=(k == 1)), r=[("PT", 0), ("vmt", 0), ("vmt", 1)], w=[bk(ib2)])
            S.add("act", lambda e, ib2=ib2, h=h, mtok=mtok: e.activation(
                out=mtok[:, h * 128:(h + 1) * 128], in_=bank[ib2][:, 0:128], func=AF.Copy, scale=rinv),
                r=[bk(ib2), "rinv"], w=[(mtok_key, h)])
        bi = 4 + (tr_rr[0] % 2)
        tr_rr[0] += 1
        pt = bank[bi][:, :].bitcast(BF16)
        for h in range(4):
            S.add("pe", lambda e, h=h, pt=pt, mtok=mtok: e.transpose(
                out=pt[:, h * 128:(h + 1) * 128], in_=mtok[:, h * 128:(h + 1) * 128], identity=identb[:, :]),
                r=[(mtok_key, h), "identb"], w=[bk(bi)])
        S.add("act", lambda e, pt=pt, j=j: e.copy(out=qmT[:, :, j * 128:(j + 1) * 128],
                                                   in_=pt[:, 0:512].rearrange("p (h t) -> p h t", h=4)),
              r=[bk(bi)], w=[("qmT", h, j // 4) for h in range(4)])
    S.flush()

    if stage == 3:
        for c in range(16):
            src = qT[:, c, :] if c < 8 else (pT[:, c - 8, :] if c < 12 else qmT[:, c - 12, :])
            st, st_key = wst.next()
            S.add("dve", lambda e, st=st, src=src: e.tensor_copy(out=st[:, 0:1024], in_=src), w=[st_key])
            S.dma("sp", out_d[c * 64:(c + 1) * 64, :].rearrange("r (pl t) -> (r pl) t", pl=2), st[:, 0:1024], r=[st_key])
        S.flush()
        es.close()
        return nc
    for j in range(NSLOT):
        xt, xt_key = accb[:, 0:2048] if False else (wst.tiles[j % 2][:, :], ("wst", j % 2))
        S.dma("sp", xt, h1_scr[j // 4][(j % 4) * 128:(j % 4 + 1) * 128, :], w=[xt_key])
        S.add("act", lambda e, j=j, xt=xt: e.mul(out=acc[:, j, :], in_=xt, mul=ALPHA), r=[xt_key], w=[("acc", j)])
        to_hT(j, xt, xt_key)
    S.flush()
    yT = qiT
    wbf_cur[0] = Ring(wbf.tiles + [regG[:, 12 * T:14 * T], regG[:, 14 * T:16 * T]], "wbf")
    sg = [xbf.tiles[0][:, 0:1024].bitcast(F32), xbf.tiles[0][:, 1024:2048].bitcast(F32),
          xbf.tiles[1][:, 0:1024].bitcast(F32), xbf.tiles[1][:, 1024:2048].bitcast(F32)]
    wba_v = wba_d.rearrange("(c p) n -> p c n", p=128)
    wbp_v = wbp_d.rearrange("(c p) n -> p c n", p=128)
    wbm_v = wbm_d.rearrange("(c p) n -> p c n", p=128)
    wout_v = wout_d.rearrange("(c p) n -> p c n", p=128)
    for G in range(4):
        for n4 in range(4):
            n = G * 4 + n4
            wts = []
            for i in range(3):
                col0 = 5200 + i * D + n * 128
                wts.append(load_w(win_v[:, :, col0:col0 + 128], v3))
            st, st_key = wst.next()
            bf, bf_key = wbf_cur[0].next()
            cols = slice(n * 128, (n + 1) * 128)
            vv = lambda t, a, b, c: t[:, a:b].rearrange("p (c n) -> p c n", c=c)
            S.dma("sp", vv(st, 0, 1024, 8), wba_v[:, :, cols], w=[st_key])
            S.dma("sp", vv(st, 1024, 1536, 4), wbp_v[:, :, cols], w=[st_key])
            S.dma("sp", vv(st, 1536, 2048, 4), wbm_v[:, :, cols], w=[st_key])
            cast(bf[:, :], st[:, :], r=[st_key], w=[bf_key])
            wb = [(vv(bf, 0, 1024, 8), bf_key), (vv(bf, 1024, 1536, 4), bf_key), (vv(bf, 1536, 2048, 4), bf_key)]
            brs = [(qT, 8), (pT, 4), (qmT, 4)]
            for half in range(2):
                ts = slice(half * 512, (half + 1) * 512)
                for i in range(3):
                    wt, wk = wts[i]
                    for c in range(NDC):
                        S.add("pe", lambda e, wt=wt, c=c, i=i, ts=ts: e.matmul(
                            out=bank[i][:, :], lhsT=wt[:, c, :], rhs=hT[:, c, ts], start=(c == 0), stop=(c == NDC - 1)),
                            r=[wk] + hT_all, w=[bk(i)])
                    S.add("act", lambda e, i=i, n=n: e.activation(
                        out=sg[i], in_=bank[i][:, :], func=AF.Sigmoid, bias=bgate[:, i * 16 + n:i * 16 + n + 1]),
                        r=[bk(i), "bgate"], w=[("sg", i)])
                for i in range(3):
                    wt, wk = wb[i]
                    src, nkc = brs[i]
                    for c in range(nkc):
                        S.add("pe", lambda e, wt=wt, c=c, i=i, ts=ts, src=src, nkc=nkc: e.matmul(
                            out=bank[3 + i][:, :], lhsT=wt[:, c, :], rhs=src[:, c, ts], start=(c == 0), stop=(c == nkc - 1)),
                            r=[wk], w=[bk(3 + i)])
                    S.add("dve", lambda e, i=i: e.tensor_tensor(out=sg[i], in0=sg[i], in1=bank[3 + i][:, :], op=ALU.mult),
                          r=[("sg", i), bk(3 + i)], w=[("sg", i)])
                S.add("dve", lambda e: e.tensor_tensor(out=sg[0], in0=sg[0], in1=sg[1], op=ALU.add),
                      r=[("sg", 0), ("sg", 1)], w=[("sg", 0)])
                S.add("dve", lambda e, n4=n4, ts=ts: e.tensor_tensor(out=yT[:, n4, ts], in0=sg[0], in1=sg[2], op=ALU.add),
                      r=[("sg", 0), ("sg", 2)], w=[("yT", n4, half)])
        y_all = [("yT", n4, h) for n4 in range(4) for h in range(2)]
        for dq in range(4):
            wt, wk = load_w(wout_v[:, G * 4:G * 4 + 4, dq * 512:(dq + 1) * 512], v4)
            for k4 in range(4):
                for j in range(NSLOT):
                    S.add("pe", lambda e, k4=k4, j=j, wt=wt: e.matmul(
                        out=bank[j][:, :], lhsT=yT[:, k4, j * 128:(j + 1) * 128], rhs=wt[:, k4, :],
                        start=(k4 == 0), stop=(k4 == 3)), r=[wk] + y_all, w=[bk(j)])
            for j in range(NSLOT):
                dst = acc[:, j, dq * 512:(dq + 1) * 512]
                S.add("dve", lambda e, dst=dst, j=j: e.tensor_tensor(out=dst, in0=dst, in1=bank[j][:, :], op=ALU.add),
                      r=[bk(j), ("acc", j)], w=[("acc", j)])
    S.flush()
    wbf_cur[0] = wbf
    if stage == 4:
        layernorm(ln2g_d, ln2b_d, store_d=out_d, make_hT=True, post_scale=ALPHA)
        es.close()
        return nc
    layernorm(ln2g_d, ln2b_d, store_d=None, make_hT=True, post_scale=ALPHA)

    ffn(w2u_d, w2d_d)
    layernorm(ln3g_d, ln3b_d, store_d=out_d, make_hT=False)

    es.close()
    return nc


def _prep_inputs(inputs):
    f = lambda k: np.asarray(inputs[k], dtype=np.float32)
    x = f("x")
    mem = f("mem")
    shared = {"ident": np.eye(128, dtype=np.float32)}
    for k in ("w_ffn1_up", "w_ffn1_down", "w_in", "w_mem_kv", "w_br_att", "w_br_pool", "w_br_mem", "w_out",
              "w_ffn2_up", "w_ffn2_down"):
        shared[k] = np.ascontiguousarray(f(k)[0])
    for k in ("ln1_g", "ln1_b", "ln2_g", "ln2_b", "ln3_g", "ln3_b"):
        shared[k] = np.ascontiguousarray(f(k).reshape(1, D))
    shared["b_gate"] = np.ascontiguousarray(f("b_gate").reshape(48, 128).T)
    shared["w_pool"] = np.ascontiguousarray(f("w_pool")[0].transpose(1, 0, 2))
    shared["pool_scale"] = np.ascontiguousarray(f("pool_scale").reshape(4, 128).T)
    shared["iota512"] = np.arange(512, dtype=np.float32).reshape(1, 512)
    shared["c512"] = (512.0 * np.arange(8, dtype=np.float32)).reshape(1, 8)
    shared["pow2"] = (2.0 ** -(np.arange(32, dtype=np.float64) + 1)).astype(np.float32).reshape(1, 32)
    shared["slopes"] = (2.0 ** -(np.arange(8, dtype=np.float64) + 1)).astype(np.float32).reshape(1, 8)
    wins = np.array([2, 4, 8, 16], dtype=np.float32)
    import ml_dtypes
    kp = np.arange(SEQ)
    shared["ak"] = np.stack([kp // 64, kp % 64, np.ones(SEQ), np.ones(SEQ)]).astype(np.float32).astype(ml_dtypes.bfloat16)
    maps = []
    for c in range(NCORE):
        b, r = divmod(c, 4)
        m = dict(shared)
        m["x"] = np.ascontiguousarray(x[b].reshape(32, 128, D)[r::4].reshape(T, D))
        m["mem"] = np.ascontiguousarray(mem[b])
        p = np.arange(128, dtype=np.float32)[:, None]
        j = np.arange(NSLOT, dtype=np.float32)[None, :]
        m["qpos"] = np.ascontiguousarray((4 * j + r) * 128 + p).astype(np.float32)
        t = np.arange(128, dtype=np.float32)[None, :]
        if r == 0:
            invc = 1.0 / np.minimum(t + 1.0, wins[:, None])
        else:
            invc = np.broadcast_to(1.0 / wins[:, None], (4, 128))
        m["invc0"] = np.ascontiguousarray(invc, dtype=np.float32).reshape(1, 512)
        sel = np.zeros((1, 4), dtype=np.float32)
        sel[0, (r - 1) % 4] = 1.0
        m["psel"] = sel
        qp = ((4 * np.arange(NSLOT)[:, None] + r) * 128 + np.arange(128)[None, :]).reshape(-1)
        m["aq"] = np.stack([np.full(T, 64.0), np.ones(T), -64.0 * (qp // 64), -1.0 * (qp % 64)]).astype(np.float32).astype(ml_dtypes.bfloat16)
        maps.append(m)
    return maps


def _assemble(results):
    out = np.zeros((2, SEQ, D), dtype=np.float32)
    for c in range(NCORE):
        b, r = divmod(c, 4)
        o = np.asarray(results[c]["out"]).reshape(NSLOT, 128, D)
        out[b].reshape(32, 128, D)[r::4] = o
    return out


def kernel(**inputs):
    nc = build_program(stage=DEBUG_STAGE)
    maps = _prep_inputs(inputs)
    res = run_bass_kernel_spmd(nc, maps, core_ids=list(range(NCORE)))
    return _assemble(res.results)
```

```python
import numpy as np
import concourse.bass as bass
import concourse.mybir as mybir
from concourse.bass_utils import run_bass_kernel_spmd

F32 = mybir.dt.float32
BF16 = mybir.dt.bfloat16
AF = mybir.ActivationFunctionType
ALU = mybir.AluOpType

D = 2048
SEQ = 4096
NCORE = 8
T = 1024
NSLOT = 8
DFF = 5632
NFC = DFF // 128
NDC = D // 128
ALPHA = 2.0 ** 0.25
LN_EPS = 1e-5
DIN = 11344

DEBUG_STAGE = None
TEST_SKIP = False


class Sched:
    COMPUTE = ("pe", "act", "dve", "pool")
    ALL = ("pe", "act", "dve", "pool", "sp")

    def __init__(self, nc, sems, dsems):
        self.nc = nc
        self.sem = sems
        self.dsem = dsems
        self.cnt = {e: 0 for e in self.COMPUTE}
        self.ops = {e: [] for e in self.ALL}
        self.waited = {e: {} for e in self.ALL}
        self.lastw = {}
        self.readers = {}
        self.dma_k = 0
        self.dma_val = [0] * len(dsems)
        self.dma_owner = [None] * len(dsems)
        self.nops = 0

    def _deps(self, eng, r, w):
        raw = {}
        oth = {}

        def put(d, tok):
            k, v = tok
            if d.get(k, 0) < v:
                d[k] = v

        for res in r:
            if res in self.lastw:
                put(raw, self.lastw[res])
        for res in w:
            if res in self.lastw:
                put(oth, self.lastw[res])
            for k, v in self.readers.get(res, {}).items():
                put(oth, (k, v))
        waits = {}
        for k, v in raw.items():
            if k == eng and eng == "pe":
                continue
            put(waits, (k, v))
        for k, v in oth.items():
            if k == eng:
                continue
            put(waits, (k, v))
        out = []
        wd = self.waited[eng]
        for k, v in waits.items():
            if wd.get(k, 0) >= v:
                continue
            wd[k] = v
            out.append((k, v))
        return out

    def _commit(self, tok, r, w):
        for res in w:
            self.lastw[res] = tok
            self.readers[res] = {}
        for res in r:
            d = self.readers.setdefault(res, {})
            if d.get(tok[0], 0) < tok[1]:
                d[tok[0]] = tok[1]

    def add(self, eng, fn, r=(), w=()):
        waits = self._deps(eng, r, w)
        self.cnt[eng] += 1
        tok = (eng, self.cnt[eng])
        self.ops[eng].append((waits, fn, tok, "c"))
        self._commit(tok, r, w)
        self.nops += 1
        return tok

    def dma(self, q, out, in_, r=(), w=(), **kw):
        waits = self._deps(q, r, w)
        i = self.dma_k % len(self.dsem)
        self.dma_k += 1
        prev = self.dma_val[i]
        key = ("d", i)
        if prev > 0 and self.waited[q].get(key, 0) < prev:
            self.waited[q][key] = prev
            waits.append((key, prev))
        self.dma_val[i] = prev + 16
        self.dma_owner[i] = q
        tok = (key, prev + 16)
        self.ops[q].append((waits, lambda e: e.dma_start(out=out, in_=in_, **kw), tok, "d"))
        self._commit(tok, r, w)
        self.nops += 1
        return tok

    def coll(self, i, fn, r=(), w=()):
        waits = self._deps("pool", r, w)
        tok = (("c", i), 1)
        self.cc_pending = getattr(self, "cc_pending", []) + [tok]
        self.ops["pool"].append((waits, fn, tok, "cc"))
        self._commit(tok, r, w)
        return tok

    def _semobj(self, key):
        if isinstance(key, tuple):
            if key[0] == "c":
                return self.csem[key[1]]
            return self.dsem[key[1]]
        return self.sem[key]

    def flush(self):
        nc = self.nc
        with nc.Block() as block:
            decos = {"sp": block.sync, "act": block.scalar, "dve": block.vector,
                     "pool": block.gpsimd, "pe": block.tensor}
            for eng in self.ALL:
                ops = self.ops[eng]
                tail = [(("d", i), self.dma_val[i]) for i in range(len(self.dsem))
                        if self.dma_owner[i] == eng and self.dma_val[i] > 0]
                if eng == "pool":
                    tail = tail + list(getattr(self, "cc_pending", []))

                def body(e, ops=ops, eng=eng, tail=tail):
                    for waits, fn, tok, kind in ops:
                        for k, v in waits[1:]:
                            e.wait_ge(self._semobj(k), v)
                        ins = fn(e)
                        if waits:
                            ins.wait_op(self._semobj(waits[0][0]), waits[0][1], "sem-ge")
                        if kind == "c":
                            ins.then_inc(self.sem[eng], 1)
                        elif kind == "cc":
                            ins.then_inc(self.csem[tok[0][1]])
                        else:
                            ins.then_inc(self.dsem[tok[0][1]], 16)
                    for k, v in tail:
                        e.wait_ge(self._semobj(k), v)

                decos[eng](body)
        for eng in self.ALL:
            self.ops[eng] = []
            for i in range(len(self.dsem)):
                if self.dma_owner[i] is not None:
                    self.waited[eng][("d", i)] = self.dma_val[i]
            for c in self.COMPUTE:
                self.waited[eng][c] = self.cnt[c]
        self.lastw = {}
        self.readers = {}
        self.cc_pending = []


class Ring:
    def __init__(self, tiles, name):
        self.tiles = tiles
        self.name = name
        self.i = 0

    def next(self):
        k = self.i % len(self.tiles)
        self.i += 1
        return self.tiles[k], (self.name, k)


def build_program(stage=None):
    from contextlib import ExitStack
    nc = bass.Bass("TRN2", target_bir_lowering=False)

    def din(name, shape, dt=F32):
        return nc.dram_tensor(name, list(shape), dt, kind="ExternalInput").ap()

    x_d = din("x", [T, D])
    w1u_d = din("w_ffn1_up", [D, 2 * DFF])
    w1d_d = din("w_ffn1_down", [DFF, D])
    ln1g_d = din("ln1_g", [1, D])
    ln1b_d = din("ln1_b", [1, D])
    ident_d = din("ident", [128, 128])
    win_d = din("w_in", [D, DIN])
    bgate_d = din("b_gate", [128, 48])
    wmkv_d = din("w_mem_kv", [D, 1024])
    mem_d = din("mem", [256, D])
    wpool_d = din("w_pool", [128, 4, 128])
    pscale_d = din("pool_scale", [128, 4])
    wba_d = din("w_br_att", [1024, D])
    wbp_d = din("w_br_pool", [512, D])
    wbm_d = din("w_br_mem", [512, D])
    wout_d = din("w_out", [D, D])
    ln2g_d = din("ln2_g", [1, D])
    ln2b_d = din("ln2_b", [1, D])
    w2u_d = din("w_ffn2_up", [D, 2 * DFF])
    w2d_d = din("w_ffn2_down", [DFF, D])
    ln3g_d = din("ln3_g", [1, D])
    ln3b_d = din("ln3_b", [1, D])
    qpos_d = din("qpos", [128, NSLOT])
    iota_d = din("iota512", [1, 512])
    invc0_d = din("invc0", [1, 4 * 128])
    psel_d = din("psel", [1, 4])
    pow2_d = din("pow2", [1, 32])
    slopes_d = din("slopes", [1, 8])
    c512_d = din("c512", [1, 8])
    aq_d = din("aq", [4, T], BF16)
    ak_d = din("ak", [4, SEQ], BF16)
    h1_scr = [nc.dram_tensor("h1_scr%d" % i, [512, D], F32).ap() for i in range(2)]
    k_loc = [nc.dram_tensor("k_loc%d" % i, [512, 1024], BF16).ap() for i in range(2)]
    k_all = [nc.dram_tensor("k_all%d" % i, [4 * 512, 1024], BF16).ap() for i in range(2)]
    v_loc = [nc.dram_tensor("v_loc%d" % i, [512, 1024], BF16).ap() for i in range(2)]
    v_all = [nc.dram_tensor("v_all%d" % i, [4 * 512, 1024], BF16).ap() for i in range(2)]
    ki_loc = nc.dram_tensor("ki_loc", [64, 1024], BF16).ap()
    ki_all = nc.dram_tensor("ki_all", [4 * 64, 1024], BF16).ap()
    ut_loc = nc.dram_tensor("ut_loc", [NSLOT * 128, 64], F32).ap()
    ut_all = nc.dram_tensor("ut_all", [4 * NSLOT * 128, 64], F32).ap()
    out_d = nc.dram_tensor("out", [T, D], F32, kind="ExternalOutput").ap()

    es = ExitStack()

    def sb(name, shape, dt):
        return es.enter_context(nc.sbuf_tensor("sb_" + name, list(shape), dt))

    sems = {e: es.enter_context(nc.semaphore("s_" + e)) for e in Sched.COMPUTE}
    dsems = [es.enter_context(nc.semaphore("d%d" % i)) for i in range(24)]
    csems = [es.enter_context(nc.semaphore("c%d" % i)) for i in range(6)]
    S = Sched(nc, sems, dsems)
    S.csem = csems

    bank = [es.enter_context(nc.psum_tensor("bank%d" % i, [128, 512], F32)) for i in range(8)]

    def bk(i):
        return ("bank", i)

    hT = sb("hT", [128, NDC, T], BF16)
    acc = sb("acc", [128, NSLOT, D], F32)
    identb = sb("identb", [128, 128], BF16)
    identf = sb("identf", [128, 128], F32)
    wst = Ring([sb("wst%d" % i, [128, 2048], F32) for i in range(3)], "wst")
    wbf = Ring([sb("wbf%d" % i, [128, 2048], BF16) for i in range(3)], "wbf")
    regG = sb("regG", [128, 24 * T], BF16)
    xbf = Ring([sb("xbf%d" % i, [128, D], BF16) for i in range(2)], "xbf")
    sa_ring = Ring([sb("sa%d" % i, [128, 512], F32) for i in range(2)], "sa")
    stats = sb("stats", [128, 4, 6], F32)
    mv = sb("mv", [128, 2], F32)
    rstd = sb("rstd", [128, 1], F32)
    epsb = sb("epsb", [128, 1], F32)

    cast_rr = [0]

    cast_engs = [("act", "dve", "pool")]

    def cast(out, in_, r, w):
        engs = cast_engs[0]
        eng = engs[cast_rr[0] % len(engs)]
        cast_rr[0] += 1
        if eng == "act":
            S.add("act", lambda e: e.copy(out=out, in_=in_), r=r, w=w)
        else:
            S.add(eng, lambda e: e.tensor_copy(out=out, in_=in_), r=r, w=w)

    wbf_cur = [wbf]

    def load_w(src_ap, view):
        st, st_key = wst.next()
        bf, bf_key = wbf_cur[0].next()
        S.dma("sp", view(st), src_ap, w=[st_key])
        cast(view(bf), view(st), r=[st_key], w=[bf_key])
        return view(bf), bf_key

    S.dma("sp", identf[:, :], ident_d, w=["identf"])
    S.add("dve", lambda e: e.tensor_copy(out=identb[:, :], in_=identf[:, :]), r=["identf"], w=["identb"])
    S.add("dve", lambda e: e.memset(epsb[:, :], LN_EPS), w=["epsb"])

    tr_rr = [0]

    def to_hT(j, src_f32, src_key):
        xb, xb_key = xbf.next()
        S.add("dve", lambda e: e.tensor_copy(out=xb[:, :], in_=src_f32), r=[src_key], w=[xb_key])
        for half in range(2):
            bi = 6 + (tr_rr[0] % 2)
            tr_rr[0] += 1
            pt = bank[bi][:, :].bitcast(BF16)
            for c8 in range(8):
                c = half * 8 + c8
                S.add("pe", lambda e, c=c, c8=c8, pt=pt: e.transpose(
                    out=pt[:, c8 * 128:(c8 + 1) * 128], in_=xb[:, c * 128:(c + 1) * 128], identity=identb[:, :]),
                    r=[xb_key, "identb"], w=[bk(bi)])
            dst = hT[:, half * 8:(half + 1) * 8, j * 128:(j + 1) * 128]
            src = pt.rearrange("p (c n) -> p c n", c=8)
            S.add("act", lambda e, dst=dst, src=src: e.copy(out=dst, in_=src),
                  r=[bk(bi)], w=[("hT", j)])

    xin = Ring([regG[:, 0:2 * D].bitcast(F32), regG[:, 2 * D:4 * D].bitcast(F32)], "xin")
    for j in range(NSLOT):
        xt, xt_key = xin.next()
        S.dma("sp", xt, x_d[j * 128:(j + 1) * 128, :], w=[xt_key])
        S.add("act", lambda e, j=j, xt=xt: e.mul(out=acc[:, j, :], in_=xt, mul=ALPHA),
              r=[xt_key], w=[("acc", j)])
        to_hT(j, xt, xt_key)
    if stage == 0:
        for j in range(NSLOT):
            S.dma("sp", out_d[j * 128:(j + 1) * 128, :], acc[:, j, :], r=[("acc", j)])
        S.flush()
        es.close()
        return nc
    S.flush()

    hT_all = [("hT", j) for j in range(NSLOT)]

    def ffn(wu_d, wd_d):
        gT = regG
        wu_v = wu_d.rearrange("(c p) n -> p c n", p=128)
        wd_v = wd_d.rearrange("(c p) n -> p c n", p=128)
        groups = [(0, 24), (24, 20)]
        bno = [0]
        v3 = lambda t: t[:, :].rearrange("p (c n) -> p c n", c=16)
        v4 = lambda t: t[:, :].rearrange("p (c n) -> p c n", c=4)
        for (f0, nf) in groups:
            for fl in range(nf):
                f = f0 + fl
                wa, wa_key = load_w(wu_v[:, :, f * 128:(f + 1) * 128], v3)
                wu, wu_key = load_w(wu_v[:, :, DFF + f * 128:DFF + (f + 1) * 128], v3)
                for half in range(2):
                    ia = bno[0] % 4
                    iu = (bno[0] + 1) % 4
                    bno[0] += 2
                    for (wt, wk, ib) in ((wa, wa_key, ia), (wu, wu_key, iu)):
                        for c in range(NDC):
                            S.add("pe", lambda e, wt=wt, ib=ib, c=c, half=half: e.matmul(
                                out=bank[ib][:, :], lhsT=wt[:, c, :], rhs=hT[:, c, half * 512:(half + 1) * 512],
                                start=(c == 0), stop=(c == NDC - 1)),
                                r=[wk] + hT_all, w=[bk(ib)])
                    sa, sa_key = sa_ring.next()
                    S.add("act", lambda e, sa=sa, ia=ia: e.activation(out=sa[:, :], in_=bank[ia][:, :], func=AF.Silu),
                          r=[bk(ia)], w=[sa_key])
                    gdst = gT[:, fl * T + half * 512: fl * T + (half + 1) * 512]
                    S.add("dve", lambda e, gdst=gdst, sa=sa, iu=iu: e.tensor_tensor(
                        out=gdst, in0=sa[:, :], in1=bank[iu][:, :], op=ALU.mult),
                        r=[sa_key, bk(iu)], w=[("gT", fl, half)])
            g_all = [("gT", fl, h) for fl in range(nf) for h in range(2)]
            for dq in range(4):
                for u4 in range(nf // 4):
                    c0 = f0 + u4 * 4
                    wd, wd_key = load_w(wd_v[:, c0:c0 + 4, dq * 512:(dq + 1) * 512], v4)
                    for k4 in range(4):
                        fl = u4 * 4 + k4
                        for j in range(NSLOT):
                            S.add("pe", lambda e, fl=fl, j=j, wd=wd, k4=k4, nf=nf: e.matmul(
                                out=bank[j][:, :], lhsT=gT[:, fl * T + j * 128: fl * T + (j + 1) * 128],
                                rhs=wd[:, k4, :], start=(fl == 0), stop=(fl == nf - 1)),
                                r=[wd_key] + g_all, w=[bk(j)])
                for j in range(NSLOT):
                    dst = acc[:, j, dq * 512:(dq + 1) * 512]
                    S.add("dve", lambda e, dst=dst, j=j: e.scalar_tensor_tensor(
                        out=dst, in0=bank[j][:, :], scalar=0.5, in1=dst, op0=ALU.mult, op1=ALU.add),
                        r=[bk(j), ("acc", j)], w=[("acc", j)])
        S.flush()

    def layernorm(g_d, b_d, store_d=None, make_hT=True, post_scale=None):
        gt = regG[:, 0:2 * D].bitcast(F32)
        bt = regG[:, 2 * D:4 * D].bitcast(F32)
        S.dma("sp", gt, g_d.partition_broadcast(128) if False else g_d.broadcast_to([128, D]), w=["lng"])
        S.dma("sp", bt, b_d.broadcast_to([128, D]), w=["lnb"])
        for j in range(NSLOT):
            a_j = acc[:, j, :]
            for q in range(4):
                S.add("dve", lambda e, j=j, q=q: e.bn_stats(out=stats[:, q, :], in_=acc[:, j, q * 512:(q + 1) * 512]),
                      r=[("acc", j)], w=[("stats", q)])
            S.add("dve", lambda e: e.bn_aggr(out=mv[:, :], in_=stats[:, :, :]),
                  r=[("stats", q) for q in range(4)], w=["mv"])
            S.add("act", lambda e: e.activation(out=rstd[:, :], in_=mv[:, 1:2], func=AF.Sqrt, bias=epsb[:, :]),
                  r=["mv", "epsb"], w=["rstd"])
            S.add("dve", lambda e: e.reciprocal(out=rstd[:, :], in_=rstd[:, :]), r=["rstd"], w=["rstd"])
            S.add("dve", lambda e, a_j=a_j: e.tensor_scalar(
                out=a_j, in0=a_j, scalar1=mv[:, 0:1], scalar2=rstd[:, :], op0=ALU.subtract, op1=ALU.mult),
                r=[("acc", j), "mv", "rstd"], w=[("acc", j)])
            S.add("pool", lambda e, a_j=a_j: e.tensor_tensor(out=a_j, in0=a_j, in1=gt, op=ALU.mult),
                  r=[("acc", j), "lng"], w=[("acc", j)])
            S.add("dve", lambda e, a_j=a_j: e.tensor_tensor(out=a_j, in0=a_j, in1=bt, op=ALU.add),
                  r=[("acc", j), "lnb"], w=[("acc", j)])
            if store_d is not None:
                sd = store_d[j // 4][(j % 4) * 128:(j % 4 + 1) * 128, :] if isinstance(store_d, list) else store_d[j * 128:(j + 1) * 128, :]
                S.dma("sp", sd, a_j, r=[("acc", j)])
            if make_hT:
                to_hT(j, a_j, ("acc", j))
            if post_scale is not None:
                S.add("act", lambda e, a_j=a_j: e.mul(out=a_j, in_=a_j, mul=post_scale),
                      r=[("acc", j)], w=[("acc", j)])
        S.flush()

    ffn(w1u_d, w1d_d)
    if stage == 1:
        layernorm(ln1g_d, ln1b_d, store_d=out_d, make_hT=True)
        es.close()
        return nc
    layernorm(ln1g_d, ln1b_d, store_d=h1_scr, make_hT=True)

    accb = acc[:, :, :].rearrange("p a b -> p (a b)")
    score = accb[:, 0:4096]
    logit = accb[:, 4096:8192]
    rel = accb[:, 8192:12288]
    mb = accb[:, 12288:14336].bitcast(BF16)
    pj = accb[:, 14336:16384].bitcast(BF16)
    uT = accb[:, 0:4096].rearrange("p (g t) -> p g t", g=4)
    vtok = accb[:, 4096:8192].bitcast(BF16).rearrange("p (j n) -> p j n", j=NSLOT)
    qT = regG[:, 0:8 * T].rearrange("p (h t) -> p h t", h=8)
    qiT = regG[:, 8 * T:16 * T].rearrange("p (h t) -> p h t", h=8)
    qmT = regG[:, 16 * T:20 * T].rearrange("p (h t) -> p h t", h=4)
    pT = regG[:, 20 * T:24 * T].rearrange("p (h t) -> p h t", h=4)
    hTf = hT[:, :, :].rearrange("p c t -> p (c t)")
    kiT = hTf[:, 0:4096]
    PT = hTf[:, 4096:8192].rearrange("p (b t) -> p b t", b=32)
    kh_ring = Ring([hTf[:, 8192:12288], wst.tiles[0][:, :].bitcast(BF16)], "kh")
    vh_ring = Ring([hTf[:, 12288:16384].rearrange("p (b d) -> p b d", b=32),
                    wst.tiles[1][:, :].bitcast(BF16).rearrange("p (b d) -> p b d", b=32)], "vh")
    memT = accb[:, 8192:10240].bitcast(BF16).rearrange("p (c m) -> p c m", c=16)
    wi_t = sb("wi_t", [128, NSLOT, 16], F32)
    absw = sb("absw", [128, NSLOT, 16], F32)
    sgnw = sb("sgnw", [128, NSLOT, 16], F32)
    qpos = sb("qpos", [128, NSLOT], F32)
    qoff = sb("qoff", [128, 8], F32)
    iota = sb("iota", [128, 512], F32)
    pow2 = sb("pow2", [128, 32], F32)
    steps = sb("steps", [128, 32], F32)
    slopes = sb("slopes", [128, 8], F32)
    psel = sb("psel", [128, 4], F32)
    invc0 = sb("invc0", [128, 4, 128], F32)
    pscale = sb("pscale", [128, 4], F32)
    bgate = sb("bgate", [128, 48], F32)
    wpool_f = accb[:, 15488:16000].rearrange("p (g d) -> p g d", g=4)
    wpool_b = sb("wpool_b", [128, 4, 128], BF16)
    c512 = sb("c512", [128, 8], F32)
    negq = sb("negq", [128, 8], F32)
    sm = sb("sm", [128, 16], F32)
    lo, mid, cnt, ge, w0, rmax, rsum, rinv, hi = (sm[:, i:i + 1] for i in range(9))
    halo = accb[:, 14336:14912].rearrange("p (g t) -> p g t", g=4)
    halo2 = accb[:, 14912:15488].rearrange("p (g t) -> p g t", g=4)
    tails = accb[:, 12288:14336].rearrange("p (r j n) -> p r j n", r=4, j=NSLOT)
    kmT = sb("kmT", [128, 4, 256], BF16)
    vmt = sb("vmt", [128, 2, 512], BF16)

    for (dst, src, key) in ((qpos[:, :], qpos_d, "qpos"), (bgate[:, :], bgate_d, "bgate"),
                            (pscale[:, :], pscale_d, "pscale"), (wpool_f[:, :, :], wpool_d, "wpool_f")):
        S.dma("sp", dst, src, w=[key])
    for (dst, src, key, n) in ((iota[:, :], iota_d, "iota", 512), (pow2[:, :], pow2_d, "pow2", 32),
                               (slopes[:, :], slopes_d, "slopes", 8), (psel[:, :], psel_d, "psel", 4), (c512[:, :], c512_d, "c512", 8),
                               (invc0[:, :, :].rearrange("p g t -> p (g t)"), invc0_d, "invc0", 512)):
        S.dma("sp", dst, src.broadcast_to([128, n]), w=[key])
    S.add("dve", lambda e: e.tensor_copy(out=wpool_b[:, :, :], in_=wpool_f[:, :, :]), r=["wpool_f"], w=["wpool_b"])

    v3 = lambda t: t[:, :].rearrange("p (c n) -> p c n", c=16)
    v4 = lambda t: t[:, :].rearrange("p (c n) -> p c n", c=4)
    win_v = win_d.rearrange("(c p) n -> p c n", p=128)
    ev_rr = [0]

    def evac(out, in_, r, w, scale=None):
        eng = ("act", "dve")[ev_rr[0] % 2]
        ev_rr[0] += 1
        if eng == "act":
            S.add("act", lambda e: e.copy(out=out, in_=in_), r=r, w=w)
        else:
            S.add("dve", lambda e: e.tensor_copy(out=out, in_=in_), r=r, w=w)

    fb = [0]

    def proj_feat(w_v, col0, ncols, act_T, act_keys, nk, ntok, consume):
        view = lambda t: t[:, 0:nk * ncols].rearrange("p (c n) -> p c n", c=nk)
        wt, wk = load_w(w_v[:, 0:nk, col0:col0 + ncols], view)
        for half in range((ntok + 511) // 512):
            n = min(512, ntok - half * 512)
            ib = fb[0] % 4
            fb[0] += 1
            for c in range(nk):
                S.add("pe", lambda e, c=c, ib=ib, half=half, n=n: e.matmul(
                    out=bank[ib][0:ncols, 0:n], lhsT=wt[:, c, :], rhs=act_T[:, c, half * 512:half * 512 + n],
                    start=(c == 0), stop=(c == nk - 1)), r=[wk] + act_keys, w=[bk(ib)])
            consume(half, ib, n)

    kst = Ring([xbf.tiles[0], xbf.tiles[1]], "xbf")

    def to_sbuf(dst3, ci, key, scale=None):
        def f(half, ib, n):
            if scale is None:
                evac(dst3[:, ci, half * 512:half * 512 + n], bank[ib][:, 0:n], r=[bk(ib)], w=[(key, ci, half)])
            else:
                S.add("act", lambda e: e.mul(out=dst3[:, ci, half * 512:half * 512 + n], in_=bank[ib][:, 0:n], mul=scale),
                      r=[bk(ib)], w=[(key, ci, half)])
        return f

    for ci in range(4):
        proj_feat(win_v, 4176 + ci * 128, 128, hT, hT_all, NDC, T, to_sbuf(uT, ci, "uT"))
    for ci in range(9):
        col0, ncols = (1024 + ci * 128, 128) if ci < 8 else (4096, 64)
        st_t, st_k = kst.next()

        def to_stage(half, ib, n, st_t=st_t, st_k=st_k, ncols=ncols):
            evac(st_t[0:ncols, half * 512:half * 512 + n], bank[ib][0:ncols, 0:n], r=[bk(ib)], w=[(st_k, half)])
        proj_feat(win_v, col0, ncols, hT, hT_all, NDC, T, to_stage)
        kdst = k_loc[ci // 4][(ci % 4) * 128:(ci % 4 + 1) * 128, :] if ci < 8 else ki_loc[:, :]
        S.dma("sp", kdst, st_t[0:ncols, 0:T], r=[(st_k, 0), (st_k, 1)], w=["kv_loc"])
    for hv in range(2):
        for u4 in range(4):
            wt, wk = load_w(win_v[:, u4 * 4:u4 * 4 + 4, 2048 + hv * 512:2048 + (hv + 1) * 512], v4)
            for k4 in range(4):
                c = u4 * 4 + k4
                for j in range(NSLOT):
                    S.add("pe", lambda e, c=c, j=j, wt=wt, k4=k4: e.matmul(
                        out=bank[j][:, :], lhsT=hT[:, c, j * 128:(j + 1) * 128], rhs=wt[:, k4, :],
                        start=(c == 0), stop=(c == NDC - 1)), r=[wk] + hT_all, w=[bk(j)])
        for j in range(NSLOT):
            evac(vtok[:, j, hv * 512:(hv + 1) * 512], bank[j][:, :], r=[bk(j)], w=[("vtok", j, hv)])
    for j in range(NSLOT):
        S.dma("sp", v_loc[j // 4][(j % 4) * 128:(j % 4 + 1) * 128, :], vtok[:, j, :],
              r=[("vtok", j, 0), ("vtok", j, 1)], w=["kv_loc"])
    for j in range(NSLOT):
        S.dma("sp", ut_loc[j * 128:(j + 1) * 128, :].rearrange("p (g t) -> p g t", g=4),
              uT[:, :, j * 128 + 112:(j + 1) * 128], r=[("uT", g, h) for g in range(4) for h in range(2)], w=["ut_loc"])
    groups = [[0, 1, 2, 3], [4, 5, 6, 7]]
    for ci_, (src_, dst_) in enumerate(((k_loc[0], k_all[0]), (k_loc[1], k_all[1]), (v_loc[0], v_all[0]),
                                        (v_loc[1], v_all[1]), (ki_loc, ki_all))):
        S.coll(ci_, lambda e, src_=src_, dst_=dst_: e.collective_compute(
            "AllGather", ALU.bypass, replica_groups=groups, ins=[src_.opt()], outs=[dst_.opt()]),
            r=["kv_loc"], w=["kv_all"])
    S.coll(5, lambda e: e.collective_compute("AllGather", ALU.bypass, replica_groups=groups,
                                             ins=[ut_loc.opt()], outs=[ut_all.opt()]), r=["ut_loc"], w=["ut_all"])
    cast_engs[0] = ("act", "dve")
    for ci in range(8):
        proj_feat(win_v, ci * 128, 128, hT, hT_all, NDC, T, to_sbuf(qT, ci, "qT", scale=128.0 ** -0.5))
    for ci in range(8):
        proj_feat(win_v, 3072 + ci * 128, 128, hT, hT_all, NDC, T, to_sbuf(qiT, ci, "qiT"))
    for ci in range(4):
        proj_feat(win_v, 4688 + ci * 128, 128, hT, hT_all, NDC, T, to_sbuf(qmT, ci, "qmT"))
    vw = lambda t: t[:, 0:256].rearrange("p (c n) -> p c n", c=16)
    wt, wk = load_w(win_v[:, :, 4160:4176], vw)
    for j in range(NSLOT):
        for c in range(NDC):
            S.add("pe", lambda e, c=c, j=j, wt=wt: e.matmul(
                out=bank[j][:, 0:16], lhsT=hT[:, c, j * 128:(j + 1) * 128], rhs=wt[:, c, :],
                start=(c == 0), stop=(c == NDC - 1)), r=[wk] + hT_all, w=[bk(j)])
        S.add("dve", lambda e, j=j: e.tensor_copy(out=wi_t[:, j, :], in_=bank[j][:, 0:16]), r=[bk(j)], w=["wi_t"])
    S.add("act", lambda e: e.activation(out=sgnw[:, :, :], in_=wi_t[:, :, :], func=AF.Sign), r=["wi_t"], w=["sgnw"])
    S.add("dve", lambda e: e.scalar_tensor_tensor(out=absw[:, :, :], in0=wi_t[:, :, :], scalar=0.125 * 0.25, in1=sgnw[:, :, :],
                                                   op0=ALU.mult, op1=ALU.mult), r=["wi_t", "sgnw"], w=["absw"])
    mT_keys = []
    for mbk in range(2):
        xt, xt_key = accb[:, 10240:12288], "memx"
        S.dma("sp", xt, mem_d[mbk * 128:(mbk + 1) * 128, :], w=[xt_key])
        xb, xb_key = kst.next()
        S.add("dve", lambda e, xb=xb, xt=xt: e.tensor_copy(out=xb[:, :], in_=xt), r=[xt_key], w=[xb_key])
        for half in range(2):
            bi = 6 + half
            pt = bank[bi][:, :].bitcast(BF16)
            for c8 in range(8):
                c = half * 8 + c8
                S.add("pe", lambda e, c=c, c8=c8, pt=pt, xb=xb: e.transpose(
                    out=pt[:, c8 * 128:(c8 + 1) * 128], in_=xb[:, c * 128:(c + 1) * 128], identity=identb[:, :]),
                    r=[xb_key, "identb"], w=[bk(bi)])
            S.add("act", lambda e, half=half, mbk=mbk, pt=pt: e.copy(
                out=memT[:, half * 8:(half + 1) * 8, mbk * 128:(mbk + 1) * 128],
                in_=pt.rearrange("p (c n) -> p c n", c=8)), r=[bk(bi)], w=[("memT", mbk, half)])
            mT_keys.append(("memT", mbk, half))
    wmkv_v = wmkv_d.rearrange("(c p) n -> p c n", p=128)
    for ci in range(4):
        def to_km(half, ib, n, ci=ci):
            evac(kmT[:, ci, 0:n], bank[ib][:, 0:n], r=[bk(ib)], w=[("kmT", ci)])
        proj_feat(wmkv_v, ci * 128, 128, memT, mT_keys, NDC, 256, to_km)
    for u4 in range(4):
        wt, wk = load_w(wmkv_v[:, u4 * 4:u4 * 4 + 4, 512:1024], v4)
        for k4 in range(4):
            c = u4 * 4 + k4
            for mbk in range(2):
                S.add("pe", lambda e, c=c, mbk=mbk, wt=wt, k4=k4: e.matmul(
                    out=bank[mbk][:, :], lhsT=memT[:, c, mbk * 128:(mbk + 1) * 128], rhs=wt[:, k4, :],
                    start=(c == 0), stop=(c == NDC - 1)), r=[wk] + mT_keys, w=[bk(mbk)])
    for mbk in range(2):
        evac(vmt[:, mbk, :], bank[mbk][:, :], r=[bk(mbk)], w=[("vmt", mbk)])
    for rr in range(4):
        src = ut_all[rr * 1024:(rr + 1) * 1024, :].rearrange("(j p) n -> p j n", p=128)
        S.dma("sp", tails[:, rr, :, :], src, r=["ut_all"], w=["tails"])
    S.flush()
    cast_engs[0] = ("act", "dve", "pool")
    if stage == 20:
        es.close()
        return nc
    ak = wst.tiles[2][0:4, :].bitcast(BF16)
    aq = wbf.tiles[0][0:4, 0:1024]
    logit_ring = Ring([accb[:, 4096:8192], accb[:, 8192:12288]], "logit")
    aqs_ring = Ring([sb("aqs%d" % i, [4, 128], BF16) for i in range(2)], "aqs")
    S.dma("sp", ak, ak_d, w=["ak"])
    S.dma("sp", aq, aq_d, w=["aq"])
    for hp in range(2):
        for rr in range(4):
            dst = kiT[hp * 64:(hp + 1) * 64, :].rearrange("p (c r i) -> p c r i", c=8, r=4)[:, :, rr, :]
            src = ki_all[rr * 64:(rr + 1) * 64, :].rearrange("p (c i) -> p c i", c=8)
            S.dma("sp", dst, src, r=["kv_all"], w=["kiT"])

    u_keys = [("uT", g, h) for g in range(4) for h in range(2)]
    WIN = (2, 4, 8, 16)
    sC = sa_ring.tiles[0][:, 0:432].rearrange("p (g t) -> p g t", g=3)
    for j in range(NSLOT):
        for i in range(4):
            if i == 3 and j == 0:
                continue
            cand = (tails[:, i, j, :] if i < 3 else tails[:, 3, j - 1, :]).rearrange("p (g t) -> p g t", g=4)
            if i == 0:
                S.add("dve", lambda e, cand=cand: e.tensor_scalar(
                    out=halo[:, :, 0:16], in0=cand, scalar1=psel[:, 0:1], scalar2=None, op0=ALU.mult),
                    r=["tails", "psel"], w=["halo"])
            else:
                S.add("dve", lambda e, cand=cand, i=i: e.scalar_tensor_tensor(
                    out=halo[:, :, 0:16], in0=cand, scalar=psel[:, i:i + 1], in1=halo[:, :, 0:16],
                    op0=ALU.mult, op1=ALU.add), r=["tails", "psel", "halo"], w=["halo"])
        S.add("dve", lambda e, j=j: e.tensor_copy(out=halo[:, :, 16:144], in_=uT[:, :, j * 128:(j + 1) * 128]),
              r=u_keys + ["halo"], w=["halo"])
        S.add("dve", lambda e: e.tensor_tensor(out=halo2[:, :, 1:144], in0=halo[:, :, 1:144], in1=halo[:, :, 0:143],
                                                op=ALU.add), r=["halo"], w=["halo2"])
        pb, pb_key = kst.next()
        pooled = pb[:, 0:512].rearrange("p (g t) -> p g t", g=4)

        def emit_pooled(g, src, src_key, j=j, pooled=pooled, pb_key=pb_key):
            if j == 0:
                S.add("dve", lambda e: e.tensor_tensor(out=src, in0=src, in1=invc0[:, g, :], op=ALU.mult),
                      r=[src_key, "invc0"], w=[src_key])
                S.add("dve", lambda e: e.tensor_tensor(out=pooled[:, g, :], in0=src, in1=halo[:, g, 16:144],
                                                        op=ALU.subtract), r=[src_key, "halo"], w=[(pb_key, g)])
            else:
                S.add("dve", lambda e: e.scalar_tensor_tensor(
                    out=pooled[:, g, :], in0=src, scalar=1.0 / WIN[g], in1=halo[:, g, 16:144],
                    op0=ALU.mult, op1=ALU.subtract), r=[src_key, "halo"], w=[(pb_key, g)])

        S.add("dve", lambda e: e.tensor_tensor(out=sC[:, :, 3:144], in0=halo2[:, 1:4, 3:144], in1=halo2[:, 1:4, 1:142],
                                                op=ALU.add), r=["halo2"], w=["sC"])
        emit_pooled(0, halo2[:, 0, 16:144], "halo2")
        S.add("dve", lambda e: e.tensor_tensor(out=halo2[:, 2:4, 7:144], in0=sC[:, 1:3, 7:144], in1=sC[:, 1:3, 3:140],
                                                op=ALU.add), r=["sC", "halo2"], w=["halo2"])
        emit_pooled(1, sC[:, 0, 16:144], "sC")
        S.add("dve", lambda e: e.tensor_tensor(out=sC[:, 2, 15:144], in0=halo2[:, 3, 15:144], in1=halo2[:, 3, 7:136],
                                                op=ALU.add), r=["halo2", "sC"], w=["sC"])
        emit_pooled(2, halo2[:, 2, 16:144], "halo2")
        emit_pooled(3, sC[:, 2, 16:144], "sC")
        for g in range(4):
            ib = fb[0] % 4
            fb[0] += 1
            S.add("pe", lambda e, g=g, ib=ib, pooled=pooled: e.matmul(
                out=bank[ib][:, 0:128], lhsT=wpool_b[:, g, :], rhs=pooled[:, g, :], start=True, stop=True),
                r=["wpool_b", (pb_key, g)], w=[bk(ib)])
            S.add("act", lambda e, g=g, ib=ib, j=j: e.activation(
                out=pT[:, g, j * 128:(j + 1) * 128], in_=bank[ib][:, 0:128], func=AF.Copy, scale=pscale[:, g:g + 1]),
                r=[bk(ib), "pscale"], w=[("pT", j)])
    S.flush()

    NIT = 20
    ATT_SCALE = 128.0 ** -0.5
    for j in range(NSLOT):
        nk = 512 * (j + 1)
        nb = 4 * (j + 1)
        S.add("dve", lambda e, j=j: e.tensor_scalar(out=negq[:, :], in0=c512[:, :], scalar1=qpos[:, j:j + 1],
                                                     scalar2=None, op0=ALU.subtract), r=["c512", "qpos"], w=["negq"])
        for c in range(j + 1):
            for h in range(16 if not TEST_SKIP else 1):
                ib = fb[0] % 4
                fb[0] += 1
                p0 = (h % 2) * 64
                S.add("pe", lambda e, c=c, h=h, ib=ib, p0=p0, j=j: e.matmul(
                    out=bank[ib][:, :], lhsT=qiT[p0:p0 + 64, h // 2, j * 128:(j + 1) * 128],
                    rhs=kiT[p0:p0 + 64, c * 512:(c + 1) * 512], start=True, stop=True),
                    r=[("qiT", h // 2, 0), ("qiT", h // 2, 1), "kiT"], w=[bk(ib)])
                rt, rt_key = sa_ring.next()
                S.add("act", lambda e, rt=rt, ib=ib, h=h, j=j: e.activation(
                    out=rt[:, :], in_=bank[ib][:, :], func=AF.Relu, scale=absw[:, j, h:h + 1]),
                    r=[bk(ib), "absw"], w=[rt_key])
                sc = score[:, c * 512:(c + 1) * 512]
                if h == 0:
                    S.add("dve", lambda e, rt=rt, sc=sc, h=h, j=j: e.tensor_scalar(
                        out=sc, in0=rt[:, :], scalar1=sgnw[:, j, h:h + 1], scalar2=None, op0=ALU.mult),
                        r=[rt_key, "sgnw"], w=[("score", c)])
                else:
                    S.add("dve", lambda e, rt=rt, sc=sc, h=h, j=j: e.scalar_tensor_tensor(
                        out=sc, in0=rt[:, :], scalar=sgnw[:, j, h:h + 1], in1=sc, op0=ALU.mult, op1=ALU.add),
                        r=[rt_key, "sgnw", ("score", c)], w=[("score", c)])
        sc_keys = [("score", c) for c in range(j + 1)]
        S.add("dve", lambda e, nk=nk: e.tensor_reduce(out=hi, in_=score[:, 0:nk], axis=mybir.AxisListType.X, op=ALU.max),
              r=sc_keys, w=["hi"])
        S.add("dve", lambda e, nk=nk: e.tensor_reduce(out=lo, in_=score[:, 0:nk], axis=mybir.AxisListType.X, op=ALU.min),
              r=sc_keys, w=["lo"])
        S.add("dve", lambda e: e.tensor_scalar(out=lo, in0=lo, scalar1=-1.0, scalar2=None, op0=ALU.add), r=["lo"], w=["lo"])
        S.add("dve", lambda e: e.scalar_tensor_tensor(out=w0, in0=hi, scalar=1.0, in1=lo, op0=ALU.add, op1=ALU.subtract),
              r=["hi", "lo"], w=["w0"])
        S.add("dve", lambda e: e.tensor_scalar(out=steps[:, :], in0=pow2[:, :], scalar1=w0, scalar2=None, op0=ALU.mult),
              r=["pow2", "w0"], w=["steps"])
        cm, cm_key = sa_ring.next()
        lastc = score[:, j * 512:(j + 1) * 512]
        S.add("dve", lambda e, cm=cm, j=j: e.tensor_scalar(out=cm[:, :], in0=iota[:, :], scalar1=negq[:, j:j + 1], scalar2=0.0,
                                                            op0=ALU.add, op1=ALU.is_le), r=["iota", "negq"], w=[cm_key])
        S.add("dve", lambda e, cm=cm, lastc=lastc: e.tensor_tensor(out=lastc, in0=lastc, in1=cm[:, :], op=ALU.mult),
              r=[cm_key, ("score", j)], w=[("score", j)])
        S.add("dve", lambda e, cm=cm: e.tensor_scalar(out=cm[:, :], in0=cm[:, :], scalar1=1e30, scalar2=-1e30,
                                                       op0=ALU.mult, op1=ALU.add), r=[cm_key], w=[cm_key])
        S.add("dve", lambda e, cm=cm, lastc=lastc: e.tensor_tensor(out=lastc, in0=lastc, in1=cm[:, :], op=ALU.add),
              r=[cm_key, ("score", j)], w=[("score", j)])
        for it in range(NIT if not TEST_SKIP else 1):
            S.add("dve", lambda e, it=it: e.tensor_tensor(out=mid, in0=lo, in1=steps[:, it:it + 1], op=ALU.add),
                  r=["lo", "steps"], w=["mid"])
            S.add("dve", lambda e, nk=nk: e.tensor_scalar(out=pj[:, 0:nk], in0=score[:, 0:nk], scalar1=mid, scalar2=None,
                                                           op0=ALU.is_ge, op1=ALU.add, accum_out=cnt),
                  r=sc_keys + ["mid"], w=["pj", "cnt"])
            S.add("dve", lambda e: e.tensor_scalar(out=ge, in0=cnt, scalar1=255.5, scalar2=None, op0=ALU.is_ge),
                  r=["cnt"], w=["ge"])
            S.add("dve", lambda e, it=it: e.scalar_tensor_tensor(out=lo, in0=ge, scalar=steps[:, it:it + 1], in1=lo,
                                                                  op0=ALU.mult, op1=ALU.add), r=["ge", "steps", "lo"], w=["lo"])
        S.add("dve", lambda e, nk=nk: e.tensor_scalar(out=mb[:, 0:nk], in0=score[:, 0:nk], scalar1=lo, scalar2=None,
                                                       op0=ALU.is_ge), r=sc_keys + ["lo"], w=["mb"])
        S.add("dve", lambda e, nk=nk: e.tensor_scalar(out=mb[:, 0:nk], in0=mb[:, 0:nk], scalar1=30000.0, scalar2=-30000.0,
                                                       op0=ALU.mult, op1=ALU.add), r=["mb"], w=["mb"])
        atok, atok_key = kst.next()
        for h in range(8):
            kh, kh_key = kh_ring.next()
            rmax_h, rsum_h, rinv_h = sm[:, 5 + 4 * (h % 2) + 0:5 + 4 * (h % 2) + 1] if False else (sm[:, (5, 9)[h % 2]:(5, 9)[h % 2] + 1]), sm[:, (6, 10)[h % 2]:(6, 10)[h % 2] + 1], sm[:, (7, 11)[h % 2]:(7, 11)[h % 2] + 1]
            kx = h % 2
            vh, vh_key = vh_ring.next()
            for rr in range(4):
                ksrc = k_all[h // 4][rr * 512 + (h % 4) * 128:rr * 512 + (h % 4 + 1) * 128, 0:128 * (j + 1)]
                S.dma("sp", kh[:, 0:nk].rearrange("p (c r i) -> p c r i", c=j + 1, r=4)[:, :, rr, :],
                      ksrc.rearrange("p (c i) -> p c i", c=j + 1), r=[], w=[kh_key])
                for vp in range(2):
                    ncp = min(4, j + 1 - 4 * vp)
                    if ncp <= 0:
                        continue
                    vsrc = v_all[vp][rr * 512:rr * 512 + 128 * ncp, h * 128:(h + 1) * 128]
                    S.dma("sp", vh[:, 16 * vp:16 * vp + 4 * ncp, :].rearrange("p (c r) d -> p c r d", r=4)[:, :, rr, :],
                          vsrc.rearrange("(c p) d -> p c d", p=128), r=[], w=[vh_key])
            aqs, aqs_key = aqs_ring.next()
            lg, lg_key = logit_ring.next()
            S.add("pool", lambda e, aqs=aqs, h=h, j=j: e.tensor_scalar(
                out=aqs[:, :], in0=aq[:, j * 128:(j + 1) * 128], scalar1=2.0 ** -(h + 1), scalar2=None, op0=ALU.mult),
                r=["aq"], w=[aqs_key])
            for c in range(j + 1):
                ib = fb[0] % 4
                fb[0] += 1
                cs = slice(c * 512, (c + 1) * 512)
                S.add("pe", lambda e, cs=cs, h=h, ib=ib, kh=kh, j=j: e.matmul(
                    out=bank[ib][:, :], lhsT=qT[:, h, j * 128:(j + 1) * 128], rhs=kh[:, cs],
                    start=True, stop=False), r=[("qT", h, 0), ("qT", h, 1), kh_key], w=[bk(ib)])
                S.add("pe", lambda e, cs=cs, ib=ib, aqs=aqs: e.matmul(
                    out=bank[ib][:, :], lhsT=aqs[:, :], rhs=ak[:, cs], start=False, stop=False),
                    r=[aqs_key, "ak"], w=[bk(ib)])
                S.add("pe", lambda e, cs=cs, ib=ib: e.matmul(
                    out=bank[ib][:, :], lhsT=identb[:, :], rhs=mb[:, cs], start=False, stop=True),
                    r=["identb", "mb"], w=[bk(ib)])
                S.add("act", lambda e, ib=ib, cs=cs, lg=lg: e.copy(out=lg[:, cs], in_=bank[ib][:, :]),
                      r=[bk(ib)], w=[lg_key])
            S.add("dve", lambda e, nk=nk, lg=lg, rmax_h=rmax_h: e.tensor_reduce(out=rmax_h, in_=lg[:, 0:nk], axis=mybir.AxisListType.X,
                                                                  op=ALU.max, negate=True), r=[lg_key], w=[("rmax", kx)])
            S.add("act", lambda e, nk=nk, lg=lg, rmax_h=rmax_h, rsum_h=rsum_h: e.activation(out=pj[:, 0:nk], in_=lg[:, 0:nk], func=AF.Exp, bias=rmax_h,
                                                               accum_out=rsum_h), r=[lg_key, ("rmax", kx)], w=["pj", ("rsum", kx)])
            S.add("dve", lambda e, rinv_h=rinv_h, rsum_h=rsum_h: e.reciprocal(out=rinv_h, in_=rsum_h), r=[("rsum", kx)], w=[("rinv", kx)])
            for b8 in range((nb + 7) // 8):
                bi = 4 + (tr_rr[0] % 2)
                tr_rr[0] += 1
                n8 = min(8, nb - b8 * 8)
                pt = bank[bi][:, :].bitcast(BF16)
                for k in range(n8):
                    blk = b8 * 8 + k
                    S.add("pe", lambda e, k=k, blk=blk, pt=pt: e.transpose(
                        out=pt[:, k * 128:(k + 1) * 128], in_=pj[:, blk * 128:(blk + 1) * 128], identity=identb[:, :]),
                        r=["pj", "identb"], w=[bk(bi)])
                S.add("act", lambda e, b8=b8, n8=n8, pt=pt: e.copy(
                    out=PT[:, b8 * 8:b8 * 8 + n8, :], in_=pt[:, 0:n8 * 128].rearrange("p (b t) -> p b t", b=n8)),
                    r=[bk(bi)], w=[("PT", b8)])
            ib = 6 + (h % 2)
            pt_keys = [("PT", b8) for b8 in range((nb + 7) // 8)]
            for blk in range(nb):
                S.add("pe", lambda e, blk=blk, ib=ib, vh=vh, nb=nb: e.matmul(
                    out=bank[ib][:, 0:128], lhsT=PT[:, blk, :], rhs=vh[:, blk, :], start=(blk == 0), stop=(blk == nb - 1)),
                    r=pt_keys + [vh_key], w=[bk(ib)])
            S.add("act", lambda e, ib=ib, h=h, atok=atok, rinv_h=rinv_h: e.activation(
                out=atok[:, h * 128:(h + 1) * 128], in_=bank[ib][:, 0:128], func=AF.Copy, scale=rinv_h),
                r=[bk(ib), ("rinv", kx)], w=[(atok_key, h)])
        bi = 4 + (tr_rr[0] % 2)
        tr_rr[0] += 1
        pt = bank[bi][:, :].bitcast(BF16)
        for h in range(8):
            S.add("pe", lambda e, h=h, pt=pt, atok=atok: e.transpose(
                out=pt[:, h * 128:(h + 1) * 128], in_=atok[:, h * 128:(h + 1) * 128], identity=identb[:, :]),
                r=[(atok_key, h), "identb"], w=[bk(bi)])
        S.add("act", lambda e, pt=pt, j=j: e.copy(out=qT[:, :, j * 128:(j + 1) * 128],
                                                   in_=pt.rearrange("p (h t) -> p h t", h=8)),
              r=[bk(bi)], w=[("qT", h, j // 4) for h in range(8)])
        mtok, mtok_key = kst.next()
        for h in range(4):
            ib = fb[0] % 4
            fb[0] += 1
            S.add("pe", lambda e, h=h, ib=ib, j=j: e.matmul(
                out=bank[ib][:, 0:256], lhsT=qmT[:, h, j * 128:(j + 1) * 128], rhs=kmT[:, h, :], start=True, stop=True),
                r=[("qmT", h, 0), ("qmT", h, 1), ("kmT", h)], w=[bk(ib)])
            S.add("dve", lambda e, ib=ib: e.tensor_reduce(out=rmax, in_=bank[ib][:, 0:256], axis=mybir.AxisListType.X,
                                                           op=ALU.max), r=[bk(ib)], w=["rmax"])
            S.add("dve", lambda e: e.tensor_scalar(out=rmax, in0=rmax, scalar1=-ATT_SCALE, scalar2=None, op0=ALU.mult),
                  r=["rmax"], w=["rmax"])
            S.add("act", lambda e, ib=ib: e.activation(out=pj[:, 0:256], in_=bank[ib][:, 0:256], func=AF.Exp, bias=rmax,
                                                        scale=ATT_SCALE, accum_out=rsum), r=[bk(ib), "rmax"], w=["pj", "rsum"])
            S.add("dve", lambda e: e.reciprocal(out=rinv, in_=rsum), r=["rsum"], w=["rinv"])
            bi = 4 + (tr_rr[0] % 2)
            tr_rr[0] += 1
            pt = bank[bi][:, :].bitcast(BF16)
            for k in range(2):
                S.add("pe", lambda e, k=k, pt=pt: e.transpose(
                    out=pt[:, k * 128:(k + 1) * 128], in_=pj[:, k * 128:(k + 1) * 128], identity=identb[:, :]),
                    r=["pj", "identb"], w=[bk(bi)])
            evac(PT[:, 0:2, :], pt[:, 0:256].rearrange("p (b t) -> p b t", b=2), r=[bk(bi)], w=[("PT", 0)])
            ib2 = 6 + (h % 2)
            for k in range(2):
                S.add("pe", lambda e, k=k, ib2=ib2, h=h: e.matmul(
                    out=bank[ib2][:, 0:128], lhsT=PT[:, k, :], rhs=vmt[:, k, h * 128:(h + 1) * 128],
                    start=(k == 0), stop=(k == 1)), r=[("PT", 0), ("vmt", 0), ("vmt", 1)], w=[bk(ib2)])
            S.add("act", lambda e, ib2=ib2, h=h, mtok=mtok: e.activation(
                out=mtok[:, h * 128:(h + 1) * 128], in_=bank[ib2][:, 0:128], func=AF.Copy, scale=rinv),
                r=[bk(ib2), "rinv"], w=[(mtok_key, h)])
        bi = 4 + (tr_rr[0] % 2)
        tr_rr[0] += 1
        pt = bank[bi][:, :].bitcast(BF16)
        for h in range(4):
            S.add("pe", lambda e, h=h, pt=pt, mtok=mtok: e.transpose(
                out=pt[:, h * 128:(h + 1) * 128], in_=mtok[:, h * 128:(h + 1) * 128], identity=identb[:, :]),
                r=[(mtok_key, h), "identb"], w=[bk(bi)])
        S.add("act", lambda e, pt=pt, j=j: e.copy(out=qmT[:, :, j * 128:(j + 1) * 128],
                                                   in_=pt[:, 0:512].rearrange("p (h t) -> p h t", h=4)),
              r=[bk(bi)], w=[("qmT", h, j // 4) for h in range(4)])
    S.flush()

    if stage == 3:
        for c in range(16):
            src = qT[:, c, :] if c < 8 else (pT[:, c - 8, :] if c < 12 else qmT[:, c - 12, :])
            st, st_key = wst.next()
            S.add("dve", lambda e, st=st, src=src: e.tensor_copy(out=st[:, 0:1024], in_=src), w=[st_key])
            S.dma("sp", out_d[c * 64:(c + 1) * 64, :].rearrange("r (pl t) -> (r pl) t", pl=2), st[:, 0:1024], r=[st_key])
        S.flush()
        es.close()
        return nc
    for j in range(NSLOT):
        xt, xt_key = accb[:, 0:2048] if False else (wst.tiles[j % 2][:, :], ("wst", j % 2))
        S.dma("sp", xt, h1_scr[j // 4][(j % 4) * 128:(j % 4 + 1) * 128, :], w=[xt_key])
        S.add("act", lambda e, j=j, xt=xt: e.mul(out=acc[:, j, :], in_=xt, mul=ALPHA), r=[xt_key], w=[("acc", j)])
        to_hT(j, xt, xt_key)
    S.flush()
    yT = qiT
    wbf_cur[0] = Ring(wbf.tiles + [regG[:, 12 * T:14 * T], regG[:, 14 * T:16 * T]], "wbf")
    sg = [xbf.tiles[0][:, 0:1024].bitcast(F32), xbf.tiles[0][:, 1024:2048].bitcast(F32),
          xbf.tiles[1][:, 0:1024].bitcast(F32), xbf.tiles[1][:, 1024:2048].bitcast(F32)]
    wba_v = wba_d.rearrange("(c p) n -> p c n", p=128)
    wbp_v = wbp_d.rearrange("(c p) n -> p c n", p=128)
    wbm_v = wbm_d.rearrange("(c p) n -> p c n", p=128)
    wout_v = wout_d.rearrange("(c p) n -> p c n", p=128)
    for G in range(4):
        for n4 in range(4):
            n = G * 4 + n4
            wts = []
            for i in range(3):
                col0 = 5200 + i * D + n * 128
                wts.append(load_w(win_v[:, :, col0:col0 + 128], v3))
            st, st_key = wst.next()
            bf, bf_key = wbf_cur[0].next()
            cols = slice(n * 128, (n + 1) * 128)
            vv = lambda t, a, b, c: t[:, a:b].rearrange("p (c n) -> p c n", c=c)
            S.dma("sp", vv(st, 0, 1024, 8), wba_v[:, :, cols], w=[st_key])
            S.dma("sp", vv(st, 1024, 1536, 4), wbp_v[:, :, cols], w=[st_key])
            S.dma("sp", vv(st, 1536, 2048, 4), wbm_v[:, :, cols], w=[st_key])
            cast(bf[:, :], st[:, :], r=[st_key], w=[bf_key])
            wb = [(vv(bf, 0, 1024, 8), bf_key), (vv(bf, 1024, 1536, 4), bf_key), (vv(bf, 1536, 2048, 4), bf_key)]
            brs = [(qT, 8), (pT, 4), (qmT, 4)]
            for half in range(2):
                ts = slice(half * 512, (half + 1) * 512)
                for i in range(3):
                    wt, wk = wts[i]
                    for c in range(NDC):
                        S.add("pe", lambda e, wt=wt, c=c, i=i, ts=ts: e.matmul(
                            out=bank[i][:, :], lhsT=wt[:, c, :], rhs=hT[:, c, ts], start=(c == 0), stop=(c == NDC - 1)),
                            r=[wk] + hT_all, w=[bk(i)])
                    S.add("act", lambda e, i=i, n=n: e.activation(
                        out=sg[i], in_=bank[i][:, :], func=AF.Sigmoid, bias=bgate[:, i * 16 + n:i * 16 + n + 1]),
                        r=[bk(i), "bgate"], w=[("sg", i)])
                for i in range(3):
                    wt, wk = wb[i]
                    src, nkc = brs[i]
                    for c in range(nkc):
                        S.add("pe", lambda e, wt=wt, c=c, i=i, ts=ts, src=src, nkc=nkc: e.matmul(
                            out=bank[3 + i][:, :], lhsT=wt[:, c, :], rhs=src[:, c, ts], start=(c == 0), stop=(c == nkc - 1)),
                            r=[wk], w=[bk(3 + i)])
                    S.add("dve", lambda e, i=i: e.tensor_tensor(out=sg[i], in0=sg[i], in1=bank[3 + i][:, :], op=ALU.mult),
                          r=[("sg", i), bk(3 + i)], w=[("sg", i)])
                S.add("dve", lambda e: e.tensor_tensor(out=sg[0], in0=sg[0], in1=sg[1], op=ALU.add),
                      r=[("sg", 0), ("sg", 1)], w=[("sg", 0)])
                S.add("dve", lambda e, n4=n4, ts=ts: e.tensor_tensor(out=yT[:, n4, ts], in0=sg[0], in1=sg[2], op=ALU.add),
                      r=[("sg", 0), ("sg", 2)], w=[("yT", n4, half)])
        y_all = [("yT", n4, h) for n4 in range(4) for h in range(2)]
        for dq in range(4):
            wt, wk = load_w(wout_v[:, G * 4:G * 4 + 4, dq * 512:(dq + 1) * 512], v4)
            for k4 in range(4):
                for j in range(NSLOT):
                    S.add("pe", lambda e, k4=k4, j=j, wt=wt: e.matmul(
                        out=bank[j][:, :], lhsT=yT[:, k4, j * 128:(j + 1) * 128], rhs=wt[:, k4, :],
                        start=(k4 == 0), stop=(k4 == 3)), r=[wk] + y_all, w=[bk(j)])
            for j in range(NSLOT):
                dst = acc[:, j, dq * 512:(dq + 1) * 512]
                S.add("dve", lambda e, dst=dst, j=j: e.tensor_tensor(out=dst, in0=dst, in1=bank[j][:, :], op=ALU.add),
                      r=[bk(j), ("acc", j)], w=[("acc", j)])
    S.flush()
    wbf_cur[0] = wbf
    if stage == 4:
        layernorm(ln2g_d, ln2b_d, store_d=out_d, make_hT=True, post_scale=ALPHA)
        es.close()
        return nc
    layernorm(ln2g_d, ln2b_d, store_d=None, make_hT=True, post_scale=ALPHA)

    ffn(w2u_d, w2d_d)
    layernorm(ln3g_d, ln3b_d, store_d=out_d, make_hT=False)

    es.close()
    return nc


def _prep_inputs(inputs):
    f = lambda k: np.asarray(inputs[k], dtype=np.float32)
    x = f("x")
    mem = f("mem")
    shared = {"ident": np.eye(128, dtype=np.float32)}
    for k in ("w_ffn1_up", "w_ffn1_down", "w_in", "w_mem_kv", "w_br_att", "w_br_pool", "w_br_mem", "w_out",
              "w_ffn2_up", "w_ffn2_down"):
        shared[k] = np.ascontiguousarray(f(k)[0])
    for k in ("ln1_g", "ln1_b", "ln2_g", "ln2_b", "ln3_g", "ln3_b"):
        shared[k] = np.ascontiguousarray(f(k).reshape(1, D))
    shared["b_gate"] = np.ascontiguousarray(f("b_gate").reshape(48, 128).T)
    shared["w_pool"] = np.ascontiguousarray(f("w_pool")[0].transpose(1, 0, 2))
    shared["pool_scale"] = np.ascontiguousarray(f("pool_scale").reshape(4, 128).T)
    shared["iota512"] = np.arange(512, dtype=np.float32).reshape(1, 512)
    shared["c512"] = (512.0 * np.arange(8, dtype=np.float32)).reshape(1, 8)
    shared["pow2"] = (2.0 ** -(np.arange(32, dtype=np.float64) + 1)).astype(np.float32).reshape(1, 32)
    shared["slopes"] = (2.0 ** -(np.arange(8, dtype=np.float64) + 1)).astype(np.float32).reshape(1, 8)
    wins = np.array([2, 4, 8, 16], dtype=np.float32)
    import ml_dtypes
    kp = np.arange(SEQ)
    shared["ak"] = np.stack([kp // 64, kp % 64, np.ones(SEQ), np.ones(SEQ)]).astype(np.float32).astype(ml_dtypes.bfloat16)
    maps = []
    for c in range(NCORE):
        b, r = divmod(c, 4)
        m = dict(shared)
        m["x"] = np.ascontiguousarray(x[b].reshape(32, 128, D)[r::4].reshape(T, D))
        m["mem"] = np.ascontiguousarray(mem[b])
        p = np.arange(128, dtype=np.float32)[:, None]
        j = np.arange(NSLOT, dtype=np.float32)[None, :]
        m["qpos"] = np.ascontiguousarray((4 * j + r) * 128 + p).astype(np.float32)
        t = np.arange(128, dtype=np.float32)[None, :]
        if r == 0:
            invc = 1.0 / np.minimum(t + 1.0, wins[:, None])
        else:
            invc = np.broadcast_to(1.0 / wins[:, None], (4, 128))
        m["invc0"] = np.ascontiguousarray(invc, dtype=np.float32).reshape(1, 512)
        sel = np.zeros((1, 4), dtype=np.float32)
        sel[0, (r - 1) % 4] = 1.0
        m["psel"] = sel
        qp = ((4 * np.arange(NSLOT)[:, None] + r) * 128 + np.arange(128)[None, :]).reshape(-1)
        m["aq"] = np.stack([np.full(T, 64.0), np.ones(T), -64.0 * (qp // 64), -1.0 * (qp % 64)]).astype(np.float32).astype(ml_dtypes.bfloat16)
        maps.append(m)
    return maps


def _assemble(results):
    out = np.zeros((2, SEQ, D), dtype=np.float32)
    for c in range(NCORE):
        b, r = divmod(c, 4)
        o = np.asarray(results[c]["out"]).reshape(NSLOT, 128, D)
        out[b].reshape(32, 128, D)[r::4] = o
    return out


def kernel(**inputs):
    nc = build_program(stage=DEBUG_STAGE)
    maps = _prep_inputs(inputs)
    res = run_bass_kernel_spmd(nc, maps, core_ids=list(range(NCORE)))
    return _assemble(res.results)
```

```python
import numpy as np
import concourse.bass as bass
import concourse.mybir as mybir
from concourse.bass_utils import run_bass_kernel_spmd

F32 = mybir.dt.float32
BF16 = mybir.dt.bfloat16
AF = mybir.ActivationFunctionType
ALU = mybir.AluOpType

D = 2048
SEQ = 4096
NCORE = 8
T = 1024
NSLOT = 8
DFF = 5632
NFC = DFF // 128
NDC = D // 128
ALPHA = 2.0 ** 0.25
LN_EPS = 1e-5
DIN = 11344

DEBUG_STAGE = None
TEST_SKIP = False


class Sched:
    COMPUTE = ("pe", "act", "dve", "pool")
    ALL = ("pe", "act", "dve", "pool", "sp")

    def __init__(self, nc, sems, dsems):
        self.nc = nc
        self.sem = sems
        self.dsem = dsems
        self.cnt = {e: 0 for e in self.COMPUTE}
        self.ops = {e: [] for e in self.ALL}
        self.waited = {e: {} for e in self.ALL}
        self.lastw = {}
        self.readers = {}
        self.dma_k = 0
        self.dma_val = [0] * len(dsems)
        self.dma_owner = [None] * len(dsems)
        self.nops = 0

    def _deps(self, eng, r, w):
        raw = {}
        oth = {}

        def put(d, tok):
            k, v = tok
            if d.get(k, 0) < v:
                d[k] = v

        for res in r:
            if res in self.lastw:
                put(raw, self.lastw[res])
        for res in w:
            if res in self.lastw:
                put(oth, self.lastw[res])
            for k, v in self.readers.get(res, {}).items():
                put(oth, (k, v))
        waits = {}
        for k, v in raw.items():
            if k == eng and eng == "pe":
                continue
            put(waits, (k, v))
        for k, v in oth.items():
            if k == eng:
                continue
            put(waits, (k, v))
        out = []
        wd = self.waited[eng]
        for k, v in waits.items():
            if wd.get(k, 0) >= v:
                continue
            wd[k] = v
            out.append((k, v))
        return out

    def _commit(self, tok, r, w):
        for res in w:
            self.lastw[res] = tok
            self.readers[res] = {}
        for res in r:
            d = self.readers.setdefault(res, {})
            if d.get(tok[0], 0) < tok[1]:
                d[tok[0]] = tok[1]

    def add(self, eng, fn, r=(), w=()):
        waits = self._deps(eng, r, w)
        self.cnt[eng] += 1
        tok = (eng, self.cnt[eng])
        self.ops[eng].append((waits, fn, tok, "c"))
        self._commit(tok, r, w)
        self.nops += 1
        return tok

    def dma(self, q, out, in_, r=(), w=(), **kw):
        waits = self._deps(q, r, w)
        i = self.dma_k % len(self.dsem)
        self.dma_k += 1
        prev = self.dma_val[i]
        key = ("d", i)
        if prev > 0 and self.waited[q].get(key, 0) < prev:
            self.waited[q][key] = prev
            waits.append((key, prev))
        self.dma_val[i] = prev + 16
        self.dma_owner[i] = q
        tok = (key, prev + 16)
        self.ops[q].append((waits, lambda e: e.dma_start(out=out, in_=in_, **kw), tok, "d"))
        self._commit(tok, r, w)
        self.nops += 1
        return tok

    def coll(self, i, fn, r=(), w=()):
        waits = self._deps("pool", r, w)
        tok = (("c", i), 1)
        self.cc_pending = getattr(self, "cc_pending", []) + [tok]
        self.ops["pool"].append((waits, fn, tok, "cc"))
        self._commit(tok, r, w)
        return tok

    def _semobj(self, key):
        if isinstance(key, tuple):
            if key[0] == "c":
                return self.csem[key[1]]
            return self.dsem[key[1]]
        return self.sem[key]

    def flush(self):
        nc = self.nc
        with nc.Block() as block:
            decos = {"sp": block.sync, "act": block.scalar, "dve": block.vector,
                     "pool": block.gpsimd, "pe": block.tensor}
            for eng in self.ALL:
                ops = self.ops[eng]
                tail = [(("d", i), self.dma_val[i]) for i in range(len(self.dsem))
                        if self.dma_owner[i] == eng and self.dma_val[i] > 0]
                if eng == "pool":
                    tail = tail + list(getattr(self, "cc_pending", []))

                def body(e, ops=ops, eng=eng, tail=tail):
                    for waits, fn, tok, kind in ops:
                        for k, v in waits[1:]:
                            e.wait_ge(self._semobj(k), v)
                        ins = fn(e)
                        if waits:
                            ins.wait_op(self._semobj(waits[0][0]), waits[0][1], "sem-ge")
                        if kind == "c":
                            ins.then_inc(self.sem[eng], 1)
                        elif kind == "cc":
                            ins.then_inc(self.csem[tok[0][1]])
                        else:
                            ins.then_inc(self.dsem[tok[0][1]], 16)
                    for k, v in tail:
                        e.wait_ge(self._semobj(k), v)

                decos[eng](body)
        for eng in self.ALL:
            self.ops[eng] = []
            for i in range(len(self.dsem)):
                if self.dma_owner[i] is not None:
                    self.waited[eng][("d", i)] = self.dma_val[i]
            for c in self.COMPUTE:
                self.waited[eng][c] = self.cnt[c]
        self.lastw = {}
        self.readers = {}
        self.cc_pending = []


class Ring:
    def __init__(self, tiles, name):
        self.tiles = tiles
        self.name = name
        self.i = 0

    def next(self):
        k = self.i % len(self.tiles)
        self.i += 1
        return self.tiles[k], (self.name, k)


def build_program(stage=None):
    from contextlib import ExitStack
    nc = bass.Bass("TRN2", target_bir_lowering=False)

    def din(name, shape, dt=F32):
        return nc.dram_tensor(name, list(shape), dt, kind="ExternalInput").ap()

    x_d = din("x", [T, D])
    w1u_d = din("w_ffn1_up", [D, 2 * DFF])
    w1d_d = din("w_ffn1_down", [DFF, D])
    ln1g_d = din("ln1_g", [1, D])
    ln1b_d = din("ln1_b", [1, D])
    ident_d = din("ident", [128, 128])
    win_d = din("w_in", [D, DIN])
    bgate_d = din("b_gate", [128, 48])
    wmkv_d = din("w_mem_kv", [D, 1024])
    mem_d = din("mem", [256, D])
    wpool_d = din("w_pool", [128, 4, 128])
    pscale_d = din("pool_scale", [128, 4])
    wba_d = din("w_br_att", [1024, D])
    wbp_d = din("w_br_pool", [512, D])
    wbm_d = din("w_br_mem", [512, D])
    wout_d = din("w_out", [D, D])
    ln2g_d = din("ln2_g", [1, D])
    ln2b_d = din("ln2_b", [1, D])
    w2u_d = din("w_ffn2_up", [D, 2 * DFF])
    w2d_d = din("w_ffn2_down", [DFF, D])
    ln3g_d = din("ln3_g", [1, D])
    ln3b_d = din("ln3_b", [1, D])
    qpos_d = din("qpos", [128, NSLOT])
    iota_d = din("iota512", [1, 512])
    invc0_d = din("invc0", [1, 4 * 128])
    psel_d = din("psel", [1, 4])
    pow2_d = din("pow2", [1, 32])
    slopes_d = din("slopes", [1, 8])
    c512_d = din("c512", [1, 8])
    aq_d = din("aq", [4, T], BF16)
    ak_d = din("ak", [4, SEQ], BF16)
    h1_scr = [nc.dram_tensor("h1_scr%d" % i, [512, D], F32).ap() for i in range(2)]
    k_loc = [nc.dram_tensor("k_loc%d" % i, [512, 1024], BF16).ap() for i in range(2)]
    k_all = [nc.dram_tensor("k_all%d" % i, [4 * 512, 1024], BF16).ap() for i in range(2)]
    v_loc = [nc.dram_tensor("v_loc%d" % i, [512, 1024], BF16).ap() for i in range(2)]
    v_all = [nc.dram_tensor("v_all%d" % i, [4 * 512, 1024], BF16).ap() for i in range(2)]
    ki_loc = nc.dram_tensor("ki_loc", [64, 1024], BF16).ap()
    ki_all = nc.dram_tensor("ki_all", [4 * 64, 1024], BF16).ap()
    ut_loc = nc.dram_tensor("ut_loc", [NSLOT * 128, 64], F32).ap()
    ut_all = nc.dram_tensor("ut_all", [4 * NSLOT * 128, 64], F32).ap()
    out_d = nc.dram_tensor("out", [T, D], F32, kind="ExternalOutput").ap()

    es = ExitStack()

    def sb(name, shape, dt):
        return es.enter_context(nc.sbuf_tensor("sb_" + name, list(shape), dt))

    sems = {e: es.enter_context(nc.semaphore("s_" + e)) for e in Sched.COMPUTE}
    dsems = [es.enter_context(nc.semaphore("d%d" % i)) for i in range(24)]
    csems = [es.enter_context(nc.semaphore("c%d" % i)) for i in range(6)]
    S = Sched(nc, sems, dsems)
    S.csem = csems

    bank = [es.enter_context(nc.psum_tensor("bank%d" % i, [128, 512], F32)) for i in range(8)]

    def bk(i):
        return ("bank", i)

    hT = sb("hT", [128, NDC, T], BF16)
    acc = sb("acc", [128, NSLOT, D], F32)
    identb = sb("identb", [128, 128], BF16)
    identf = sb("identf", [128, 128], F32)
    wst = Ring([sb("wst%d" % i, [128, 2048], F32) for i in range(3)], "wst")
    wbf = Ring([sb("wbf%d" % i, [128, 2048], BF16) for i in range(3)], "wbf")
    regG = sb("regG", [128, 24 * T], BF16)
    xbf = Ring([sb("xbf%d" % i, [128, D], BF16) for i in range(2)], "xbf")
    sa_ring = Ring([sb("sa%d" % i, [128, 512], F32) for i in range(2)], "sa")
    stats = sb("stats", [128, 4, 6], F32)
    mv = sb("mv", [128, 2], F32)
    rstd = sb("rstd", [128, 1], F32)
    epsb = sb("epsb", [128, 1], F32)

    cast_rr = [0]

    cast_engs = [("act", "dve", "pool")]

    def cast(out, in_, r, w):
        engs = cast_engs[0]
        eng = engs[cast_rr[0] % len(engs)]
        cast_rr[0] += 1
        if eng == "act":
            S.add("act", lambda e: e.copy(out=out, in_=in_), r=r, w=w)
        else:
            S.add(eng, lambda e: e.tensor_copy(out=out, in_=in_), r=r, w=w)

    wbf_cur = [wbf]

    def load_w(src_ap, view):
        st, st_key = wst.next()
        bf, bf_key = wbf_cur[0].next()
        S.dma("sp", view(st), src_ap, w=[st_key])
        cast(view(bf), view(st), r=[st_key], w=[bf_key])
        return view(bf), bf_key

    S.dma("sp", identf[:, :], ident_d, w=["identf"])
    S.add("dve", lambda e: e.tensor_copy(out=identb[:, :], in_=identf[:, :]), r=["identf"], w=["identb"])
    S.add("dve", lambda e: e.memset(epsb[:, :], LN_EPS), w=["epsb"])

    tr_rr = [0]

    def to_hT(j, src_f32, src_key):
        xb, xb_key = xbf.next()
        S.add("dve", lambda e: e.tensor_copy(out=xb[:, :], in_=src_f32), r=[src_key], w=[xb_key])
        for half in range(2):
            bi = 6 + (tr_rr[0] % 2)
            tr_rr[0] += 1
            pt = bank[bi][:, :].bitcast(BF16)
            for c8 in range(8):
                c = half * 8 + c8
                S.add("pe", lambda e, c=c, c8=c8, pt=pt: e.transpose(
                    out=pt[:, c8 * 128:(c8 + 1) * 128], in_=xb[:, c * 128:(c + 1) * 128], identity=identb[:, :]),
                    r=[xb_key, "identb"], w=[bk(bi)])
            dst = hT[:, half * 8:(half + 1) * 8, j * 128:(j + 1) * 128]
            src = pt.rearrange("p (c n) -> p c n", c=8)
            S.add("act", lambda e, dst=dst, src=src: e.copy(out=dst, in_=src),
                  r=[bk(bi)], w=[("hT", j)])

    xin = Ring([regG[:, 0:2 * D].bitcast(F32), regG[:, 2 * D:4 * D].bitcast(F32)], "xin")
    for j in range(NSLOT):
        xt, xt_key = xin.next()
        S.dma("sp", xt, x_d[j * 128:(j + 1) * 128, :], w=[xt_key])
        S.add("act", lambda e, j=j, xt=xt: e.mul(out=acc[:, j, :], in_=xt, mul=ALPHA),
              r=[xt_key], w=[("acc", j)])
        to_hT(j, xt, xt_key)
    if stage == 0:
        for j in range(NSLOT):
            S.dma("sp", out_d[j * 128:(j + 1) * 128, :], acc[:, j, :], r=[("acc", j)])
        S.flush()
        es.close()
        return nc
    S.flush()

    hT_all = [("hT", j) for j in range(NSLOT)]

    def ffn(wu_d, wd_d):
        gT = regG
        wu_v = wu_d.rearrange("(c p) n -> p c n", p=128)
        wd_v = wd_d.rearrange("(c p) n -> p c n", p=128)
        groups = [(0, 24), (24, 20)]
        bno = [0]
        v3 = lambda t: t[:, :].rearrange("p (c n) -> p c n", c=16)
        v4 = lambda t: t[:, :].rearrange("p (c n) -> p c n", c=4)
        for (f0, nf) in groups:
            for fl in range(nf):
                f = f0 + fl
                wa, wa_key = load_w(wu_v[:, :, f * 128:(f + 1) * 128], v3)
                wu, wu_key = load_w(wu_v[:, :, DFF + f * 128:DFF + (f + 1) * 128], v3)
                for half in range(2):
                    ia = bno[0] % 4
                    iu = (bno[0] + 1) % 4
                    bno[0] += 2
                    for (wt, wk, ib) in ((wa, wa_key, ia), (wu, wu_key, iu)):
                        for c in range(NDC):
                            S.add("pe", lambda e, wt=wt, ib=ib, c=c, half=half: e.matmul(
                                out=bank[ib][:, :], lhsT=wt[:, c, :], rhs=hT[:, c, half * 512:(half + 1) * 512],
                                start=(c == 0), stop=(c == NDC - 1)),
                                r=[wk] + hT_all, w=[bk(ib)])
                    sa, sa_key = sa_ring.next()
                    S.add("act", lambda e, sa=sa, ia=ia: e.activation(out=sa[:, :], in_=bank[ia][:, :], func=AF.Silu),
                          r=[bk(ia)], w=[sa_key])
                    gdst = gT[:, fl * T + half * 512: fl * T + (half + 1) * 512]
                    S.add("dve", lambda e, gdst=gdst, sa=sa, iu=iu: e.tensor_tensor(
                        out=gdst, in0=sa[:, :], in1=bank[iu][:, :], op=ALU.mult),
                        r=[sa_key, bk(iu)], w=[("gT", fl, half)])
            g_all = [("gT", fl, h) for fl in range(nf) for h in range(2)]
            for dq in range(4):
                for u4 in range(nf // 4):
                    c0 = f0 + u4 * 4
                    wd, wd_key = load_w(wd_v[:, c0:c0 + 4, dq * 512:(dq + 1) * 512], v4)
                    for k4 in range(4):
                        fl = u4 * 4 + k4
                        for j in range(NSLOT):
                            S.add("pe", lambda e, fl=fl, j=j, wd=wd, k4=k4, nf=nf: e.matmul(
                                out=bank[j][:, :], lhsT=gT[:, fl * T + j * 128: fl * T + (j + 1) * 128],
                                rhs=wd[:, k4, :], start=(fl == 0), stop=(fl == nf - 1)),
                                r=[wd_key] + g_all, w=[bk(j)])
                for j in range(NSLOT):
                    dst = acc[:, j, dq * 512:(dq + 1) * 512]
                    S.add("dve", lambda e, dst=dst, j=j: e.scalar_tensor_tensor(
                        out=dst, in0=bank[j][:, :], scalar=0.5, in1=dst, op0=ALU.mult, op1=ALU.add),
                        r=[bk(j), ("acc", j)], w=[("acc", j)])
        S.flush()

    def layernorm(g_d, b_d, store_d=None, make_hT=True, post_scale=None):
        gt = regG[:, 0:2 * D].bitcast(F32)
        bt = regG[:, 2 * D:4 * D].bitcast(F32)
        S.dma("sp", gt, g_d.partition_broadcast(128) if False else g_d.broadcast_to([128, D]), w=["lng"])
        S.dma("sp", bt, b_d.broadcast_to([128, D]), w=["lnb"])
        for j in range(NSLOT):
            a_j = acc[:, j, :]
            for q in range(4):
                S.add("dve", lambda e, j=j, q=q: e.bn_stats(out=stats[:, q, :], in_=acc[:, j, q * 512:(q + 1) * 512]),
                      r=[("acc", j)], w=[("stats", q)])
            S.add("dve", lambda e: e.bn_aggr(out=mv[:, :], in_=stats[:, :, :]),
                  r=[("stats", q) for q in range(4)], w=["mv"])
            S.add("act", lambda e: e.activation(out=rstd[:, :], in_=mv[:, 1:2], func=AF.Sqrt, bias=epsb[:, :]),
                  r=["mv", "epsb"], w=["rstd"])
            S.add("dve", lambda e: e.reciprocal(out=rstd[:, :], in_=rstd[:, :]), r=["rstd"], w=["rstd"])
            S.add("dve", lambda e, a_j=a_j: e.tensor_scalar(
                out=a_j, in0=a_j, scalar1=mv[:, 0:1], scalar2=rstd[:, :], op0=ALU.subtract, op1=ALU.mult),
                r=[("acc", j), "mv", "rstd"], w=[("acc", j)])
            S.add("pool", lambda e, a_j=a_j: e.tensor_tensor(out=a_j, in0=a_j, in1=gt, op=ALU.mult),
                  r=[("acc", j), "lng"], w=[("acc", j)])
            S.add("dve", lambda e, a_j=a_j: e.tensor_tensor(out=a_j, in0=a_j, in1=bt, op=ALU.add),
                  r=[("acc", j), "lnb"], w=[("acc", j)])
            if store_d is not None:
                sd = store_d[j // 4][(j % 4) * 128:(j % 4 + 1) * 128, :] if isinstance(store_d, list) else store_d[j * 128:(j + 1) * 128, :]
                S.dma("sp", sd, a_j, r=[("acc", j)])
            if make_hT:
                to_hT(j, a_j, ("acc", j))
            if post_scale is not None:
                S.add("act", lambda e, a_j=a_j: e.mul(out=a_j, in_=a_j, mul=post_scale),
                      r=[("acc", j)], w=[("acc", j)])
        S.flush()

    ffn(w1u_d, w1d_d)
    if stage == 1:
        layernorm(ln1g_d, ln1b_d, store_d=out_d, make_hT=True)
        es.close()
        return nc
    layernorm(ln1g_d, ln1b_d, store_d=h1_scr, make_hT=True)

    accb = acc[:, :, :].rearrange("p a b -> p (a b)")
    score = accb[:, 0:4096]
    logit = accb[:, 4096:8192]
    rel = accb[:, 8192:12288]
    mb = accb[:, 12288:14336].bitcast(BF16)
    pj = accb[:, 14336:16384].bitcast(BF16)
    uT = accb[:, 0:4096].rearrange("p (g t) -> p g t", g=4)
    vtok = accb[:, 4096:8192].bitcast(BF16).rearrange("p (j n) -> p j n", j=NSLOT)
    qT = regG[:, 0:8 * T].rearrange("p (h t) -> p h t", h=8)
    qiT = regG[:, 8 * T:16 * T].rearrange("p (h t) -> p h t", h=8)
    qmT = regG[:, 16 * T:20 * T].rearrange("p (h t) -> p h t", h=4)
    pT = regG[:, 20 * T:24 * T].rearrange("p (h t) -> p h t", h=4)
    hTf = hT[:, :, :].rearrange("p c t -> p (c t)")
    kiT = hTf[:, 0:4096]
    PT = hTf[:, 4096:8192].rearrange("p (b t) -> p b t", b=32)
    kh_ring = Ring([hTf[:, 8192:12288], wst.tiles[0][:, :].bitcast(BF16)], "kh")
    vh_ring = Ring([hTf[:, 12288:16384].rearrange("p (b d) -> p b d", b=32),
                    wst.tiles[1][:, :].bitcast(BF16).rearrange("p (b d) -> p b d", b=32)], "vh")
    memT = accb[:, 8192:10240].bitcast(BF16).rearrange("p (c m) -> p c m", c=16)
    wi_t = sb("wi_t", [128, NSLOT, 16], F32)
    absw = sb("absw", [128, NSLOT, 16], F32)
    sgnw = sb("sgnw", [128, NSLOT, 16], F32)
    qpos = sb("qpos", [128, NSLOT], F32)
    qoff = sb("qoff", [128, 8], F32)
    iota = sb("iota", [128, 512], F32)
    pow2 = sb("pow2", [128, 32], F32)
    steps = sb("steps", [128, 32], F32)
    slopes = sb("slopes", [128, 8], F32)
    psel = sb("psel", [128, 4], F32)
    invc0 = sb("invc0", [128, 4, 128], F32)
    pscale = sb("pscale", [128, 4], F32)
    bgate = sb("bgate", [128, 48], F32)
    wpool_f = accb[:, 15488:16000].rearrange("p (g d) -> p g d", g=4)
    wpool_b = sb("wpool_b", [128, 4, 128], BF16)
    c512 = sb("c512", [128, 8], F32)
    negq = sb("negq", [128, 8], F32)
    sm = sb("sm", [128, 16], F32)
    lo, mid, cnt, ge, w0, rmax, rsum, rinv, hi = (sm[:, i:i + 1] for i in range(9))
    halo = accb[:, 14336:14912].rearrange("p (g t) -> p g t", g=4)
    halo2 = accb[:, 14912:15488].rearrange("p (g t) -> p g t", g=4)
    tails = accb[:, 12288:14336].rearrange("p (r j n) -> p r j n", r=4, j=NSLOT)
    kmT = sb("kmT", [128, 4, 256], BF16)
    vmt = sb("vmt", [128, 2, 512], BF16)

    for (dst, src, key) in ((qpos[:, :], qpos_d, "qpos"), (bgate[:, :], bgate_d, "bgate"),
                            (pscale[:, :], pscale_d, "pscale"), (wpool_f[:, :, :], wpool_d, "wpool_f")):
        S.dma("sp", dst, src, w=[key])
    for (dst, src, key, n) in ((iota[:, :], iota_d, "iota", 512), (pow2[:, :], pow2_d, "pow2", 32),
                               (slopes[:, :], slopes_d, "slopes", 8), (psel[:, :], psel_d, "psel", 4), (c512[:, :], c512_d, "c512", 8),
                               (invc0[:, :, :].rearrange("p g t -> p (g t)"), invc0_d, "invc0", 512)):
        S.dma("sp", dst, src.broadcast_to([128, n]), w=[key])
    S.add("dve", lambda e: e.tensor_copy(out=wpool_b[:, :, :], in_=wpool_f[:, :, :]), r=["wpool_f"], w=["wpool_b"])

    v3 = lambda t: t[:, :].rearrange("p (c n) -> p c n", c=16)
    v4 = lambda t: t[:, :].rearrange("p (c n) -> p c n", c=4)
    win_v = win_d.rearrange("(c p) n -> p c n", p=128)
    ev_rr = [0]

    def evac(out, in_, r, w, scale=None):
        eng = ("act", "dve")[ev_rr[0] % 2]
        ev_rr[0] += 1
        if eng == "act":
            S.add("act", lambda e: e.copy(out=out, in_=in_), r=r, w=w)
        else:
            S.add("dve", lambda e: e.tensor_copy(out=out, in_=in_), r=r, w=w)

    fb = [0]

    def proj_feat(w_v, col0, ncols, act_T, act_keys, nk, ntok, consume):
        view = lambda t: t[:, 0:nk * ncols].rearrange("p (c n) -> p c n", c=nk)
        wt, wk = load_w(w_v[:, 0:nk, col0:col0 + ncols], view)
        for half in range((ntok + 511) // 512):
            n = min(512, ntok - half * 512)
            ib = fb[0] % 4
            fb[0] += 1
            for c in range(nk):
                S.add("pe", lambda e, c=c, ib=ib, half=half, n=n: e.matmul(
                    out=bank[ib][0:ncols, 0:n], lhsT=wt[:, c, :], rhs=act_T[:, c, half * 512:half * 512 + n],
                    start=(c == 0), stop=(c == nk - 1)), r=[wk] + act_keys, w=[bk(ib)])
            consume(half, ib, n)

    kst = Ring([xbf.tiles[0], xbf.tiles[1]], "xbf")

    def to_sbuf(dst3, ci, key, scale=None):
        def f(half, ib, n):
            if scale is None:
                evac(dst3[:, ci, half * 512:half * 512 + n], bank[ib][:, 0:n], r=[bk(ib)], w=[(key, ci, half)])
            else:
                S.add("act", lambda e: e.mul(out=dst3[:, ci, half * 512:half * 512 + n], in_=bank[ib][:, 0:n], mul=scale),
                      r=[bk(ib)], w=[(key, ci, half)])
        return f

    for ci in range(4):
        proj_feat(win_v, 4176 + ci * 128, 128, hT, hT_all, NDC, T, to_sbuf(uT, ci, "uT"))
    for ci in range(9):
        col0, ncols = (1024 + ci * 128, 128) if ci < 8 else (4096, 64)
        st_t, st_k = kst.next()

        def to_stage(half, ib, n, st_t=st_t, st_k=st_k, ncols=ncols):
            evac(st_t[0:ncols, half * 512:half * 512 + n], bank[ib][0:ncols, 0:n], r=[bk(ib)], w=[(st_k, half)])
        proj_feat(win_v, col0, ncols, hT, hT_all, NDC, T, to_stage)
        kdst = k_loc[ci // 4][(ci % 4) * 128:(ci % 4 + 1) * 128, :] if ci < 8 else ki_loc[:, :]
        S.dma("sp", kdst, st_t[0:ncols, 0:T], r=[(st_k, 0), (st_k, 1)], w=["kv_loc"])
    for hv in range(2):
        for u4 in range(4):
            wt, wk = load_w(win_v[:, u4 * 4:u4 * 4 + 4, 2048 + hv * 512:2048 + (hv + 1) * 512], v4)
            for k4 in range(4):
                c = u4 * 4 + k4
                for j in range(NSLOT):
                    S.add("pe", lambda e, c=c, j=j, wt=wt, k4=k4: e.matmul(
                        out=bank[j][:, :], lhsT=hT[:, c, j * 128:(j + 1) * 128], rhs=wt[:, k4, :],
                        start=(c == 0), stop=(c == NDC - 1)), r=[wk] + hT_all, w=[bk(j)])
        for j in range(NSLOT):
            evac(vtok[:, j, hv * 512:(hv + 1) * 512], bank[j][:, :], r=[bk(j)], w=[("vtok", j, hv)])
    for j in range(NSLOT):
        S.dma("sp", v_loc[j // 4][(j % 4) * 128:(j % 4 + 1) * 128, :], vtok[:, j, :],
              r=[("vtok", j, 0), ("vtok", j, 1)], w=["kv_loc"])
    for j in range(NSLOT):
        S.dma("sp", ut_loc[j * 128:(j + 1) * 128, :].rearrange("p (g t) -> p g t", g=4),
              uT[:, :, j * 128 + 112:(j + 1) * 128], r=[("uT", g, h) for g in range(4) for h in range(2)], w=["ut_loc"])
    groups = [[0, 1, 2, 3], [4, 5, 6, 7]]
    for ci_, (src_, dst_) in enumerate(((k_loc[0], k_all[0]), (k_loc[1], k_all[1]), (v_loc[0], v_all[0]),
                                        (v_loc[1], v_all[1]), (ki_loc, ki_all))):
        S.coll(ci_, lambda e, src_=src_, dst_=dst_: e.collective_compute(
            "AllGather", ALU.bypass, replica_groups=groups, ins=[src_.opt()], outs=[dst_.opt()]),
            r=["kv_loc"], w=["kv_all"])
    S.coll(5, lambda e: e.collective_compute("AllGather", ALU.bypass, replica_groups=groups,
                                             ins=[ut_loc.opt()], outs=[ut_all.opt()]), r=["ut_loc"], w=["ut_all"])
    cast_engs[0] = ("act", "dve")
    for ci in range(8):
        proj_feat(win_v, ci * 128, 128, hT, hT_all, NDC, T, to_sbuf(qT, ci, "qT", scale=128.0 ** -0.5))
    for ci in range(8):
        proj_feat(win_v, 3072 + ci * 128, 128, hT, hT_all, NDC, T, to_sbuf(qiT, ci, "qiT"))
    for ci in range(4):
        proj_feat(win_v, 4688 + ci * 128, 128, hT, hT_all, NDC, T, to_sbuf(qmT, ci, "qmT"))
    vw = lambda t: t[:, 0:256].rearrange("p (c n) -> p c n", c=16)
    wt, wk = load_w(win_v[:, :, 4160:4176], vw)
    for j in range(NSLOT):
        for c in range(NDC):
            S.add("pe", lambda e, c=c, j=j, wt=wt: e.matmul(
                out=bank[j][:, 0:16], lhsT=hT[:, c, j * 128:(j + 1) * 128], rhs=wt[:, c, :],
                start=(c == 0), stop=(c == NDC - 1)), r=[wk] + hT_all, w=[bk(j)])
        S.add("dve", lambda e, j=j: e.tensor_copy(out=wi_t[:, j, :], in_=bank[j][:, 0:16]), r=[bk(j)], w=["wi_t"])
    S.add("act", lambda e: e.activation(out=sgnw[:, :, :], in_=wi_t[:, :, :], func=AF.Sign), r=["wi_t"], w=["sgnw"])
    S.add("dve", lambda e: e.scalar_tensor_tensor(out=absw[:, :, :], in0=wi_t[:, :, :], scalar=0.125 * 0.25, in1=sgnw[:, :, :],
                                                   op0=ALU.mult, op1=ALU.mult), r=["wi_t", "sgnw"], w=["absw"])
    mT_keys = []
    for mbk in range(2):
        xt, xt_key = accb[:, 10240:12288], "memx"
        S.dma("sp", xt, mem_d[mbk * 128:(mbk + 1) * 128, :], w=[xt_key])
        xb, xb_key = kst.next()
        S.add("dve", lambda e, xb=xb, xt=xt: e.tensor_copy(out=xb[:, :], in_=xt), r=[xt_key], w=[xb_key])
        for half in range(2):
            bi = 6 + half
            pt = bank[bi][:, :].bitcast(BF16)
            for c8 in range(8):
                c = half * 8 + c8
                S.add("pe", lambda e, c=c, c8=c8, pt=pt, xb=xb: e.transpose(
                    out=pt[:, c8 * 128:(c8 + 1) * 128], in_=xb[:, c * 128:(c + 1) * 128], identity=identb[:, :]),
                    r=[xb_key, "identb"], w=[bk(bi)])
            S.add("act", lambda e, half=half, mbk=mbk, pt=pt: e.copy(
                out=memT[:, half * 8:(half + 1) * 8, mbk * 128:(mbk + 1) * 128],
                in_=pt.rearrange("p (c n) -> p c n", c=8)), r=[bk(bi)], w=[("memT", mbk, half)])
            mT_keys.append(("memT", mbk, half))
    wmkv_v = wmkv_d.rearrange("(c p) n -> p c n", p=128)
    for ci in range(4):
        def to_km(half, ib, n, ci=ci):
            evac(kmT[:, ci, 0:n], bank[ib][:, 0:n], r=[bk(ib)], w=[("kmT", ci)])
        proj_feat(wmkv_v, ci * 128, 128, memT, mT_keys, NDC, 256, to_km)
    for u4 in range(4):
        wt, wk = load_w(wmkv_v[:, u4 * 4:u4 * 4 + 4, 512:1024], v4)
        for k4 in range(4):
            c = u4 * 4 + k4
            for mbk in range(2):
                S.add("pe", lambda e, c=c, mbk=mbk, wt=wt, k4=k4: e.matmul(
                    out=bank[mbk][:, :], lhsT=memT[:, c, mbk * 128:(mbk + 1) * 128], rhs=wt[:, k4, :],
                    start=(c == 0), stop=(c == NDC - 1)), r=[wk] + mT_keys, w=[bk(mbk)])
    for mbk in range(2):
        evac(vmt[:, mbk, :], bank[mbk][:, :], r=[bk(mbk)], w=[("vmt", mbk)])
    for rr in range(4):
        src = ut_all[rr * 1024:(rr + 1) * 1024, :].rearrange("(j p) n -> p j n", p=128)
        S.dma("sp", tails[:, rr, :, :], src, r=["ut_all"], w=["tails"])
    S.flush()
    cast_engs[0] = ("act", "dve", "pool")
    if stage == 20:
        es.close()
        return nc
    ak = wst.tiles[2][0:4, :].bitcast(BF16)
    aq = wbf.tiles[0][0:4, 0:1024]
    logit_ring = Ring([accb[:, 4096:8192], accb[:, 8192:12288]], "logit")
    aqs_ring = Ring([sb("aqs%d" % i, [4, 128], BF16) for i in range(2)], "aqs")
    S.dma("sp", ak, ak_d, w=["ak"])
    S.dma("sp", aq, aq_d, w=["aq"])
    for hp in range(2):
        for rr in range(4):
            dst = kiT[hp * 64:(hp + 1) * 64, :].rearrange("p (c r i) -> p c r i", c=8, r=4)[:, :, rr, :]
            src = ki_all[rr * 64:(rr + 1) * 64, :].rearrange("p (c i) -> p c i", c=8)
            S.dma("sp", dst, src, r=["kv_all"], w=["kiT"])

    u_keys = [("uT", g, h) for g in range(4) for h in range(2)]
    WIN = (2, 4, 8, 16)
    sC = sa_ring.tiles[0][:, 0:432].rearrange("p (g t) -> p g t", g=3)
    for j in range(NSLOT):
        for i in range(4):
            if i == 3 and j == 0:
                continue
            cand = (tails[:, i, j, :] if i < 3 else tails[:, 3, j - 1, :]).rearrange("p (g t) -> p g t", g=4)
            if i == 0:
                S.add("dve", lambda e, cand=cand: e.tensor_scalar(
                    out=halo[:, :, 0:16], in0=cand, scalar1=psel[:, 0:1], scalar2=None, op0=ALU.mult),
                    r=["tails", "psel"], w=["halo"])
            else:
                S.add("dve", lambda e, cand=cand, i=i: e.scalar_tensor_tensor(
                    out=halo[:, :, 0:16], in0=cand, scalar=psel[:, i:i + 1], in1=halo[:, :, 0:16],
                    op0=ALU.mult, op1=ALU.add), r=["tails", "psel", "halo"], w=["halo"])
        S.add("dve", lambda e, j=j: e.tensor_copy(out=halo[:, :, 16:144], in_=uT[:, :, j * 128:(j + 1) * 128]),
              r=u_keys + ["halo"], w=["halo"])
        S.add("dve", lambda e: e.tensor_tensor(out=halo2[:, :, 1:144], in0=halo[:, :, 1:144], in1=halo[:, :, 0:143],
                                                op=ALU.add), r=["halo"], w=["halo2"])
        pb, pb_key = kst.next()
        pooled = pb[:, 0:512].rearrange("p (g t) -> p g t", g=4)

        def emit_pooled(g, src, src_key, j=j, pooled=pooled, pb_key=pb_key):
            if j == 0:
                S.add("dve", lambda e: e.tensor_tensor(out=src, in0=src, in1=invc0[:, g, :], op=ALU.mult),
                      r=[src_key, "invc0"], w=[src_key])
                S.add("dve", lambda e: e.tensor_tensor(out=pooled[:, g, :], in0=src, in1=halo[:, g, 16:144],
                                                        op=ALU.subtract), r=[src_key, "halo"], w=[(pb_key, g)])
            else:
                S.add("dve", lambda e: e.scalar_tensor_tensor(
                    out=pooled[:, g, :], in0=src, scalar=1.0 / WIN[g], in1=halo[:, g, 16:144],
                    op0=ALU.mult, op1=ALU.subtract), r=[src_key, "halo"], w=[(pb_key, g)])

        S.add("dve", lambda e: e.tensor_tensor(out=sC[:, :, 3:144], in0=halo2[:, 1:4, 3:144], in1=halo2[:, 1:4, 1:142],
                                                op=ALU.add), r=["halo2"], w=["sC"])
        emit_pooled(0, halo2[:, 0, 16:144], "halo2")
        S.add("dve", lambda e: e.tensor_tensor(out=halo2[:, 2:4, 7:144], in0=sC[:, 1:3, 7:144], in1=sC[:, 1:3, 3:140],
                                                op=ALU.add), r=["sC", "halo2"], w=["halo2"])
        emit_pooled(1, sC[:, 0, 16:144], "sC")
        S.add("dve", lambda e: e.tensor_tensor(out=sC[:, 2, 15:144], in0=halo2[:, 3, 15:144], in1=halo2[:, 3, 7:136],
                                                op=ALU.add), r=["halo2", "sC"], w=["sC"])
        emit_pooled(2, halo2[:, 2, 16:144], "halo2")
        emit_pooled(3, sC[:, 2, 16:144], "sC")
        for g in range(4):
            ib = fb[0] % 4
            fb[0] += 1
            S.add("pe", lambda e, g=g, ib=ib, pooled=pooled: e.matmul(
                out=bank[ib][:, 0:128], lhsT=wpool_b[:, g, :], rhs=pooled[:, g, :], start=True, stop=True),
                r=["wpool_b", (pb_key, g)], w=[bk(ib)])
            S.add("act", lambda e, g=g, ib=ib, j=j: e.activation(
                out=pT[:, g, j * 128:(j + 1) * 128], in_=bank[ib][:, 0:128], func=AF.Copy, scale=pscale[:, g:g + 1]),
                r=[bk(ib), "pscale"], w=[("pT", j)])
    S.flush()

    NIT = 20
    ATT_SCALE = 128.0 ** -0.5
    def indexer_units(j):
        units = []
        for c in range(j + 1):
            for h in range(16 if not TEST_SKIP else 1):
                def unit(c=c, h=h, j=j):
                    ib = fb[0] % 4
                    fb[0] += 1
                    p0 = (h % 2) * 64
                    S.add("pe", lambda e: e.matmul(
                        out=bank[ib][:, :], lhsT=qiT[p0:p0 + 64, h // 2, j * 128:(j + 1) * 128],
                        rhs=kiT[p0:p0 + 64, c * 512:(c + 1) * 512], start=True, stop=True),
                        r=[("qiT", h // 2, 0), ("qiT", h // 2, 1), "kiT"], w=[bk(ib)])
                    rt, rt_key = sa_ring.next()
                    S.add("act", lambda e: e.activation(
                        out=rt[:, :], in_=bank[ib][:, :], func=AF.Relu, scale=absw[:, j, h:h + 1]),
                        r=[bk(ib), "absw"], w=[rt_key])
                    sc = score[:, c * 512:(c + 1) * 512]
                    if h == 0:
                        S.add("dve", lambda e: e.tensor_scalar(
                            out=sc, in0=rt[:, :], scalar1=sgnw[:, j, h:h + 1], scalar2=None, op0=ALU.mult),
                            r=[rt_key, "sgnw"], w=[("score", c)])
                    else:
                        S.add("dve", lambda e: e.scalar_tensor_tensor(
                            out=sc, in0=rt[:, :], scalar=sgnw[:, j, h:h + 1], in1=sc, op0=ALU.mult, op1=ALU.add),
                            r=[rt_key, "sgnw", ("score", c)], w=[("score", c)])
                units.append(unit)
        return units

    for j in range(NSLOT):
        nk = 512 * (j + 1)
        nb = 4 * (j + 1)
        S.add("dve", lambda e, j=j: e.tensor_scalar(out=negq[:, :], in0=c512[:, :], scalar1=qpos[:, j:j + 1],
                                                     scalar2=None, op0=ALU.subtract), r=["c512", "qpos"], w=["negq"])
        if j == 0:
            for u_ in indexer_units(0):
                u_()
        sc_keys = [("score", c) for c in range(j + 1)]
        S.add("dve", lambda e, nk=nk: e.tensor_reduce(out=hi, in_=score[:, 0:nk], axis=mybir.AxisListType.X, op=ALU.max),
              r=sc_keys, w=["hi"])
        S.add("dve", lambda e, nk=nk: e.tensor_reduce(out=lo, in_=score[:, 0:nk], axis=mybir.AxisListType.X, op=ALU.min),
              r=sc_keys, w=["lo"])
        S.add("dve", lambda e: e.tensor_scalar(out=lo, in0=lo, scalar1=-1.0, scalar2=None, op0=ALU.add), r=["lo"], w=["lo"])
        S.add("dve", lambda e: e.scalar_tensor_tensor(out=w0, in0=hi, scalar=1.0, in1=lo, op0=ALU.add, op1=ALU.subtract),
              r=["hi", "lo"], w=["w0"])
        S.add("dve", lambda e: e.tensor_scalar(out=steps[:, :], in0=pow2[:, :], scalar1=w0, scalar2=None, op0=ALU.mult),
              r=["pow2", "w0"], w=["steps"])
        cm, cm_key = sa_ring.next()
        lastc = score[:, j * 512:(j + 1) * 512]
        S.add("dve", lambda e, cm=cm, j=j: e.tensor_scalar(out=cm[:, :], in0=iota[:, :], scalar1=negq[:, j:j + 1], scalar2=0.0,
                                                            op0=ALU.add, op1=ALU.is_le), r=["iota", "negq"], w=[cm_key])
        S.add("dve", lambda e, cm=cm, lastc=lastc: e.tensor_tensor(out=lastc, in0=lastc, in1=cm[:, :], op=ALU.mult),
              r=[cm_key, ("score", j)], w=[("score", j)])
        S.add("dve", lambda e, cm=cm: e.tensor_scalar(out=cm[:, :], in0=cm[:, :], scalar1=1e30, scalar2=-1e30,
                                                       op0=ALU.mult, op1=ALU.add), r=[cm_key], w=[cm_key])
        S.add("dve", lambda e, cm=cm, lastc=lastc: e.tensor_tensor(out=lastc, in0=lastc, in1=cm[:, :], op=ALU.add),
              r=[cm_key, ("score", j)], w=[("score", j)])
        for it in range(NIT if not TEST_SKIP else 1):
            S.add("dve", lambda e, it=it: e.tensor_tensor(out=mid, in0=lo, in1=steps[:, it:it + 1], op=ALU.add),
                  r=["lo", "steps"], w=["mid"])
            S.add("dve", lambda e, nk=nk: e.tensor_scalar(out=pj[:, 0:nk], in0=score[:, 0:nk], scalar1=mid, scalar2=None,
                                                           op0=ALU.is_ge, op1=ALU.add, accum_out=cnt),
                  r=sc_keys + ["mid"], w=["pj", "cnt"])
            S.add("dve", lambda e: e.tensor_scalar(out=ge, in0=cnt, scalar1=255.5, scalar2=None, op0=ALU.is_ge),
                  r=["cnt"], w=["ge"])
            S.add("dve", lambda e, it=it: e.scalar_tensor_tensor(out=lo, in0=ge, scalar=steps[:, it:it + 1], in1=lo,
                                                                  op0=ALU.mult, op1=ALU.add), r=["ge", "steps", "lo"], w=["lo"])
        S.add("dve", lambda e, nk=nk: e.tensor_scalar(out=mb[:, 0:nk], in0=score[:, 0:nk], scalar1=lo, scalar2=None,
                                                       op0=ALU.is_ge), r=sc_keys + ["lo"], w=["mb"])
        S.add("dve", lambda e, nk=nk: e.tensor_scalar(out=mb[:, 0:nk], in0=mb[:, 0:nk], scalar1=30000.0, scalar2=-30000.0,
                                                       op0=ALU.mult, op1=ALU.add), r=["mb"], w=["mb"])
        atok, atok_key = kst.next()
        nxt_units = indexer_units(j + 1) if j + 1 < NSLOT else []
        per_head = len(nxt_units) // 8
        for h in range(8):
            kh, kh_key = kh_ring.next()
            rmax_h, rsum_h, rinv_h = sm[:, 5 + 4 * (h % 2) + 0:5 + 4 * (h % 2) + 1] if False else (sm[:, (5, 9)[h % 2]:(5, 9)[h % 2] + 1]), sm[:, (6, 10)[h % 2]:(6, 10)[h % 2] + 1], sm[:, (7, 11)[h % 2]:(7, 11)[h % 2] + 1]
            kx = h % 2
            vh, vh_key = vh_ring.next()
            for rr in range(4):
                ksrc = k_all[h // 4][rr * 512 + (h % 4) * 128:rr * 512 + (h % 4 + 1) * 128, 0:128 * (j + 1)]
                S.dma("sp", kh[:, 0:nk].rearrange("p (c r i) -> p c r i", c=j + 1, r=4)[:, :, rr, :],
                      ksrc.rearrange("p (c i) -> p c i", c=j + 1), r=[], w=[kh_key])
                for vp in range(2):
                    ncp = min(4, j + 1 - 4 * vp)
                    if ncp <= 0:
                        continue
                    vsrc = v_all[vp][rr * 512:rr * 512 + 128 * ncp, h * 128:(h + 1) * 128]
                    S.dma("sp", vh[:, 16 * vp:16 * vp + 4 * ncp, :].rearrange("p (c r) d -> p c r d", r=4)[:, :, rr, :],
                          vsrc.rearrange("(c p) d -> p c d", p=128), r=[], w=[vh_key])
            aqs, aqs_key = aqs_ring.next()
            lg, lg_key = logit_ring.next()
            S.add("pool", lambda e, aqs=aqs, h=h, j=j: e.tensor_scalar(
                out=aqs[:, :], in0=aq[:, j * 128:(j + 1) * 128], scalar1=2.0 ** -(h + 1), scalar2=None, op0=ALU.mult),
                r=["aq"], w=[aqs_key])
            for c in range(j + 1):
                ib = fb[0] % 4
                fb[0] += 1
                cs = slice(c * 512, (c + 1) * 512)
                S.add("pe", lambda e, cs=cs, h=h, ib=ib, kh=kh, j=j: e.matmul(
                    out=bank[ib][:, :], lhsT=qT[:, h, j * 128:(j + 1) * 128], rhs=kh[:, cs],
                    start=True, stop=False), r=[("qT", h, 0), ("qT", h, 1), kh_key], w=[bk(ib)])
                S.add("pe", lambda e, cs=cs, ib=ib, aqs=aqs: e.matmul(
                    out=bank[ib][:, :], lhsT=aqs[:, :], rhs=ak[:, cs], start=False, stop=False),
                    r=[aqs_key, "ak"], w=[bk(ib)])
                S.add("pe", lambda e, cs=cs, ib=ib: e.matmul(
                    out=bank[ib][:, :], lhsT=identb[:, :], rhs=mb[:, cs], start=False, stop=True),
                    r=["identb", "mb"], w=[bk(ib)])
                S.add("act", lambda e, ib=ib, cs=cs, lg=lg: e.copy(out=lg[:, cs], in_=bank[ib][:, :]),
                      r=[bk(ib)], w=[lg_key])
            S.add("dve", lambda e, nk=nk, lg=lg, rmax_h=rmax_h: e.tensor_reduce(out=rmax_h, in_=lg[:, 0:nk], axis=mybir.AxisListType.X,
                                                                  op=ALU.max, negate=True), r=[lg_key], w=[("rmax", kx)])
            S.add("act", lambda e, nk=nk, lg=lg, rmax_h=rmax_h, rsum_h=rsum_h: e.activation(out=pj[:, 0:nk], in_=lg[:, 0:nk], func=AF.Exp, bias=rmax_h,
                                                               accum_out=rsum_h), r=[lg_key, ("rmax", kx)], w=["pj", ("rsum", kx)])
            S.add("dve", lambda e, rinv_h=rinv_h, rsum_h=rsum_h: e.reciprocal(out=rinv_h, in_=rsum_h), r=[("rsum", kx)], w=[("rinv", kx)])
            for b8 in range((nb + 7) // 8):
                bi = 4 + (tr_rr[0] % 2)
                tr_rr[0] += 1
                n8 = min(8, nb - b8 * 8)
                pt = bank[bi][:, :].bitcast(BF16)
                for k in range(n8):
                    blk = b8 * 8 + k
                    S.add("pe", lambda e, k=k, blk=blk, pt=pt: e.transpose(
                        out=pt[:, k * 128:(k + 1) * 128], in_=pj[:, blk * 128:(blk + 1) * 128], identity=identb[:, :]),
                        r=["pj", "identb"], w=[bk(bi)])
                S.add("act", lambda e, b8=b8, n8=n8, pt=pt: e.copy(
                    out=PT[:, b8 * 8:b8 * 8 + n8, :], in_=pt[:, 0:n8 * 128].rearrange("p (b t) -> p b t", b=n8)),
                    r=[bk(bi)], w=[("PT", b8)])
            ib = 6 + (h % 2)
            pt_keys = [("PT", b8) for b8 in range((nb + 7) // 8)]
            for blk in range(nb):
                S.add("pe", lambda e, blk=blk, ib=ib, vh=vh, nb=nb: e.matmul(
                    out=bank[ib][:, 0:128], lhsT=PT[:, blk, :], rhs=vh[:, blk, :], start=(blk == 0), stop=(blk == nb - 1)),
                    r=pt_keys + [vh_key], w=[bk(ib)])
            S.add("act", lambda e, ib=ib, h=h, atok=atok, rinv_h=rinv_h: e.activation(
                out=atok[:, h * 128:(h + 1) * 128], in_=bank[ib][:, 0:128], func=AF.Copy, scale=rinv_h),
                r=[bk(ib), ("rinv", kx)], w=[(atok_key, h)])
            if j + 1 < NSLOT:
                for u_ in nxt_units[h * per_head:(h + 1) * per_head if h < 7 else len(nxt_units)]:
                    u_()
        bi = 4 + (tr_rr[0] % 2)
        tr_rr[0] += 1
        pt = bank[bi][:, :].bitcast(BF16)
        for h in range(8):
            S.add("pe", lambda e, h=h, pt=pt, atok=atok: e.transpose(
                out=pt[:, h * 128:(h + 1) * 128], in_=atok[:, h * 128:(h + 1) * 128], identity=identb[:, :]),
                r=[(atok_key, h), "identb"], w=[bk(bi)])
        S.add("act", lambda e, pt=pt, j=j: e.copy(out=qT[:, :, j * 128:(j + 1) * 128],
                                                   in_=pt.rearrange("p (h t) -> p h t", h=8)),
              r=[bk(bi)], w=[("qT", h, j // 4) for h in range(8)])
        mtok, mtok_key = kst.next()
        for h in range(4):
            ib = fb[0] % 4
            fb[0] += 1
            S.add("pe", lambda e, h=h, ib=ib, j=j: e.matmul(
                out=bank[ib][:, 0:256], lhsT=qmT[:, h, j * 128:(j + 1) * 128], rhs=kmT[:, h, :], start=True, stop=True),
                r=[("qmT", h, 0), ("qmT", h, 1), ("kmT", h)], w=[bk(ib)])
            S.add("dve", lambda e, ib=ib: e.tensor_reduce(out=rmax, in_=bank[ib][:, 0:256], axis=mybir.AxisListType.X,
                                                           op=ALU.max), r=[bk(ib)], w=["rmax"])
            S.add("dve", lambda e: e.tensor_scalar(out=rmax, in0=rmax, scalar1=-ATT_SCALE, scalar2=None, op0=ALU.mult),
                  r=["rmax"], w=["rmax"])
            S.add("act", lambda e, ib=ib: e.activation(out=pj[:, 0:256], in_=bank[ib][:, 0:256], func=AF.Exp, bias=rmax,
                                                        scale=ATT_SCALE, accum_out=rsum), r=[bk(ib), "rmax"], w=["pj", "rsum"])
            S.add("dve", lambda e: e.reciprocal(out=rinv, in_=rsum), r=["rsum"], w=["rinv"])
            bi = 4 + (tr_rr[0] % 2)
            tr_rr[0] += 1
            pt = bank[bi][:, :].bitcast(BF16)
            for k in range(2):
                S.add("pe", lambda e, k=k, pt=pt: e.transpose(
                    out=pt[:, k * 128:(k + 1) * 128], in_=pj[:, k * 128:(k + 1) * 128], identity=identb[:, :]),
                    r=["pj", "identb"], w=[bk(bi)])
            evac(PT[:, 0:2, :], pt[:, 0:256].rearrange("p (b t) -> p b t", b=2), r=[bk(bi)], w=[("PT", 0)])
            ib2 = 6 + (h % 2)
            for k in range(2):
                S.add("pe", lambda e, k=k, ib2=ib2, h=h: e.matmul(
                    out=bank[ib2][:, 0:128], lhsT=PT[:, k, :], rhs=vmt[:, k, h * 128:(h + 1) * 128],
                    start=(k == 0), stop=(k == 1)), r=[("PT", 0), ("vmt", 0), ("vmt", 1)], w=[bk(ib2)])
            S.add("act", lambda e, ib2=ib2, h=h, mtok=mtok: e.activation(
                out=mtok[:, h * 128:(h + 1) * 128], in_=bank[ib2][:, 0:128], func=AF.Copy, scale=rinv),
                r=[bk(ib2), "rinv"], w=[(mtok_key, h)])
        bi = 4 + (tr_rr[0] % 2)
        tr_rr[0] += 1
        pt = bank[bi][:, :].bitcast(BF16)
        for h in range(4):
            S.add("pe", lambda e, h=h, pt=pt, mtok=mtok: e.transpose(
                out=pt[:, h * 128:(h + 1) * 128], in_=mtok[:, h * 128:(h + 1) * 128], identity=identb[:, :]),
                r=[(mtok_key, h), "identb"], w=[bk(bi)])
        S.add("act", lambda e, pt=pt, j=j: e.copy(out=qmT[:, :, j * 128:(j + 1) * 128],
                                                   in_=pt[:, 0:512].rearrange("p (h t) -> p h t", h=4)),
              r=[bk(bi)], w=[("qmT", h, j // 4) for h in range(4)])
    S.flush()

    if stage == 3:
        for c in range(16):
            src = qT[:, c, :] if c < 8 else (pT[:, c - 8, :] if c < 12 else qmT[:, c - 12, :])
            st, st_key = wst.next()
            S.add("dve", lambda e, st=st, src=src: e.tensor_copy(out=st[:, 0:1024], in_=src), w=[st_key])
            S.dma("sp", out_d[c * 64:(c + 1) * 64, :].rearrange("r (pl t) -> (r pl) t", pl=2), st[:, 0:1024], r=[st_key])
        S.flush()
        es.close()
        return nc
    for j in range(NSLOT):
        xt, xt_key = accb[:, 0:2048] if False else (wst.tiles[j % 2][:, :], ("wst", j % 2))
        S.dma("sp", xt, h1_scr[j // 4][(j % 4) * 128:(j % 4 + 1) * 128, :], w=[xt_key])
        S.add("act", lambda e, j=j, xt=xt: e.mul(out=acc[:, j, :], in_=xt, mul=ALPHA), r=[xt_key], w=[("acc", j)])
        to_hT(j, xt, xt_key)
    S.flush()
    yT = qiT
    wbf_cur[0] = Ring(wbf.tiles + [regG[:, 12 * T:14 * T], regG[:, 14 * T:16 * T]], "wbf")
    sg = [xbf.tiles[0][:, 0:1024].bitcast(F32), xbf.tiles[0][:, 1024:2048].bitcast(F32),
          xbf.tiles[1][:, 0:1024].bitcast(F32), xbf.tiles[1][:, 1024:2048].bitcast(F32)]
    wba_v = wba_d.rearrange("(c p) n -> p c n", p=128)
    wbp_v = wbp_d.rearrange("(c p) n -> p c n", p=128)
    wbm_v = wbm_d.rearrange("(c p) n -> p c n", p=128)
    wout_v = wout_d.rearrange("(c p) n -> p c n", p=128)
    for G in range(4):
        for n4 in range(4):
            n = G * 4 + n4
            wts = []
            for i in range(3):
                col0 = 5200 + i * D + n * 128
                wts.append(load_w(win_v[:, :, col0:col0 + 128], v3))
            st, st_key = wst.next()
            bf, bf_key = wbf_cur[0].next()
            cols = slice(n * 128, (n + 1) * 128)
            vv = lambda t, a, b, c: t[:, a:b].rearrange("p (c n) -> p c n", c=c)
            S.dma("sp", vv(st, 0, 1024, 8), wba_v[:, :, cols], w=[st_key])
            S.dma("sp", vv(st, 1024, 1536, 4), wbp_v[:, :, cols], w=[st_key])
            S.dma("sp", vv(st, 1536, 2048, 4), wbm_v[:, :, cols], w=[st_key])
            cast(bf[:, :], st[:, :], r=[st_key], w=[bf_key])
            wb = [(vv(bf, 0, 1024, 8), bf_key), (vv(bf, 1024, 1536, 4), bf_key), (vv(bf, 1536, 2048, 4), bf_key)]
            brs = [(qT, 8), (pT, 4), (qmT, 4)]
            for half in range(2):
                ts = slice(half * 512, (half + 1) * 512)
                for i in range(3):
                    wt, wk = wts[i]
                    for c in range(NDC):
                        S.add("pe", lambda e, wt=wt, c=c, i=i, ts=ts: e.matmul(
                            out=bank[i][:, :], lhsT=wt[:, c, :], rhs=hT[:, c, ts], start=(c == 0), stop=(c == NDC - 1)),
                            r=[wk] + hT_all, w=[bk(i)])
                    S.add("act", lambda e, i=i, n=n: e.activation(
                        out=sg[i], in_=bank[i][:, :], func=AF.Sigmoid, bias=bgate[:, i * 16 + n:i * 16 + n + 1]),
                        r=[bk(i), "bgate"], w=[("sg", i)])
                for i in range(3):
                    wt, wk = wb[i]
                    src, nkc = brs[i]
                    for c in range(nkc):
                        S.add("pe", lambda e, wt=wt, c=c, i=i, ts=ts, src=src, nkc=nkc: e.matmul(
                            out=bank[3 + i][:, :], lhsT=wt[:, c, :], rhs=src[:, c, ts], start=(c == 0), stop=(c == nkc - 1)),
                            r=[wk], w=[bk(3 + i)])
                    S.add("dve", lambda e, i=i: e.tensor_tensor(out=sg[i], in0=sg[i], in1=bank[3 + i][:, :], op=ALU.mult),
                          r=[("sg", i), bk(3 + i)], w=[("sg", i)])
                S.add("dve", lambda e: e.tensor_tensor(out=sg[0], in0=sg[0], in1=sg[1], op=ALU.add),
                      r=[("sg", 0), ("sg", 1)], w=[("sg", 0)])
                S.add("dve", lambda e, n4=n4, ts=ts: e.tensor_tensor(out=yT[:, n4, ts], in0=sg[0], in1=sg[2], op=ALU.add),
                      r=[("sg", 0), ("sg", 2)], w=[("yT", n4, half)])
        y_all = [("yT", n4, h) for n4 in range(4) for h in range(2)]
        for dq in range(4):
            wt, wk = load_w(wout_v[:, G * 4:G * 4 + 4, dq * 512:(dq + 1) * 512], v4)
            for k4 in range(4):
                for j in range(NSLOT):
                    S.add("pe", lambda e, k4=k4, j=j, wt=wt: e.matmul(
                        out=bank[j][:, :], lhsT=yT[:, k4, j * 128:(j + 1) * 128], rhs=wt[:, k4, :],
                        start=(k4 == 0), stop=(k4 == 3)), r=[wk] + y_all, w=[bk(j)])
            for j in range(NSLOT):
                dst = acc[:, j, dq * 512:(dq + 1) * 512]
                S.add("dve", lambda e, dst=dst, j=j: e.tensor_tensor(out=dst, in0=dst, in1=bank[j][:, :], op=ALU.add),
                      r=[bk(j), ("acc", j)], w=[("acc", j)])
    S.flush()
    wbf_cur[0] = wbf
    if stage == 4:
        layernorm(ln2g_d, ln2b_d, store_d=out_d, make_hT=True, post_scale=ALPHA)
        es.close()
        return nc
    layernorm(ln2g_d, ln2b_d, store_d=None, make_hT=True, post_scale=ALPHA)

    ffn(w2u_d, w2d_d)
    layernorm(ln3g_d, ln3b_d, store_d=out_d, make_hT=False)

    es.close()
    return nc


def _prep_inputs(inputs):
    f = lambda k: np.asarray(inputs[k], dtype=np.float32)
    x = f("x")
    mem = f("mem")
    shared = {"ident": np.eye(128, dtype=np.float32)}
    for k in ("w_ffn1_up", "w_ffn1_down", "w_in", "w_mem_kv", "w_br_att", "w_br_pool", "w_br_mem", "w_out",
              "w_ffn2_up", "w_ffn2_down"):
        shared[k] = np.ascontiguousarray(f(k)[0])
    for k in ("ln1_g", "ln1_b", "ln2_g", "ln2_b", "ln3_g", "ln3_b"):
        shared[k] = np.ascontiguousarray(f(k).reshape(1, D))
    shared["b_gate"] = np.ascontiguousarray(f("b_gate").reshape(48, 128).T)
    shared["w_pool"] = np.ascontiguousarray(f("w_pool")[0].transpose(1, 0, 2))
    shared["pool_scale"] = np.ascontiguousarray(f("pool_scale").reshape(4, 128).T)
    shared["iota512"] = np.arange(512, dtype=np.float32).reshape(1, 512)
    shared["c512"] = (512.0 * np.arange(8, dtype=np.float32)).reshape(1, 8)
    shared["pow2"] = (2.0 ** -(np.arange(32, dtype=np.float64) + 1)).astype(np.float32).reshape(1, 32)
    shared["slopes"] = (2.0 ** -(np.arange(8, dtype=np.float64) + 1)).astype(np.float32).reshape(1, 8)
    wins = np.array([2, 4, 8, 16], dtype=np.float32)
    import ml_dtypes
    kp = np.arange(SEQ)
    shared["ak"] = np.stack([kp // 64, kp % 64, np.ones(SEQ), np.ones(SEQ)]).astype(np.float32).astype(ml_dtypes.bfloat16)
    maps = []
    for c in range(NCORE):
        b, r = divmod(c, 4)
        m = dict(shared)
        m["x"] = np.ascontiguousarray(x[b].reshape(32, 128, D)[r::4].reshape(T, D))
        m["mem"] = np.ascontiguousarray(mem[b])
        p = np.arange(128, dtype=np.float32)[:, None]
        j = np.arange(NSLOT, dtype=np.float32)[None, :]
        m["qpos"] = np.ascontiguousarray((4 * j + r) * 128 + p).astype(np.float32)
        t = np.arange(128, dtype=np.float32)[None, :]
        if r == 0:
            invc = 1.0 / np.minimum(t + 1.0, wins[:, None])
        else:
            invc = np.broadcast_to(1.0 / wins[:, None], (4, 128))
        m["invc0"] = np.ascontiguousarray(invc, dtype=np.float32).reshape(1, 512)
        sel = np.zeros((1, 4), dtype=np.float32)
        sel[0, (r - 1) % 4] = 1.0
        m["psel"] = sel
        qp = ((4 * np.arange(NSLOT)[:, None] + r) * 128 + np.arange(128)[None, :]).reshape(-1)
        m["aq"] = np.stack([np.full(T, 64.0), np.ones(T), -64.0 * (qp // 64), -1.0 * (qp % 64)]).astype(np.float32).astype(ml_dtypes.bfloat16)
        maps.append(m)
    return maps


def _assemble(results):
    out = np.zeros((2, SEQ, D), dtype=np.float32)
    for c in range(NCORE):
        b, r = divmod(c, 4)
        o = np.asarray(results[c]["out"]).reshape(NSLOT, 128, D)
        out[b].reshape(32, 128, D)[r::4] = o
    return out


def kernel(**inputs):
    nc = build_program(stage=DEBUG_STAGE)
    maps = _prep_inputs(inputs)
    res = run_bass_kernel_spmd(nc, maps, core_ids=list(range(NCORE)))
    return _assemble(res.results)
```
